# Optimizing a Trainium2 kernel written in Bass

```python
import jax, jax.numpy as jnp
from jax import lax
import numpy as np

D_MODEL = 1024
BATCH = 2
SEQ = 16384
DEPTH = 4

GRID_W = 64
CTX_LEN = 256
HEAD_DIM = 64
ROPE_THETA = 10000.0
Q_BLOCK = 128
RMS_EPS = 1e-6
H_A = 8
NOPE_A = 64
ROPE_A = 32
V_A = 64
Q_LORA = 384
KV_LORA = 256
H_B = 8
KV_B = 2
G_B = H_B // KV_B
AB_IN = Q_LORA + KV_LORA + ROPE_A + H_B * HEAD_DIM + 2 * KV_B * HEAD_DIM
AB_OUT = H_A * V_A + H_B * HEAD_DIM
H_C = 16
WIN_R = 8
WIN_C = 16
C_WIDTH = H_C * HEAD_DIM
N_GROUPS = 4
EXPERTS_PER_GROUP = 8
N_EXPERTS = N_GROUPS * EXPERTS_PER_GROUP
TOP_K = 2
D_EXPERT = 512
MOE_BLOCK = 128
N_EVEN = (DEPTH + 1) // 2
N_ODD = DEPTH // 2

kernel_name = "hybrid_mla_gqa_natten_hmoe_prefix_dit"


def rmsnorm(x, g):
    xf = x.astype(jnp.float32)
    y = xf * lax.rsqrt(jnp.mean(xf * xf, axis=-1, keepdims=True) + RMS_EPS)
    return (y * g.astype(jnp.float32)).astype(x.dtype)


def modulate(x, g, shift, scale):
    return rmsnorm(x, g) * (1 + scale) + shift


def axial_rope_table(rows, cols, rot_dim):
    n = rot_dim // 4
    inv = ROPE_THETA ** (-jnp.arange(n, dtype=jnp.float32) / n)
    ang = jnp.concatenate([rows[:, None] * inv, cols[:, None] * inv], axis=-1)
    return jnp.cos(ang), jnp.sin(ang)


def apply_rope(x, rope):
    cos, sin = rope
    cos = cos.astype(x.dtype)
    sin = sin.astype(x.dtype)
    x1 = x[..., 0::2]
    x2 = x[..., 1::2]
    return jnp.stack([x1 * cos - x2 * sin, x1 * sin + x2 * cos], axis=-1).reshape(x.shape)


def heads_to_tokens(o):
    b, h, t, d = o.shape
    return o.transpose(0, 2, 1, 3).reshape(b, t, h * d)


def attend(q, k, v):
    s = jnp.einsum('bhgqd,bhkd->bhgqk', q, k, preferred_element_type=jnp.float32)
    p = jax.nn.softmax(s, axis=-1).astype(v.dtype)
    return jnp.einsum('bhgqk,bhkd->bhgqd', p, v)


def prefix_attention(q, k_lat, v_lat, k_ctx, v_ctx):
    b, hk, g, s, d = q.shape
    nb = s // Q_BLOCK
    k_all = jnp.concatenate([k_ctx, k_lat], axis=2)
    v_all = jnp.concatenate([v_ctx, v_lat], axis=2)
    qb = q.reshape(b, hk, g, nb, Q_BLOCK, d).transpose(3, 0, 1, 2, 4, 5)
    o = lax.map(lambda qblk: attend(qblk, k_all, v_all), qb)
    return o.transpose(1, 2, 3, 0, 4, 5).reshape(b, hk, g, s, v_all.shape[-1])


def mixer_ab(h_ctx, h_lat, rope_a, rope_b, w_in, w_out, q_norm, w_q_up, kv_norm, w_kv_up, gq_norm, gk_norm, need_ctx):
    def project(h, ra, rb):
        bsz, t, _ = h.shape
        p = h @ w_in
        o0 = Q_LORA
        o1 = o0 + KV_LORA
        o2 = o1 + ROPE_A
        o3 = o2 + H_B * HEAD_DIM
        o4 = o3 + KV_B * HEAD_DIM
        cq, ckv, kr = p[..., :o0], p[..., o0:o1], p[..., o1:o2]
        gq, gk, gv = p[..., o2:o3], p[..., o3:o4], p[..., o4:]
        qa = (rmsnorm(cq, q_norm) @ w_q_up).reshape(bsz, t, H_A, NOPE_A + ROPE_A).transpose(0, 2, 1, 3)
        kva = (rmsnorm(ckv, kv_norm) @ w_kv_up).reshape(bsz, t, H_A, NOPE_A + V_A).transpose(0, 2, 1, 3)
        q_nope, q_rope = qa[..., :NOPE_A], qa[..., NOPE_A:]
        k_nope, va = kva[..., :NOPE_A], kva[..., NOPE_A:]
        k_rope = kr[:, None]
        if ra is not None:
            q_rope = apply_rope(q_rope, ra)
            k_rope = apply_rope(k_rope, ra)
        qa = jnp.concatenate([q_nope, q_rope], axis=-1) * ((NOPE_A + ROPE_A) ** -0.5)
        ka = jnp.concatenate([k_nope, jnp.broadcast_to(k_rope, (bsz, H_A, t, ROPE_A))], axis=-1)
        qb = rmsnorm(gq.reshape(bsz, t, H_B, HEAD_DIM).transpose(0, 2, 1, 3), gq_norm)
        kb = rmsnorm(gk.reshape(bsz, t, KV_B, HEAD_DIM).transpose(0, 2, 1, 3), gk_norm)
        vb = gv.reshape(bsz, t, KV_B, HEAD_DIM).transpose(0, 2, 1, 3)
        if rb is not None:
            qb = apply_rope(qb, rb)
            kb = apply_rope(kb, rb)
        qb = qb.reshape(bsz, KV_B, G_B, t, HEAD_DIM) * (HEAD_DIM ** -0.5)
        return qa[:, :, None], ka, va, qb, kb, vb

    qa_l, ka_l, va_l, qb_l, kb_l, vb_l = project(h_lat, rope_a, rope_b)
    qa_c, ka_c, va_c, qb_c, kb_c, vb_c = project(h_ctx, None, None)
    bsz, s = h_lat.shape[0], h_lat.shape[1]
    oa = prefix_attention(qa_l, ka_l, va_l, ka_c, va_c).reshape(bsz, H_A, s, V_A)
    ob = prefix_attention(qb_l, kb_l, vb_l, kb_c, vb_c).reshape(bsz, H_B, s, HEAD_DIM)
    out_lat = jnp.concatenate([heads_to_tokens(oa), heads_to_tokens(ob)], axis=-1) @ w_out
    out_ctx = None
    if need_ctx:
        t = h_ctx.shape[1]
        oa_c = attend(qa_c, ka_c, va_c).reshape(bsz, H_A, t, V_A)
        ob_c = attend(qb_c, kb_c, vb_c).reshape(bsz, H_B, t, HEAD_DIM)
        out_ctx = jnp.concatenate([heads_to_tokens(oa_c), heads_to_tokens(ob_c)], axis=-1) @ w_out
    return out_ctx, out_lat


def mixer_na(h_ctx, h_lat, nbr_idx, rel_idx, w_in, w_out, rpb, need_ctx):
    def project(h):
        bsz, t, _ = h.shape
        p = (h @ w_in).reshape(bsz, t, 3, H_C, HEAD_DIM).transpose(2, 0, 3, 1, 4)
        return p[0] * (HEAD_DIM ** -0.5), p[1], p[2]

    q_l, k_l, v_l = project(h_lat)
    q_c, k_c, v_c = project(h_ctx)
    bsz, s = h_lat.shape[0], h_lat.shape[1]
    nk = nbr_idx.shape[1]
    nb = s // Q_BLOCK
    rpb_flat = rpb.reshape(H_C, -1)
    qb = q_l.reshape(bsz, H_C, nb, Q_BLOCK, HEAD_DIM).transpose(2, 0, 1, 3, 4)
    idx_b = nbr_idx.reshape(nb, Q_BLOCK, nk)
    rel_b = rel_idx.reshape(nb, Q_BLOCK, nk)

    def one_block(args):
        qblk, ib, rb = args
        kg = jnp.take(k_l, ib, axis=2)
        vg = jnp.take(v_l, ib, axis=2)
        s_n = jnp.einsum('bhqd,bhqnd->bhqn', qblk, kg, preferred_element_type=jnp.float32) + rpb_flat[:, rb].astype(jnp.float32)
        s_c = jnp.einsum('bhqd,bhcd->bhqc', qblk, k_c, preferred_element_type=jnp.float32)
        p = jax.nn.softmax(jnp.concatenate([s_n, s_c], axis=-1), axis=-1).astype(v_l.dtype)
        return jnp.einsum('bhqn,bhqnd->bhqd', p[..., :nk], vg) + jnp.einsum('bhqc,bhcd->bhqd', p[..., nk:], v_c)

    o = lax.map(one_block, (qb, idx_b, rel_b))
    out_lat = o.transpose(1, 0, 3, 2, 4).reshape(bsz, s, C_WIDTH) @ w_out
    out_ctx = None
    if need_ctx:
        oc = attend(q_c[:, :, None], k_c, v_c)[:, :, 0]
        out_ctx = heads_to_tokens(oc) @ w_out
    return out_ctx, out_lat


def hier_moe(h, w_rg, b_rg, w_re, b_re, w_gate, w_up, w_down):
    n, d = h.shape
    hf = h.astype(jnp.float32)
    pg = jax.nn.softmax(hf @ w_rg.astype(jnp.float32) + b_rg.astype(jnp.float32), axis=-1)
    g_sel = jnp.argmax(pg, axis=-1)
    p_gsel = jnp.take_along_axis(pg, g_sel[:, None], axis=-1)
    le = (hf @ w_re.astype(jnp.float32) + b_re.astype(jnp.float32)).reshape(n, N_GROUPS, EXPERTS_PER_GROUP)
    le_sel = jnp.take_along_axis(le, g_sel[:, None, None], axis=1)[:, 0]
    top_p, top_i = lax.top_k(jax.nn.softmax(le_sel, axis=-1), TOP_K)
    gate_w = p_gsel * top_p / jnp.sum(top_p, axis=-1, keepdims=True)
    experts = g_sel[:, None] * EXPERTS_PER_GROUP + top_i

    nkk = n * TOP_K
    flat_e = experts.reshape(-1).astype(jnp.int32)
    flat_w = gate_w.reshape(-1)
    order = jnp.argsort(flat_e)
    sorted_e = flat_e[order]
    tok_sorted = (jnp.arange(nkk, dtype=jnp.int32) // TOP_K)[order]
    w_sorted = flat_w[order]
    counts = jnp.bincount(flat_e, length=N_EXPERTS)
    padded = ((counts + MOE_BLOCK - 1) // MOE_BLOCK) * MOE_BLOCK
    ends = jnp.cumsum(padded)
    pad_start = ends - padded
    start = jnp.cumsum(counts) - counts
    rank = jnp.arange(nkk, dtype=jnp.int32) - start[sorted_e]
    dest = pad_start[sorted_e] + rank
    total = -(-(nkk + N_EXPERTS * (MOE_BLOCK - 1)) // MOE_BLOCK) * MOE_BLOCK
    nblk = total // MOE_BLOCK
    buf_tok = jnp.full((total,), n, jnp.int32).at[dest].set(tok_sorted)
    h_pad = jnp.concatenate([h, jnp.zeros((1, d), h.dtype)], axis=0)
    x_buf = h_pad[buf_tok].reshape(nblk, MOE_BLOCK, d)
    blk_e = jnp.clip(jnp.searchsorted(ends, jnp.arange(nblk, dtype=ends.dtype) * MOE_BLOCK, side='right'), 0, N_EXPERTS - 1)

    def run_block(args):
        xb, e = args
        return (jax.nn.silu(xb @ w_gate[e]) * (xb @ w_up[e])) @ w_down[e]

    y = lax.map(run_block, (x_buf, blk_e)).reshape(total, d)
    return jnp.zeros((n, d), h.dtype).at[tok_sorted].add(y[dest] * w_sorted[:, None].astype(h.dtype))


def setup_inputs(seed: int = 0) -> dict:
    key = jax.random.key(seed)
    ks = jax.random.split(key, 32)

    def nrm(k, shape, scale):
        return jax.random.normal(k, shape, jnp.float32) * scale

    def gain(k, shape):
        return 1.0 + nrm(k, shape, 0.1)

    D = D_MODEL
    return {
        "x": nrm(ks[0], (BATCH, SEQ, D), 1.0),
        "c": nrm(ks[1], (BATCH, D), 1.0),
        "ctx": nrm(ks[2], (BATCH, CTX_LEN, D), 1.0),
        "c_ctx": nrm(ks[3], (D,), 1.0),
        "w_ada": nrm(ks[4], (DEPTH, D, 6 * D), 0.02),
        "b_ada": nrm(ks[5], (DEPTH, 6 * D), 0.02),
        "norm_mix": gain(ks[6], (DEPTH, D)),
        "norm_ffn": gain(ks[7], (DEPTH, D)),
        "norm_final": gain(ks[8], (D,)),
        "ab_w_in": nrm(ks[9], (N_EVEN, D, AB_IN), D ** -0.5),
        "ab_w_out": nrm(ks[10], (N_EVEN, AB_OUT, D), AB_OUT ** -0.5),
        "mla_q_norm": gain(ks[11], (N_EVEN, Q_LORA)),
        "mla_w_q_up": nrm(ks[12], (N_EVEN, Q_LORA, H_A * (NOPE_A + ROPE_A)), Q_LORA ** -0.5),
        "mla_kv_norm": gain(ks[13], (N_EVEN, KV_LORA)),
        "mla_w_kv_up": nrm(ks[14], (N_EVEN, KV_LORA, H_A * (NOPE_A + V_A)), KV_LORA ** -0.5),
        "gqa_q_norm": gain(ks[15], (N_EVEN, HEAD_DIM)),
        "gqa_k_norm": gain(ks[16], (N_EVEN, HEAD_DIM)),
        "na_w_in": nrm(ks[17], (N_ODD, D, 3 * C_WIDTH), D ** -0.5),
        "na_w_out": nrm(ks[18], (N_ODD, C_WIDTH, D), C_WIDTH ** -0.5),
        "na_rpb": nrm(ks[19], (N_ODD, H_C, 2 * WIN_R - 1, 2 * WIN_C - 1), 0.1),
        "moe_w_rg": nrm(ks[20], (DEPTH, D, N_GROUPS), D ** -0.5),
        "moe_b_rg": nrm(ks[21], (DEPTH, N_GROUPS), 0.01),
        "moe_w_re": nrm(ks[22], (DEPTH, D, N_EXPERTS), D ** -0.5),
        "moe_b_re": nrm(ks[23], (DEPTH, N_EXPERTS), 0.01),
        "moe_w_gate": nrm(ks[24], (DEPTH, N_EXPERTS, D, D_EXPERT), D ** -0.5),
        "moe_w_up": nrm(ks[25], (DEPTH, N_EXPERTS, D, D_EXPERT), D ** -0.5),
        "moe_w_down": nrm(ks[26], (DEPTH, N_EXPERTS, D_EXPERT, D), D_EXPERT ** -0.5),
    }


def reference(x, c, ctx, c_ctx, w_ada, b_ada, norm_mix, norm_ffn, norm_final, ab_w_in, ab_w_out, mla_q_norm, mla_w_q_up, mla_kv_norm, mla_w_kv_up, gqa_q_norm, gqa_k_norm, na_w_in, na_w_out, na_rpb, moe_w_rg, moe_b_rg, moe_w_re, moe_b_re, moe_w_gate, moe_w_up, moe_w_down):
    bsz, s, d = x.shape
    n_ctx = ctx.shape[1]
    rows_n = s // GRID_W
    wr = min(WIN_R, rows_n)
    t = jnp.arange(s, dtype=jnp.int32)
    r = t // GRID_W
    col = t % GRID_W
    rope_a = axial_rope_table(r.astype(jnp.float32), col.astype(jnp.float32), ROPE_A)
    rope_b = axial_rope_table(r.astype(jnp.float32), col.astype(jnp.float32), HEAD_DIM)
    rs = jnp.clip(r - wr // 2, 0, rows_n - wr)
    cs = jnp.clip(col - WIN_C // 2, 0, GRID_W - WIN_C)
    key_r = rs[:, None] + jnp.arange(wr, dtype=jnp.int32)[None]
    key_c = cs[:, None] + jnp.arange(WIN_C, dtype=jnp.int32)[None]
    nbr_idx = (key_r[:, :, None] * GRID_W + key_c[:, None, :]).reshape(s, wr * WIN_C)
    rel_idx = ((key_r - r[:, None] + WIN_R - 1)[:, :, None] * (2 * WIN_C - 1)
               + (key_c - col[:, None] + WIN_C - 1)[:, None, :]).reshape(s, wr * WIN_C)

    x_lat = x
    x_ctx = ctx
    silu_c = jax.nn.silu(c)
    silu_cc = jax.nn.silu(c_ctx)
    for l in range(DEPTH):
        last = l == DEPTH - 1
        mod_l = (silu_c @ w_ada[l] + b_ada[l])[:, None, :]
        mod_c = (silu_cc @ w_ada[l] + b_ada[l])[None, None, :]
        sh1, sc1, g1, sh2, sc2, g2 = jnp.split(mod_l, 6, axis=-1)
        sh1c, sc1c, g1c, sh2c, sc2c, g2c = jnp.split(mod_c, 6, axis=-1)
        h_l = modulate(x_lat, norm_mix[l], sh1, sc1)
        h_c = modulate(x_ctx, norm_mix[l], sh1c, sc1c)
        if l % 2 == 0:
            i = l // 2
            o_c, o_l = mixer_ab(h_c, h_l, rope_a, rope_b, ab_w_in[i], ab_w_out[i], mla_q_norm[i], mla_w_q_up[i],
                                mla_kv_norm[i], mla_w_kv_up[i], gqa_q_norm[i], gqa_k_norm[i], not last)
        else:
            i = l // 2
            o_c, o_l = mixer_na(h_c, h_l, nbr_idx, rel_idx, na_w_in[i], na_w_out[i], na_rpb[i], not last)
        x_lat = x_lat + g1 * o_l
        h_l = modulate(x_lat, norm_ffn[l], sh2, sc2)
        if not last:
            x_ctx = x_ctx + g1c * o_c
            h_c = modulate(x_ctx, norm_ffn[l], sh2c, sc2c)
            tokens = jnp.concatenate([h_c.reshape(-1, d), h_l.reshape(-1, d)], axis=0)
            y = hier_moe(tokens, moe_w_rg[l], moe_b_rg[l], moe_w_re[l], moe_b_re[l], moe_w_gate[l], moe_w_up[l], moe_w_down[l])
            n_c = bsz * n_ctx
            x_ctx = x_ctx + g2c * y[:n_c].reshape(bsz, n_ctx, d)
            x_lat = x_lat + g2 * y[n_c:].reshape(bsz, s, d)
        else:
            y = hier_moe(h_l.reshape(-1, d), moe_w_rg[l], moe_b_rg[l], moe_w_re[l], moe_b_re[l], moe_w_gate[l], moe_w_up[l], moe_w_down[l])
            x_lat = x_lat + g2 * y.reshape(bsz, s, d)
    return rmsnorm(x_lat, norm_final)
```

```python
import numpy as np
import ml_dtypes
import contextlib
import concourse.bass as bass
import concourse.mybir as mybir
from concourse.bass_utils import run_bass_kernel_spmd

F32 = mybir.dt.float32
BF16 = mybir.dt.bfloat16
AF = mybir.ActivationFunctionType
ALU = mybir.AluOpType
AX = mybir.AxisListType

D = 1024
NCTX = 256
CPB = 4
NEG = -30000.0
EPS = 1e-6
N_DMA_SEMS = 24

CQ, CKV, KRA, KRB, GQA, GQB, GKA, GKB, GV, WIN_COLS = 0, 384, 640, 672, 704, 1216, 1728, 1856, 1984, 2112


class Cfg:
    def __init__(self, SEQ):
        self.SEQ = SEQ
        self.TOK = SEQ // CPB
        self.TOKX = self.TOK + NCTX
        self.ROWS = self.TOK // 64
        self.SROWS = SEQ // 64
        self.NKEY = NCTX + SEQ
        self.NKT = self.NKEY // 128
        self.NP = self.ROWS // 2
        self.NKC = NCTX + (self.ROWS + 8) * 64
        self.NKCT = self.NKC // 128

    def chunks(self, with_ctx=True):
        out = [(0, NCTX)] if with_ctx else []
        for i in range(self.TOK // 512):
            out.append((NCTX + i * 512, 512))
        return out


class Prog:
    ENGS = ("pe", "act", "dve", "pool", "sp")

    def __init__(self, nc):
        self.nc = nc
        self.q = {k: [] for k in self.ENGS}
        self.cnt = {k: 0 for k in ("pe", "act", "dve", "pool")}
        self.sem = {}
        self.dsem = []
        self.dcnt = [0] * N_DMA_SEMS
        self.drr = 0
        self.known = {k: {} for k in self.ENGS}
        self.res = {}
        self.n_inst = 0
        self.n_wait = 0

    def _need(self, eng, deps):
        kn = self.known[eng]
        for (sk, v) in deps:
            if sk == "pe" and eng == "pe":
                continue
            if kn.get(sk, 0) >= v:
                continue
            kn[sk] = v
            self.n_wait += 1
            self.q[eng].append(("wait", sk, v))

    def _collect(self, reads, writes):
        deps = set()
        for r in reads:
            st = self.res.get(r)
            if st and st[0] is not None:
                deps.add(st[0])
        for w in writes:
            st = self.res.get(w)
            if st:
                if st[0] is not None:
                    deps.add(st[0])
                deps.update(st[1])
        return deps

    def _commit(self, me, reads, writes):
        for r in reads:
            st = self.res.setdefault(r, [None, []])
            st[1] = [d for d in st[1] if d[0] != me[0]] + [me]
        for w in writes:
            self.res[w] = [me, []]

    def op(self, eng, fn, reads=(), writes=()):
        deps = self._collect(reads, writes)
        self._need(eng, deps)
        self.cnt[eng] += 1
        me = (eng, self.cnt[eng])
        self.q[eng].append(("op", fn))
        self.n_inst += 1
        self._commit(me, reads, writes)
        return me

    def dma(self, out, in_, reads=(), writes=(), q="sp"):
        deps = self._collect(reads, writes)
        self._need(q, deps)
        k = self.drr
        self.drr = (self.drr + 1) % N_DMA_SEMS
        sk = ("d", k)
        if self.dcnt[k]:
            self._need(q, [(sk, self.dcnt[k])])
        self.dcnt[k] += 16
        me = (sk, self.dcnt[k])
        self.q[q].append(("dma", out, in_, k))
        self.n_inst += 1
        self._commit(me, reads, writes)
        return me

    def coll(self, kind, groups, src, dst, reads=(), writes=()):
        deps = self._collect(reads, writes)
        self._need("pool", deps)
        self.ccnt = getattr(self, "ccnt", 0) + 1
        me = ("cc", self.ccnt)
        self.q["pool"].append(("coll", kind, groups, src, dst))
        self.n_inst += 1
        self._commit(me, reads, writes)
        self._need("pool", [me])
        return me

    def full_barrier(self):
        deps = [(k, self.cnt[k]) for k in self.cnt if self.cnt[k]]
        deps += [(("d", k), self.dcnt[k]) for k in range(N_DMA_SEMS) if self.dcnt[k]]
        if getattr(self, "ccnt", 0):
            deps.append(("cc", self.ccnt))
        for e in self.ENGS:
            self._need(e, deps)
        self.res = {}

    def _sem(self, sk):
        return self.dsem[sk[1]] if isinstance(sk, tuple) else self.sem[sk]

    def emit(self):
        nc = self.nc
        with contextlib.ExitStack() as es:
            for k in ("pe", "act", "dve", "pool", "cc"):
                self.sem[k] = es.enter_context(nc.semaphore("s_" + k))
            for i in range(N_DMA_SEMS):
                self.dsem.append(es.enter_context(nc.semaphore("s_d%d" % i)))
            block = es.enter_context(nc.Block())
            for name, deco in (("sp", block.sync), ("pe", block.tensor), ("act", block.scalar),
                               ("dve", block.vector), ("pool", block.gpsimd)):
                items = self.q[name]

                def body(eng, items=items, name=name):
                    for it in items:
                        if it[0] == "wait":
                            eng.wait_ge(self._sem(it[1]), it[2])
                        elif it[0] == "op":
                            it[1](eng).then_inc(self.sem[name], 1)
                        elif it[0] == "coll":
                            eng.collective_compute(it[1], ALU.bypass, replica_groups=it[2], ins=[it[3]], outs=[it[4]]).then_inc(self.sem["cc"])
                        else:
                            eng.dma_start(out=it[1], in_=it[2]).then_inc(self.dsem[it[3]], 16)
                deco(body)


def _isz(dt):
    return 4 if dt == F32 else 2


class Bld:
    SBUF_BYTES = 229000

    def __init__(self, cfg):
        self.cfg = cfg
        self.nc = bass.Bass("TRN2", target_bir_lowering=False)
        self.p = Prog(self.nc)
        self.off = 16384 + 512
        self.uid = 0
        self.pools = {}
        self.dram = {}
        nc = self.nc
        self.pw = [nc.alloc_psum_tensor("pw%d" % i, [128, 1024], F32) for i in range(4)]
        self.ident_f = self.alloc("ident_f", [128, 128], F32)
        self.ident_b = self.alloc("ident_b", [128, 128], BF16)
        self.ones_f = self.alloc("ones_f", [128, 128], F32)
        self.ones_b = self.alloc("ones_b", [128, 128], BF16)
        self.blk_f = self.alloc("blk_f", [128, 128], F32)
        self.epsD = self.alloc("epsD", [128, 4], F32)
        self.mv = self.alloc("mv", [128, 4, 2, 6, 8], F32)
        p = self.p
        p.op("pool", lambda e: e.memset(self.ident_f[:], 0.0), writes=["ident_f"])
        p.op("pool", lambda e: e.affine_select(out=self.ident_f[:], in_=self.ident_f[:], pattern=[[-1, 128]],
                                               compare_op=ALU.not_equal, fill=1.0, base=0, channel_multiplier=1),
             reads=["ident_f"], writes=["ident_f"])
        p.op("dve", lambda e: e.tensor_copy(out=self.ident_b[:], in_=self.ident_f[:]), reads=["ident_f"], writes=["ident_b"])
        p.op("dve", lambda e: e.memset(self.ones_f[:], 1.0), writes=["ones_f"])
        p.op("dve", lambda e: e.memset(self.ones_b[:], 1.0), writes=["ones_b"])
        p.op("dve", lambda e: e.memset(self.blk_f[:], 0.0), writes=["blk_f"])
        p.op("dve", lambda e: e.memset(self.blk_f[0:64, 0:64], 1.0), reads=["blk_f"], writes=["blk_f"])
        p.op("dve", lambda e: e.memset(self.blk_f[64:128, 64:128], 1.0), reads=["blk_f"], writes=["blk_f"])
        p.op("dve", lambda e: e.memset(self.epsD[:], EPS), writes=["epsD"])
        self.base_off = self.off

    def alloc(self, name, shape, dt):
        nb = int(np.prod(shape[1:])) * _isz(dt)
        nb = (nb + 63) // 64 * 64
        assert self.off + nb <= self.SBUF_BYTES, ("SBUF overflow", name, self.off, nb)
        self.uid += 1
        t = self.nc.alloc_sbuf_tensor_at("%s_%d" % (name, self.uid), list(shape), dt, offset=self.off)
        self.off += nb
        return t

    def phase_reset(self):
        self.p.full_barrier()
        self.off = self.base_off
        self.pools = {}

    def tmp(self, tag, shape, dt, bufs=2):
        if tag not in self.pools:
            self.pools[tag] = [[(self.alloc(tag, shape, dt)) for _ in range(bufs)], 0]
        pl = self.pools[tag]
        t = pl[0][pl[1] % bufs]
        k = "%s#%d@%d" % (tag, pl[1] % bufs, id(pl))
        pl[1] += 1
        return t, k

    def din(self, name, shape, dt):
        self.dram[name] = self.nc.dram_tensor(name, list(shape), dt, kind="ExternalInput").ap()
        return self.dram[name]

    def dout(self, name, shape, dt):
        self.dram[name] = self.nc.dram_tensor(name, list(shape), dt, kind="ExternalOutput").ap()
        return self.dram[name]

    def dint(self, name, shape, dt):
        self.dram[name] = self.nc.dram_tensor(name, list(shape), dt).ap()
        return self.dram[name]

    def bank(self, i):
        return self.pw[i // 2][:, (i % 2) * 512:(i % 2) * 512 + 512], "bank%d" % i

    def mm(self, out, lhsT, rhs, start, stop, reads, writes):
        self.p.op("pe", lambda e: e.matmul(out, lhsT, rhs, start=start, stop=stop), reads, writes)

    def act(self, out, in_, func, reads, writes, bias=None, scale=1.0, accum_out=None):
        kw = {}
        if bias is not None:
            kw["bias"] = bias
        if accum_out is not None:
            kw["accum_out"] = accum_out
        self.p.op("act", lambda e: e.activation(out=out, in_=in_, func=func, scale=scale, **kw), reads, writes)

    def stt(self, out, in0, scalar, in1, op0, op1, reads, writes, eng="dve"):
        self.p.op(eng, lambda e: e.scalar_tensor_tensor(out=out, in0=in0, scalar=scalar, in1=in1, op0=op0, op1=op1), reads, writes)

    def tt(self, out, in0, in1, op, reads, writes, eng="dve"):
        self.p.op(eng, lambda e: e.tensor_tensor(out=out, in0=in0, in1=in1, op=op), reads, writes)

    def ts(self, out, in0, s1, s2, op0, op1, reads, writes, eng="dve", accum_out=None):
        if op1 is None:
            self.p.op(eng, lambda e: e.tensor_scalar(out=out, in0=in0, scalar1=s1, scalar2=None, op0=op0), reads, writes)
        elif accum_out is None:
            self.p.op(eng, lambda e: e.tensor_scalar(out=out, in0=in0, scalar1=s1, scalar2=s2, op0=op0, op1=op1), reads, writes)
        else:
            self.p.op(eng, lambda e: e.tensor_scalar(out=out, in0=in0, scalar1=s1, scalar2=s2, op0=op0, op1=op1, accum_out=accum_out), reads, writes)

    def cp(self, out, in_, reads, writes, eng="dve"):
        if eng == "act":
            self.p.op("act", lambda e: e.copy(out=out, in_=in_), reads, writes)
        else:
            self.p.op(eng, lambda e: e.tensor_copy(out=out, in_=in_), reads, writes)

    def recip(self, out, in_, reads, writes):
        self.p.op("dve", lambda e: e.reciprocal(out=out, in_=in_), reads, writes)

    def rstd_from_sum(self, ps_ap, ps_key, n_feat, n, tag, np_=128):
        t, k = self.tmp(tag, [128, 512], F32, bufs=2)
        self.act(t[0:np_, 0:n], ps_ap, AF.Sqrt, [ps_key, "epsD"], [k], bias=self.epsD[0:np_, 0:1], scale=1.0 / n_feat)
        self.recip(t[0:np_, 0:n], t[0:np_, 0:n], [k], [k])
        return t, k


def phase_modvec(B, layers):
    p = B.p
    c_in = B.dram["c_in"]
    cs, ck = B.tmp("c_s", [128, 8, 2], F32, 1)
    cb, cbk = B.tmp("c_b", [128, 8, 2], BF16, 1)
    p.dma(cs[:], c_in[:, :, :], writes=[ck])
    B.act(cb[:], cs[:], AF.Silu, [ck], [cbk])
    nm, nmk = B.tmp("nm", [128, 4, 8], F32, 1)
    nf, nfk = B.tmp("nf", [128, 4, 8], F32, 1)
    ba, bak = B.tmp("ba", [128, 4, 48], F32, 1)
    p.dma(nm[:], B.dram["nmix_p"][:, :, :], writes=[nmk])
    p.dma(nf[:], B.dram["nffn_p"][:, :, :], writes=[nfk])
    p.dma(ba[:], B.dram["bada_p"][:, :, :], writes=[bak])
    M, Mk = B.tmp("modM", [128, 48, 2], F32, 1)
    for li, l in enumerate(layers):
        wada = B.dram["wada_p"]
        ps, psk = B.bank(0)
        for cbk_i in range(12):
            w, wk = B.tmp("wada_blk", [128, 8, 512], BF16, 2)
            p.dma(w[:], wada[li, :, :, cbk_i * 512:(cbk_i + 1) * 512], writes=[wk], q="pool")
            for f in range(4):
                ft = cbk_i * 4 + f
                for k in range(8):
                    B.mm(ps[:, ft * 2:ft * 2 + 2], w[:, k, f * 128:(f + 1) * 128], cb[:, k, :], k == 0, k == 7,
                         [wk, cbk], [psk])
        B.tt(M[:], ps[:, 0:96].rearrange("p (t j) -> p t j", j=2), ba[:, l, :].unsqueeze(2).to_broadcast([128, 48, 2]),
             ALU.add, [psk, bak], [Mk])
        for j in range(2):
            mvl = B.mv[:, l, j]
            B.stt(mvl[:, 0, :], M[:, 8:16, j], 1.0, nm[:, l, :], ALU.add, ALU.mult, [Mk, nmk], ["mv"])
            B.cp(mvl[:, 1, :], M[:, 0:8, j], [Mk], ["mv"])
            B.cp(mvl[:, 2, :], M[:, 16:24, j], [Mk], ["mv"])
            B.stt(mvl[:, 3, :], M[:, 32:40, j], 1.0, nf[:, l, :], ALU.add, ALU.mult, [Mk, nfk], ["mv"])
            B.cp(mvl[:, 4, :], M[:, 24:32, j], [Mk], ["mv"])
            B.cp(mvl[:, 5, :], M[:, 40:48, j], [Mk], ["mv"])


def norm_mod(B, xt, xk, n, l, j, kind0, want_f32=False, tagp=""):
    sq, sqk = B.tmp("nm_sq" + tagp, [128, 8, 512], F32, 1)
    B.act(sq[:, :, 0:n], xt, AF.Square, [xk], [sqk])
    ps, psk = B.bank(0)
    for k in range(8):
        B.mm(ps[:, 0:n], B.ones_f[:], sq[:, k, 0:n], k == 0, k == 7, ["ones_f", sqk], [psk])
    rs, rsk = B.rstd_from_sum(ps[:, 0:n], psk, float(D), n, "nm_rstd" + tagp)
    hT, hk = B.tmp("hT" + tagp, [128, 8, 512], BF16, 1)
    hf = hfk = None
    if want_f32:
        hf, hfk = B.tmp("hTf" + tagp, [128, 8, 512], F32, 1)
    for k in range(8):
        t, tk = B.tmp("nm_t" + tagp, [128, 512], F32, 2)
        B.stt(t[:, 0:n], xt[:, k, :], B.mv[:, l, j, kind0, k:k + 1], rs[:, 0:n], ALU.mult, ALU.mult, [xk, "mv", rsk], [tk])
        if want_f32:
            B.act(hf[:, k, 0:n], t[:, 0:n], AF.Identity, [tk, "mv"], [hfk], bias=B.mv[:, l, j, kind0 + 1, k:k + 1])
            B.cp(hT[:, k, 0:n], hf[:, k, 0:n], [hfk], [hk])
        else:
            B.act(hT[:, k, 0:n], t[:, 0:n], AF.Identity, [tk, "mv"], [hk], bias=B.mv[:, l, j, kind0 + 1, k:k + 1])
    return hT, hk, hf, hfk


def phase_A_even(B, l, xT_dram):
    cfg, p = B.cfg, B.p
    dr = B.dram
    win, wink = B.tmp("win", [128, 8, WIN_COLS], BF16, 1)
    wq, wqk = B.tmp("wq", [128, 3, 1024], BF16, 1)
    wkv, wkvk = B.tmp("wkv", [128, 2, 1024], BF16, 1)
    gn, gnk = B.tmp("gains", [128, 12], F32, 1)
    for k in range(8):
        p.dma(win[:, k, :], dr["win_p"][:, k, :], writes=[wink], q="pool")
    p.dma(wq[:], dr["wq_p"][:, :, :], writes=[wqk], q="pool")
    p.dma(wkv[:], dr["wkv_p"][:, :, :], writes=[wkvk], q="pool")
    p.dma(gn[:], dr["gains_e"][:, :], writes=[gnk])
    QTA, QTB, KTA, KTB, VA, VB = (dr[k] for k in ("QT_A_o", "QT_B_o", "KT_A_o", "KT_B_o", "V_A_o", "V_B_o"))
    for (c0, n) in cfg.chunks(True):
        j = 1 if c0 == 0 else 0
        xt, xk = B.tmp("xT_a", [128, 8, 512], F32, 1)
        p.dma(xt[:, :, 0:n], xT_dram.rearrange("(k p) t -> p k t", p=128)[:, :, c0:c0 + n], writes=[xk])
        hT, hk, _, _ = norm_mod(B, xt[:, :, 0:n], xk, n, l, j, 0)
        ta, tak = B.tmp("tabA", [128, 4, 512], F32, 1)
        tb, tbk = B.tmp("tabB", [128, 4, 512], F32, 1)
        p.dma(ta[64:96, :, 0:n], dr["tabA"][64:96, :, c0:c0 + n], writes=[tak])
        p.dma(tb[:, :, 0:n], dr["tabB"][:, :, c0:c0 + n], writes=[tbk])

        def proj(ps_ap, psk, col0, m, out_base=0):
            for k in range(8):
                B.mm(ps_ap, win[:, k, col0:col0 + m], hT[:, k, 0:n], k == 0, k == 7, [wink, hk], [psk])

        def latent(col0, ntile, gcol0, nfeat, tag):
            raw, rawk = B.tmp("lat_raw", [128, 3, 512], F32, 1)
            sq, sqk = B.tmp("lat_sq", [128, 3, 512], F32, 1)
            for m in range(ntile):
                ps, psk = B.bank(2 + (m % 2))
                proj(ps[:, 0:n], psk, col0 + m * 128, 128)
                B.cp(raw[:, m, 0:n], ps[:, 0:n], [psk], [rawk], eng="act")
                B.act(sq[:, m, 0:n], ps[:, 0:n], AF.Square, [psk], [sqk])
            ps, psk = B.bank(1)
            for m in range(ntile):
                B.mm(ps[:, 0:n], B.ones_f[:], sq[:, m, 0:n], m == 0, m == ntile - 1, ["ones_f", sqk], [psk])
            rs, rsk = B.rstd_from_sum(ps[:, 0:n], psk, float(nfeat), n, tag + "_rstd")
            o, ok_ = B.tmp(tag + "_n", [128, 3, 512], BF16, 2)
            for m in range(ntile):
                B.stt(o[:, m, 0:n], raw[:, m, 0:n], gn[:, gcol0 + m:gcol0 + m + 1], rs[:, 0:n], ALU.mult, ALU.mult,
                      [rawk, gnk, rsk], [ok_])
            return o, ok_

        cqn, cqnk = latent(CQ, 3, 0, 384, "cq")
        ckvn, ckvnk = latent(CKV, 2, 3, 256, "ckv")

        psA, psAk = B.bank(2)
        psB, psBk = B.bank(3)
        proj(psA[64:96, 0:n], psAk, KRA, 32)
        proj(psB[64:96, 0:n], psBk, KRB, 32)
        kst, kstk = B.tmp("kstage", [128, 512], BF16, 2)
        t1, t1k = B.tmp("rope_t1", [128, 512], F32, 2)
        t2, t2k = B.tmp("rope_t2", [128, 512], F32, 2)
        B.tt(t1[64:96, 0:n], psA[64:96, 0:n], ta[64:96, 2, 0:n], ALU.mult, [psAk, tak], [t1k])
        B.tt(t2[64:96, 0:n], psB[64:96, 0:n], ta[64:96, 3, 0:n], ALU.mult, [psBk, tak], [t2k])
        B.tt(kst[64:96, 0:n], t1[64:96, 0:n], t2[64:96, 0:n], ALU.add, [t1k, t2k], [kstk])
        for h in range(8):
            p.dma(KTA[h, 64:96, c0:c0 + n], kst[64:96, 0:n], reads=[kstk])
        for h in range(8):
            ps, psk = B.bank(2 + (h % 2))
            for k in range(2):
                B.mm(ps[0:64, 0:n], wkv[:, k, h * 64:(h + 1) * 64], ckvn[:, k, 0:n], k == 0, k == 1, [wkvk, ckvnk], [psk])
            kn, knk = B.tmp("knope", [64, 512], BF16, 3)
            B.cp(kn[:, 0:n], ps[0:64, 0:n], [psk], [knk], eng="act" if h % 2 else "dve")
            p.dma(KTA[h, 0:64, c0:c0 + n], kn[:, 0:n], reads=[knk])
        for jt in range(n // 128):
            ps, psk = B.bank(4 + (jt % 2))
            for k in range(2):
                B.mm(ps[:, 0:512], ckvn[:, k, jt * 128:(jt + 1) * 128], wkv[:, k, 512:1024], k == 0, k == 1, [ckvnk, wkvk], [psk])
            v, vk = B.tmp("va_tok", [128, 512], BF16, 2)
            B.cp(v[:], ps[:, 0:512], [psk], [vk], eng="act" if jt % 2 else "dve")
            if "V_hm" in dr:
                p.dma(VA.rearrange("(h t) d -> t h d", h=8)[c0 + jt * 128:c0 + (jt + 1) * 128, :, :], v[:].rearrange("p (h d) -> p h d", d=64), reads=[vk])
            else:
                p.dma(VA[c0 + jt * 128:c0 + (jt + 1) * 128, :], v[:], reads=[vk])
        for h in range(8):
            psA, psAk = B.bank(2 + 2 * (h % 2))
            psB, psBk = B.bank(3 + 2 * (h % 2))
            for k in range(3):
                B.mm(psA[0:96, 0:n], wq[:, k, h * 96:(h + 1) * 96], cqn[:, k, 0:n], k == 0, k == 2, [wqk, cqnk], [psAk])
            for k in range(3):
                B.mm(psB[64:96, 0:n], wq[:, k, 768 + h * 32:768 + (h + 1) * 32], cqn[:, k, 0:n], k == 0, k == 2, [wqk, cqnk], [psBk])
            qs, qsk = B.tmp("qstage", [128, 512], BF16, 3)
            B.act(qs[0:64, 0:n], psA[0:64, 0:n], AF.Copy, [psAk], [qsk], scale=96.0 ** -0.5)
            t1, t1k = B.tmp("rope_t1", [128, 512], F32, 2)
            t2, t2k = B.tmp("rope_t2", [128, 512], F32, 2)
            B.tt(t1[64:96, 0:n], psA[64:96, 0:n], ta[64:96, 0, 0:n], ALU.mult, [psAk, tak], [t1k])
            B.tt(t2[64:96, 0:n], psB[64:96, 0:n], ta[64:96, 1, 0:n], ALU.mult, [psBk, tak], [t2k])
            B.tt(qs[64:96, 0:n], t1[64:96, 0:n], t2[64:96, 0:n], ALU.add, [t1k, t2k, qsk], [qsk])
            p.dma(QTA[h, :, c0:c0 + n], qs[0:96, 0:n], reads=[qsk])

        def gqa_tile(colA, colB, gA, gB, ci, si, dst_fn):
            psA, psAk = B.bank(6)
            psB, psBk = B.bank(7)
            proj(psA[:, 0:n], psAk, colA, 128)
            proj(psB[:, 0:n], psBk, colB, 128)
            sq, sqk = B.tmp("g_sq", [128, 512], F32, 2)
            B.act(sq[:, 0:n], psA[:, 0:n], AF.Square, [psAk], [sqk])
            pss, pssk = B.bank(1)
            B.mm(pss[:, 0:n], B.blk_f[:], sq[:, 0:n], True, True, ["blk_f", sqk], [pssk])
            rs, rsk = B.rstd_from_sum(pss[:, 0:n], pssk, 64.0, n, "g_rstd")
            u1, u1k = B.tmp("g_u1", [128, 512], F32, 2)
            u2, u2k = B.tmp("g_u2", [128, 512], F32, 2)
            B.stt(u1[:, 0:n], psA[:, 0:n], gn[:, gA:gA + 1], tb[:, ci, 0:n], ALU.mult, ALU.mult, [psAk, gnk, tbk], [u1k])
            B.stt(u2[:, 0:n], psB[:, 0:n], gn[:, gB:gB + 1], tb[:, si, 0:n], ALU.mult, ALU.mult, [psBk, gnk, tbk], [u2k])
            B.tt(u1[:, 0:n], u1[:, 0:n], u2[:, 0:n], ALU.add, [u1k, u2k], [u1k], eng="pool")
            o, ok_ = B.tmp("g_out", [128, 512], BF16, 3)
            B.tt(o[:, 0:n], u1[:, 0:n], rs[:, 0:n], ALU.mult, [u1k, rsk], [ok_])
            dst_fn(o, ok_)

        for m in range(4):
            def dst(o, ok_, m=m):
                p.dma(QTB.rearrange("h d t -> (h d) t")[m * 128:(m + 1) * 128, c0:c0 + n], o[:, 0:n], reads=[ok_])
            gqa_tile(GQA + m * 128, GQB + m * 128, 5, 6, 0, 1, dst)

        def dstk(o, ok_):
            p.dma(KTB.rearrange("h d t -> (h d) t")[:, c0:c0 + n], o[:, 0:n], reads=[ok_])
        gqa_tile(GKA, GKB, 7, 8, 2, 3, dstk)
        for jt in range(n // 128):
            ps, psk = B.bank(4 + (jt % 2))
            for k in range(8):
                B.mm(ps[:, 0:128], hT[:, k, jt * 128:(jt + 1) * 128], win[:, k, GV:GV + 128], k == 0, k == 7, [hk, wink], [psk])
            v, vk = B.tmp("vb_tok", [128, 128], BF16, 2)
            B.cp(v[:], ps[:, 0:128], [psk], [vk], eng="act" if jt % 2 else "dve")
            if "V_hm" in dr:
                p.dma(VB.rearrange("(h t) d -> t h d", h=2)[c0 + jt * 128:c0 + (jt + 1) * 128, :, :], v[:].rearrange("p (h d) -> p h d", d=64), reads=[vk])
            else:
                p.dma(VB[c0 + jt * 128:c0 + (jt + 1) * 128, :], v[:], reads=[vk])


def attend_group(B, kt, ktk, d, vt, vtk, qt, qtk, qcol0, nq, key_tiles, out_dram_ap, bias_key=None):
    p = B.p
    per = 1024 // nq
    groups = [key_tiles[i:i + per] for i in range(0, len(key_tiles), per)]
    B._po = (getattr(B, "_po", 0) + 1) % 2
    po, pok = B.bank(4 + B._po)
    first = True
    pend = None
    for gi, grp in enumerate(groups):
        B._pw = (getattr(B, "_pw", 0) + 1) % 2
        ps = B.pw[B._pw]
        psk = "wide%d" % B._pw
        for ti, (t, bias_ap) in enumerate(grp):
            sl = ps[:, ti * nq:(ti + 1) * nq]
            B.mm(sl, kt[0:d, t * 128:(t + 1) * 128], qt[0:d, qcol0:qcol0 + nq], True, bias_ap is None, [ktk, qtk], [psk])
            if bias_ap is not None:
                B.mm(sl, B.ident_b[:], bias_ap, False, True, ["ident_b", bias_key], [psk])
        pt, ptk = B.tmp("pT", [128, 1024], BF16, 3)
        ncol = len(grp) * nq
        if pend is not None:
            pend()
        B.act(pt[:, 0:ncol], ps[:, 0:ncol], AF.Exp, [psk], [ptk])

        def pv(grp=grp, pt=pt, ptk=ptk, is_first=first, is_last=(gi == len(groups) - 1)):
            for ti, (t, _) in enumerate(grp):
                B.mm(po[0:65, 0:nq], vt[:, t, 0:65], pt[:, ti * nq:(ti + 1) * nq], is_first and ti == 0,
                     is_last and ti == len(grp) - 1, [vtk, ptk], [pok])
        pend = pv
        first = False
    pend()
    rc, rck = B.tmp("att_rc", [128, 512], F32, 2)
    B.recip(rc[64:65, 0:nq], po[64:65, 0:nq], [pok], [rck])
    pb, pbk = B.bank(6)
    B.mm(pb[0:64, 0:nq], B.ones_f[64:65, 0:64], rc[64:65, 0:nq], True, True, ["ones_f", rck], [pbk])
    bc, bck = B.tmp("att_bc", [64, 512], F32, 2)
    B.cp(bc[:, 0:nq], pb[0:64, 0:nq], [pbk], [bck], eng="act")
    o, ok_ = B.tmp("att_o", [64, 512], BF16, 3)
    B.tt(o[:, 0:nq], po[0:64, 0:nq], bc[:, 0:nq], ALU.mult, [pok, bck], [ok_])
    p.dma(out_dram_ap, o[:, 0:nq], reads=[ok_])


def load_v(B, vt, vtk, vsrc, ntiles, step=10):
    for a in range(0, ntiles, step):
        b_ = min(ntiles, a + step)
        B.p.dma(vt[:, a:b_, 0:64], vsrc[:, a:b_, :], writes=[vtk])


def phase_att_even(B, need_ctx, gathered=False):
    cfg, p, dr = B.cfg, B.p, B.dram
    TOK, TOKX = cfg.TOK, cfg.TOKX
    tpr = TOK // 128
    OT = dr["OT"]
    NKT = cfg.NKT
    vts = [B.alloc("vt%d" % i, [128, NKT, 80], BF16) for i in range(2)]
    for i in range(2):
        p.op("pool", lambda e, i=i: e.memset(vts[i][:, :, 64:80], 1.0), writes=["vt%d" % i])
    for hh in range(16):
        isA = hh < 8
        h = hh if isA else hh - 8
        d = 96 if isA else 64
        kt, ktk = B.tmp("ktA", [96, cfg.NKEY], BF16, 2)
        qt, qtk = B.tmp("qt", [96, cfg.TOKX], BF16, 2)
        vt, vtk = vts[hh % 2], "vt%d" % (hh % 2)
        if gathered:
            kg = dr["KT_A_g"] if isA else dr["KT_B_g"]
            hk, dd = (h, 96) if isA else (h // 4, 64)
            p.dma(kt[0:d, 0:NCTX], kg[(hk * CPB) * dd:(hk * CPB) * dd + d, 0:NCTX], writes=[ktk])
            for r in range(CPB):
                p.dma(kt[0:d, NCTX + r * TOK:NCTX + (r + 1) * TOK], kg[(hk * CPB + r) * dd:(hk * CPB + r) * dd + d, NCTX:TOKX], writes=[ktk])
            p.dma(qt[0:d, :], (dr["QT_A_i"] if isA else dr["QT_B_i"])[h, :, :], writes=[qtk])
            vg = dr["V_A_g"] if isA else dr["V_B_g"]
            vv = vg.rearrange("(a t p) d -> a p t d", p=128, t=TOKX // 128)
            p.dma(vt[:, 0:2, 0:64], vv[hk * CPB, :, 0:2, :], writes=[vtk])
            for r in range(CPB):
                for a in range(0, tpr, 8):
                    b_ = min(tpr, a + 8)
                    p.dma(vt[:, 2 + r * tpr + a:2 + r * tpr + b_, 0:64], vv[hk * CPB + r, :, 2 + a:2 + b_, :], writes=[vtk])
        elif isA:
            p.dma(kt[0:96, :], dr["KT_A_f"][h, :, :], writes=[ktk])
            p.dma(qt[0:96, :], dr["QT_A_i"][h, :, :], writes=[qtk])
            load_v(B, vt, vtk, dr["V_A_f"].rearrange("(t p) (h d) -> p t h d", p=128, d=64)[:, :, h, :], NKT)
        else:
            p.dma(kt[0:64, :], dr["KT_B_f"][h // 4, :, :], writes=[ktk])
            p.dma(qt[0:64, :], dr["QT_B_i"][h, :, :], writes=[qtk])
            load_v(B, vt, vtk, dr["V_B_f"].rearrange("(t p) (h d) -> p t h d", p=128, d=64)[:, :, h // 4, :], NKT)
        if need_ctx:
            attend_group(B, kt, ktk, d, vt, vtk, qt, qtk, 0, NCTX, [(0, None), (1, None)], OT[hh, :, 0:NCTX])
        for (c0, n) in cfg.chunks(False):
            attend_group(B, kt, ktk, d, vt, vtk, qt, qtk, c0, n, [(t, None) for t in range(NKT)], OT[hh, :, c0:c0 + n])


def phase_A_odd(B, l, xT_dram):
    cfg, p, dr = B.cfg, B.p, B.dram
    w, wk = B.tmp("wna", [128, 8, 3072], BF16, 1)
    for k in range(8):
        p.dma(w[:, k, :], dr["wna_p"][:, k, :], writes=[wk], q="pool")
    QT, KT, V = dr["QT_C_o"], dr["KT_C_o"], dr["V_C_o"]
    for (c0, n) in cfg.chunks(True):
        j = 1 if c0 == 0 else 0
        xt, xk = B.tmp("xT_a", [128, 8, 512], F32, 1)
        p.dma(xt[:, :, 0:n], xT_dram.rearrange("(k p) t -> p k t", p=128)[:, :, c0:c0 + n], writes=[xk])
        hT, hk, _, _ = norm_mod(B, xt[:, :, 0:n], xk, n, l, j, 0)
        for which, dst, sc in ((0, QT, 0.125), (1, KT, 1.0)):
            for m in range(8):
                ps, psk = B.bank(2 + (m % 4))
                for k in range(8):
                    B.mm(ps[:, 0:n], w[:, k, which * 1024 + m * 128:which * 1024 + (m + 1) * 128], hT[:, k, 0:n], k == 0, k == 7, [wk, hk], [psk])
                o, ok_ = B.tmp("na_qk", [128, 512], BF16, 3)
                if m % 2:
                    B.act(o[:, 0:n], ps[:, 0:n], AF.Copy, [psk], [ok_], scale=sc)
                else:
                    B.ts(o[:, 0:n], ps[:, 0:n], sc, None, ALU.mult, None, [psk], [ok_])
                p.dma(dst[m * 128:(m + 1) * 128, c0:c0 + n], o[:, 0:n], reads=[ok_])
                if which == 1 and "EK_o" in dr:
                    if c0 == NCTX:
                        p.dma(dr["EK_o"][m * 128:(m + 1) * 128, 0:256], o[:, 0:256], reads=[ok_])
                    if c0 == cfg.TOKX - 512:
                        p.dma(dr["EK_o"][m * 128:(m + 1) * 128, 256:512], o[:, 256:512], reads=[ok_])
        for jt in range(n // 128):
            for hf in range(2):
                ps, psk = B.bank(6 + hf)
                for k in range(8):
                    B.mm(ps[:, 0:512], hT[:, k, jt * 128:(jt + 1) * 128], w[:, k, 2048 + hf * 512:2048 + (hf + 1) * 512], k == 0, k == 7, [hk, wk], [psk])
                v, vk = B.tmp("na_v", [128, 512], BF16, 3)
                B.cp(v[:], ps[:, 0:512], [psk], [vk], eng="act" if hf else "dve")
                p.dma(V[c0 + jt * 128:c0 + (jt + 1) * 128, hf * 512:(hf + 1) * 512], v[:], reads=[vk])
                if "EV_o" in dr:
                    if c0 == NCTX and jt < 2:
                        p.dma(dr["EV_o"][jt * 128:(jt + 1) * 128, hf * 512:(hf + 1) * 512], v[:], reads=[vk])
                    if c0 == cfg.TOKX - 512 and jt >= 2:
                        p.dma(dr["EV_o"][jt * 128:(jt + 1) * 128, hf * 512:(hf + 1) * 512], v[:], reads=[vk])


def na_pair_tiles(cfg, i):
    P = cfg.NP
    if i == 0:
        return list(range(0, 6)), 5
    if i == 1:
        return list(range(1, 6)), 11
    if i == P - 2:
        return list(range(P - 2, P + 3)), 16
    if i == P - 1:
        return list(range(P - 2, P + 4)), 21
    return list(range(i, i + 5)), 0


def phase_att_odd(B, need_ctx):
    cfg, p, dr = B.cfg, B.p, B.dram
    OT = dr["OT"]
    NT = cfg.NKCT
    vts = [B.alloc("nvt%d" % i, [128, NT, 80], BF16) for i in range(2)]
    for i in range(2):
        p.op("pool", lambda e, i=i: e.memset(vts[i][:, :, 64:80], 1.0), writes=["nvt%d" % i])
    for h in range(16):
        kt, ktk = B.tmp("nkt", [64, cfg.NKC], BF16, 2)
        qt, qtk = B.tmp("nqt", [64, cfg.TOKX], BF16, 2)
        bt, btk = B.tmp("nbt", [128, 27, 128], BF16, 2)
        vt, vtk = vts[h % 2], "nvt%d" % (h % 2)
        p.dma(kt[:], dr["KT_C_b"][h * 64:(h + 1) * 64, :], writes=[ktk])
        p.dma(qt[:], dr["QT_C_i"][h * 64:(h + 1) * 64, :], writes=[qtk])
        p.dma(bt[:], dr["btab"][h, :, :, :], writes=[btk], q="pool")
        load_v(B, vt, vtk, dr["V_C_b"].rearrange("(t p) (h d) -> p t h d", p=128, d=64)[:, :, h, :], NT)
        if need_ctx:
            attend_group(B, kt, ktk, 64, vt, vtk, qt, qtk, 0, NCTX, [(0, None), (1, None)], OT[h, :, 0:NCTX])
        for i in range(cfg.NP):
            us, slot = na_pair_tiles(cfg, i)
            tiles = [(0, None), (1, None)] + [(2 + u, bt[:, slot + ui, :]) for ui, u in enumerate(us)]
            attend_group(B, kt, ktk, 64, vt, vtk, qt, qtk, NCTX + i * 128, 128, tiles,
                         OT[h, :, NCTX + i * 128:NCTX + (i + 1) * 128], bias_key=btk)


def phase_outproj(B, l, xT_in, xT_out, last):
    cfg, p, dr = B.cfg, B.p, B.dram
    OT = dr["OT"]
    wo, wok = B.tmp("wout", [64, 16, 1024], BF16, 1)
    for h in range(0, 16, 4):
        p.dma(wo[:, h:h + 4, :], dr["wout_p"][:, h:h + 4, :], writes=[wok], q="pool")
    for (c0, n) in cfg.chunks(not last):
        j = 1 if c0 == 0 else 0
        xt, xk = B.tmp("xT_o", [128, 8, 512], F32, 2)
        p.dma(xt[:, :, 0:n], xT_in.rearrange("(k p) t -> p k t", p=128)[:, :, c0:c0 + n], writes=[xk])
        ot, otk = B.tmp("ot_sb", [64, 16, 512], BF16, 2)
        p.dma(ot[:, :, 0:n], OT.rearrange("h d t -> d h t")[:, :, c0:c0 + n], writes=[otk])
        for f in range(8):
            ps, psk = B.bank(f % 4)
            for hh in range(16):
                B.mm(ps[:, 0:n], wo[:, hh, f * 128:(f + 1) * 128], ot[:, hh, 0:n], hh == 0, hh == 15, [wok, otk], [psk])
            B.stt(xt[:, f, 0:n], ps[:, 0:n], B.mv[:, l, j, 2, f:f + 1], xt[:, f, 0:n], ALU.mult, ALU.add, [psk, "mv", xk], [xk])
        p.dma(xT_out.rearrange("(k p) t -> p k t", p=128)[:, :, c0:c0 + n], xt[:, :, 0:n], reads=[xk])
    if last:
        xt, xk = B.tmp("xT_o", [128, 8, 512], F32, 2)
        p.dma(xt[:, :, 0:NCTX], xT_in.rearrange("(k p) t -> p k t", p=128)[:, :, 0:NCTX], writes=[xk])
        p.dma(xT_out.rearrange("(k p) t -> p k t", p=128)[:, :, 0:NCTX], xt[:, :, 0:NCTX], reads=[xk])


def phase_ffn(B, l, xT, last):
    cfg, p, dr = B.cfg, B.p, B.dram
    wr, wrk = B.tmp("wr", [128, 8, 36], F32, 1)
    p.dma(wr[:], dr["wr_p"][:, :, :], writes=[wrk])
    brt, brk = B.tmp("br", [128, 36], F32, 1)
    p.dma(brt[:], dr["br_p"][0:1, :].partition_broadcast(128), writes=[brk])
    sel, selk = B.tmp("sel", [32, 32, 128], BF16, 1)
    for e_ in range(32):
        B.cp(sel[:, e_, :], B.ident_b[0:32, e_:e_ + 1].to_broadcast([32, 128]), ["ident_b"], [selk], eng="pool" if e_ % 2 else "dve")
    lat_chunks = cfg.chunks(False)
    passes = [lat_chunks[i:i + 2] for i in range(0, len(lat_chunks), 2)]
    if not last:
        passes[0] = [(0, NCTX)] + passes[0]
    wg_d, wu_d, wd_d = dr["wg_p"], dr["wu_p"], dr["wd_p"]
    MAXN = 1280
    xv = xT.rearrange("(k p) t -> p k t", p=128)
    for subs in passes:
        xs, xsk0 = B.tmp("xs", [128, 8, MAXN], F32, 1)
        h2, h2k0 = B.tmp("h2", [128, 8, MAXN], BF16, 1)
        gT, gTk0 = B.tmp("gT", [32, MAXN], BF16, 1)
        offs, o_ = [], 0
        for (c0, n) in subs:
            offs.append(o_)
            o_ += n
        for si, (c0, n) in enumerate(subs):
            j = 1 if c0 == 0 else 0
            so = offs[si]
            xsk, h2k, gTk = "%s_%d" % (xsk0, si), "%s_%d" % (h2k0, si), "%s_%d" % (gTk0, si)
            p.dma(xs[:, :, so:so + n], xv[:, :, c0:c0 + n], writes=[xsk])
            sq, sqk = B.tmp("f_sq", [128, 8, 512], F32, 1)
            B.act(sq[:, :, 0:n], xs[:, :, so:so + n], AF.Square, [xsk], [sqk])
            ps, psk = B.bank(0)
            for k in range(8):
                B.mm(ps[:, 0:n], B.ones_f[:], sq[:, k, 0:n], k == 0, k == 7, ["ones_f", sqk], [psk])
            rs, rsk = B.rstd_from_sum(ps[:, 0:n], psk, float(D), n, "f_rstd")
            hf, hfk = sq, sqk
            for k in range(8):
                t, tk = B.tmp("f_t", [128, 512], F32, 2)
                B.stt(t[:, 0:n], xs[:, k, so:so + n], B.mv[:, l, j, 3, k:k + 1], rs[:, 0:n], ALU.mult, ALU.mult, [xsk, "mv", rsk], [tk])
                B.act(hf[:, k, 0:n], t[:, 0:n], AF.Identity, [tk, "mv", psk], [hfk], bias=B.mv[:, l, j, 4, k:k + 1])
                B.cp(h2[:, k, so:so + n], hf[:, k, 0:n], [hfk], [h2k], eng="pool" if k % 2 else "dve")
            for jt in range(n // 128):
                psl, pslk = B.bank(4 + (jt % 2))
                for k in range(8):
                    B.mm(psl[:, 0:36], hf[:, k, jt * 128:(jt + 1) * 128], wr[:, k, :], k == 0, k == 7, [hfk, wrk], [pslk])
                lg, lgk = B.tmp("r_lg", [128, 36], F32, 2)
                B.tt(lg[:], psl[:, 0:36], brt[:], ALU.add, [pslk, brk], [lgk])
                sc_, sck = B.tmp("r_sc", [128, 8], F32, 2)
                R = [lgk, sck]
                p.op("dve", lambda e, lg=lg, sc_=sc_: e.reduce_max(out=sc_[:, 0:1], in_=lg[:, 0:4], axis=AX.X), R, [sck])
                B.ts(sc_[:, 1:2], sc_[:, 0:1], -1.0, None, ALU.mult, None, [sck], [sck])
                eg, egk = B.tmp("r_eg", [128, 4], F32, 2)
                B.act(eg[:], lg[:, 0:4], AF.Exp, [lgk, sck], [egk, sck], bias=sc_[:, 1:2], accum_out=sc_[:, 2:3])
                goh, gohk = B.tmp("r_goh", [128, 4], F32, 2)
                B.ts(goh[:], lg[:, 0:4], sc_[:, 0:1], None, ALU.is_equal, None, [lgk, sck], [gohk])
                B.ts(goh[:], goh[:], -1.0, 1.0e4, ALU.add, ALU.mult, [gohk], [gohk])
                lem, lemk = B.tmp("r_lem", [128, 4, 8], F32, 2)
                B.tt(lem[:], lg[:, 4:36].rearrange("p (g e) -> p g e", e=8), goh[:].unsqueeze(2).to_broadcast([128, 4, 8]), ALU.add,
                     [lgk, gohk], [lemk])
                lem2 = lem[:].rearrange("p g e -> p (g e)")
                p.op("dve", lambda e, lem2=lem2, sc_=sc_: e.reduce_max(out=sc_[:, 3:4], in_=lem2, axis=AX.X), [lemk, sck], [sck])
                B.ts(sc_[:, 4:5], sc_[:, 3:4], -1.0, None, ALU.mult, None, [sck], [sck])
                ee, eek = B.tmp("r_ee", [128, 32], F32, 2)
                B.act(ee[:], lem2, AF.Exp, [lemk, sck], [eek], bias=sc_[:, 4:5])
                oh1, oh1k = B.tmp("r_oh1", [128, 32], F32, 2)
                B.ts(oh1[:], lem2, sc_[:, 3:4], None, ALU.is_equal, None, [lemk, sck], [oh1k])
                e2, e2k = B.tmp("r_e2", [128, 32], F32, 2)
                B.stt(e2[:], oh1[:], -2.0, ee[:], ALU.mult, ALU.add, [oh1k, eek], [e2k])
                p.op("dve", lambda e, e2=e2, sc_=sc_: e.reduce_max(out=sc_[:, 5:6], in_=e2[:], axis=AX.X), [e2k, sck], [sck])
                oh2, oh2k = B.tmp("r_oh2", [128, 32], F32, 2)
                B.ts(oh2[:], e2[:], sc_[:, 5:6], None, ALU.is_equal, None, [e2k, sck], [oh2k])
                B.tt(oh1[:], oh1[:], oh2[:], ALU.add, [oh1k, oh2k], [oh1k])
                B.ts(sc_[:, 6:7], sc_[:, 5:6], 1.0, None, ALU.add, None, [sck], [sck])
                B.tt(sc_[:, 6:7], sc_[:, 6:7], sc_[:, 2:3], ALU.mult, [sck], [sck])
                B.recip(sc_[:, 7:8], sc_[:, 6:7], [sck], [sck])
                G, Gk = B.tmp("r_G", [128, 32], F32, 2)
                B.stt(G[:], ee[:], sc_[:, 7:8], oh1[:], ALU.mult, ALU.mult, [eek, sck, oh1k], [Gk])
                if "G_dbg" in dr:
                    p.dma(dr["G_dbg"][c0 + jt * 128:c0 + (jt + 1) * 128, :], G[:], reads=[Gk])
                pst, pstk = B.bank(6)
                p.op("pe", lambda e, pst=pst, G=G: e.transpose(pst[0:32, 0:128], G[:], B.ident_f[:]), [Gk, "ident_f"], [pstk])
                B.cp(gT[:, so + jt * 128:so + (jt + 1) * 128], pst[0:32, 0:128], [pstk], [gTk], eng="act")
        for e_ in range(32):
            wg, wgk = B.tmp("wg", [128, 8, 512], BF16, 2)
            wu, wuk = B.tmp("wu", [128, 8, 512], BF16, 2)
            wd, wdk = B.tmp("wd", [128, 4, 1024], BF16, 2)
            p.dma(wg[:], wg_d[e_, :, :, :], writes=[wgk], q="pool")
            p.dma(wu[:], wu_d[e_, :, :, :], writes=[wuk], q="pool")
            p.dma(wd[:], wd_d[e_, :, :, :], writes=[wdk], q="pool")
            for si, (c0, n) in enumerate(subs):
                j = 1 if c0 == 0 else 0
                so = offs[si]
                xsk, h2k, gTk = "%s_%d" % (xsk0, si), "%s_%d" % (h2k0, si), "%s_%d" % (gTk0, si)
                pg_, pgk = B.bank(7)
                B.mm(pg_[:, 0:n], sel[:, e_, :], gT[:, so:so + n], True, True, [selk, gTk], [pgk])
                gb, gbk = B.tmp("gb", [128, 512], F32, 2)
                B.cp(gb[:, 0:n], pg_[:, 0:n], [pgk], [gbk], eng="act")
                aT, aTk = B.tmp("aT", [128, 4, 512], BF16, 2)
                for m in range(4):
                    pa, pak = B.bank(0 + (m % 2) * 2)
                    pu, puk = B.bank(1 + (m % 2) * 2)
                    for k in range(8):
                        B.mm(pa[:, 0:n], wg[:, k, m * 128:(m + 1) * 128], h2[:, k, so:so + n], k == 0, k == 7, [wgk, h2k], [pak])
                    for k in range(8):
                        B.mm(pu[:, 0:n], wu[:, k, m * 128:(m + 1) * 128], h2[:, k, so:so + n], k == 0, k == 7, [wuk, h2k], [puk])
                    sg, sgk = B.tmp("sg", [128, 512], F32, 2)
                    B.act(sg[:, 0:n], pa[:, 0:n], AF.Silu, [pak], [sgk])
                    B.tt(sg[:, 0:n], sg[:, 0:n], pu[:, 0:n], ALU.mult, [sgk, puk], [sgk])
                    B.tt(aT[:, m, 0:n], sg[:, 0:n], gb[:, 0:n], ALU.mult, [sgk, gbk], [aTk], eng="pool")
                for f in range(8):
                    pd, pdk = B.bank(4 + (f % 3))
                    for k in range(4):
                        B.mm(pd[:, 0:n], wd[:, k, f * 128:(f + 1) * 128], aT[:, k, 0:n], k == 0, k == 3, [wdk, aTk], [pdk])
                    B.stt(xs[:, f, so:so + n], pd[:, 0:n], B.mv[:, l, j, 5, f:f + 1], xs[:, f, so:so + n], ALU.mult, ALU.add,
                          [pdk, "mv", xsk], [xsk])
        for si, (c0, n) in enumerate(subs):
            so = offs[si]
            p.dma(xv[:, :, c0:c0 + n], xs[:, :, so:so + n], reads=["%s_%d" % (xsk0, si)])


def phase_final(B, xT_dram, out_dram):
    cfg, p, dr = B.cfg, B.p, B.dram
    nfin, nfk = B.tmp("nfin", [128, 8], F32, 1)
    p.dma(nfin[:], dr["nfin_p"][:, :], writes=[nfk])
    for (c0, n) in cfg.chunks(False):
        xt, xk = B.tmp("xT_a", [128, 8, 512], F32, 2)
        p.dma(xt[:, :, 0:n], xT_dram.rearrange("(k p) t -> p k t", p=128)[:, :, c0:c0 + n], writes=[xk])
        sq, sqk = B.tmp("fin_sq", [128, 8, 512], F32, 1)
        B.act(sq[:, :, 0:n], xt[:, :, 0:n], AF.Square, [xk], [sqk])
        ps, psk = B.bank(0)
        for k in range(8):
            B.mm(ps[:, 0:n], B.ones_f[:], sq[:, k, 0:n], k == 0, k == 7, ["ones_f", sqk], [psk])
        rs, rsk = B.rstd_from_sum(ps[:, 0:n], psk, float(D), n, "fin_rstd")
        o, ok_ = B.tmp("fin_o", [128, 8, 512], F32, 2)
        for k in range(8):
            B.stt(o[:, k, 0:n], xt[:, k, 0:n], nfin[:, k:k + 1], rs[:, 0:n], ALU.mult, ALU.mult, [xk, nfk, rsk], [ok_])
        p.dma(out_dram.rearrange("(k p) t -> p k t", p=128)[:, :, c0 - NCTX:c0 - NCTX + n], o[:, :, 0:n], reads=[ok_])


def declare_common(B, nl):
    B.din("c_in", [128, 8, 2], F32)
    B.din("nmix_p", [128, 4, 8], F32)
    B.din("nffn_p", [128, 4, 8], F32)
    B.din("bada_p", [128, 4, 48], F32)
    B.din("wada_p", [nl, 128, 8, 6144], F32)


def declare_A_even(B):
    c = B.cfg
    B.din("win_p", [128, 8, WIN_COLS], F32)
    B.din("wq_p", [128, 3, 1024], F32)
    B.din("wkv_p", [128, 2, 1024], F32)
    B.din("gains_e", [128, 12], F32)
    B.din("tabA", [128, 4, c.TOKX], F32)
    B.din("tabB", [128, 4, c.TOKX], F32)
    B.dout("QT_A_o", [8, 96, c.TOKX], BF16)
    B.dout("QT_B_o", [8, 64, c.TOKX], BF16)
    B.dout("KT_A_o", [8, 96, c.TOKX], BF16)
    B.dout("KT_B_o", [2, 64, c.TOKX], BF16)
    B.dout("V_A_o", [c.TOKX, 512], BF16)
    B.dout("V_B_o", [c.TOKX, 128], BF16)


def declare_A_odd(B):
    c = B.cfg
    B.din("wna_p", [128, 8, 3072], F32)
    B.dout("QT_C_o", [1024, c.TOKX], BF16)
    B.dout("KT_C_o", [1024, c.TOKX], BF16)
    B.dout("V_C_o", [c.TOKX, 1024], BF16)


def declare_att_even(B):
    c = B.cfg
    B.din("QT_A_i", [8, 96, c.TOKX], BF16)
    B.din("QT_B_i", [8, 64, c.TOKX], BF16)
    B.din("KT_A_f", [8, 96, c.NKEY], BF16)
    B.din("KT_B_f", [2, 64, c.NKEY], BF16)
    B.din("V_A_f", [c.NKEY, 512], BF16)
    B.din("V_B_f", [c.NKEY, 128], BF16)


def declare_att_odd(B):
    c = B.cfg
    B.din("QT_C_i", [1024, c.TOKX], BF16)
    B.din("KT_C_b", [1024, c.NKC], BF16)
    B.din("V_C_b", [c.NKC, 1024], BF16)
    B.din("btab", [16, 128, 27, 128], F32)


def declare_mix_ffn(B, dbg=False):
    c = B.cfg
    B.din("wout_p", [64, 16, 1024], F32)
    B.din("wr_p", [128, 8, 36], F32)
    B.din("br_p", [1, 36], F32)
    B.din("wg_p", [32, 128, 8, 512], F32)
    B.din("wu_p", [32, 128, 8, 512], F32)
    B.din("wd_p", [32, 128, 4, 1024], F32)
    B.dint("OT", [16, 64, c.TOKX], BF16)
    if dbg:
        B.dout("G_dbg", [c.TOKX, 32], F32)


def build_stage(cfg, stage, dbg=False, skip_moe=False):
    B = Bld(cfg)
    xT = B.din("xT", [1024, cfg.TOKX], F32)
    if stage == 0:
        declare_common(B, 1)
        declare_A_even(B)
        phase_modvec(B, [0]); B.phase_reset()
        phase_A_even(B, 0, xT); B.phase_reset()
    else:
        l = stage - 1
        last = stage == 4
        layers = [l] if last else [l, l + 1]
        declare_common(B, len(layers))
        if l % 2 == 0:
            declare_att_even(B)
        else:
            declare_att_odd(B)
        declare_mix_ffn(B, dbg)
        xo = B.dout("xT_o", [1024, cfg.TOKX], F32)
        if last:
            B.din("nfin_p", [128, 8], F32)
            outT = B.dout("outT", [1024, cfg.TOK], F32)
        elif (l + 1) % 2 == 0:
            declare_A_even(B)
        else:
            declare_A_odd(B)
        phase_modvec(B, layers); B.phase_reset()
        if l % 2 == 0:
            phase_att_even(B, not last)
        else:
            phase_att_odd(B, not last)
        B.phase_reset()
        phase_outproj(B, l, xT, xo, last); B.phase_reset()
        if not skip_moe:
            phase_ffn(B, l, xo, last); B.phase_reset()
        if last:
            phase_final(B, xo, outT)
        elif (l + 1) % 2 == 0:
            phase_A_even(B, l + 1, xo)
        else:
            phase_A_odd(B, l + 1, xo)
        B.phase_reset()
    B.p.emit()
    return B


def _ktile(w):
    K, N = w.shape
    return np.ascontiguousarray(w.reshape(K // 128, 128, N).transpose(1, 0, 2))


def prep_even(inp, i):
    w_in = inp["ab_w_in"][i]
    ev32, od32 = np.arange(0, 32, 2), np.arange(1, 32, 2)
    ev64, od64 = np.arange(0, 64, 2), np.arange(1, 64, 2)
    cols = list(range(0, 640))
    cols += list(640 + ev32) + list(640 + od32)
    cols += list(640 + od32) + list(640 + ev32)
    for h in range(8):
        cols += list(672 + h * 64 + ev64) + list(672 + h * 64 + od64)
    for h in range(8):
        cols += list(672 + h * 64 + od64) + list(672 + h * 64 + ev64)
    for h in range(2):
        cols += list(1184 + h * 64 + ev64) + list(1184 + h * 64 + od64)
    for h in range(2):
        cols += list(1184 + h * 64 + od64) + list(1184 + h * 64 + ev64)
    cols += list(range(1312, 1440))
    assert len(cols) == WIN_COLS
    wq = inp["mla_w_q_up"][i]
    qc = []
    for h in range(8):
        qc += list(h * 96 + np.arange(64)) + list(h * 96 + 64 + ev32) + list(h * 96 + 64 + od32)
    for h in range(8):
        qc += list(h * 96 + 64 + od32) + list(h * 96 + 64 + ev32)
    wkv = inp["mla_w_kv_up"][i]
    kc = []
    for h in range(8):
        kc += list(h * 128 + np.arange(64))
    for h in range(8):
        kc += list(h * 128 + 64 + np.arange(64))
    g = np.zeros((128, 12), np.float32)
    g[:, 0:3] = inp["mla_q_norm"][i].reshape(3, 128).T
    g[:, 3:5] = inp["mla_kv_norm"][i].reshape(2, 128).T
    gq, gk = inp["gqa_q_norm"][i], inp["gqa_k_norm"][i]
    g[:, 5] = np.tile(np.concatenate([gq[ev64], gq[od64]]), 2)
    g[:, 6] = np.tile(np.concatenate([gq[od64], gq[ev64]]), 2)
    g[:, 7] = np.tile(np.concatenate([gk[ev64], gk[od64]]), 2)
    g[:, 8] = np.tile(np.concatenate([gk[od64], gk[ev64]]), 2)
    return {"win_p": _ktile(w_in[:, cols]), "wq_p": _ktile(wq[:, qc]), "wkv_p": _ktile(wkv[:, kc]), "gains_e": g}


def rope_tables(cfg, c4):
    t = c4 * cfg.TOK + np.arange(cfg.TOK)
    r = (t // 64).astype(np.float32)
    col = (t % 64).astype(np.float32)

    def ang(rot):
        n = rot // 4
        inv = (10000.0 ** (-np.arange(n, dtype=np.float32) / n)).astype(np.float32)
        return np.concatenate([r[:, None] * inv, col[:, None] * inv], -1).astype(np.float32).T
    aA, aB = ang(32), ang(64)
    tabA = np.zeros((128, 4, cfg.TOKX), np.float32)
    tabB = np.zeros((128, 4, cfg.TOKX), np.float32)
    for tab, a, base, reps, qs in ((tabA, aA, 64, 1, 96.0 ** -0.5), (tabB, aB, 0, 2, 64.0 ** -0.5)):
        hp = a.shape[0]
        c, s = np.cos(a), np.sin(a)
        for rep in range(reps):
            o = base + rep * 2 * hp
            for blk, sgn in ((0, -1.0), (1, 1.0)):
                rows = slice(o + blk * hp, o + (blk + 1) * hp)
                tab[rows, 0, NCTX:] = c * qs
                tab[rows, 1, NCTX:] = sgn * s * qs
                tab[rows, 2, NCTX:] = c
                tab[rows, 3, NCTX:] = sgn * s
                tab[rows, 0, :NCTX] = qs
                tab[rows, 2, :NCTX] = 1.0
    return tabA, tabB


def bias_tables(cfg, rpb, c4):
    P = cfg.NP
    R0 = c4 * cfg.ROWS
    slots = [(2, u) for u in range(2, 7)] + [(0, u) for u in range(0, 6)] + [(1, u) for u in range(1, 6)] \
        + [(P - 2, u) for u in range(P - 2, P + 3)] + [(P - 1, u) for u in range(P - 2, P + 4)]
    kp = np.arange(128)
    q = np.arange(128)
    out = np.full((16, 128, 27, 128), NEG, np.float32)
    for si, (i, u) in enumerate(slots):
        kr = (R0 - 4 + 2 * u + kp // 64)[:, None]
        kc = (kp % 64)[:, None]
        r = (R0 + 2 * i + q // 64)[None, :]
        col = (q % 64)[None, :]
        rs = np.clip(r - 4, 0, cfg.SROWS - 8)
        cs = np.clip(col - 8, 0, 48)
        valid = (kr >= rs) & (kr < rs + 8) & (kc >= cs) & (kc < cs + 16)
        dr_ = np.clip(kr - r + 7, 0, 14)
        dc_ = np.clip(kc - col + 15, 0, 30)
        vals = rpb[:, dr_, dc_]
        out[:, :, si, :] = np.where(valid[None], vals, NEG)
    return out


def prep_moe(inp, l):
    return {
        "wr_p": _ktile(np.concatenate([inp["moe_w_rg"][l], inp["moe_w_re"][l]], 1)),
        "br_p": np.concatenate([inp["moe_b_rg"][l], inp["moe_b_re"][l]])[None, :].astype(np.float32),
        "wg_p": np.ascontiguousarray(inp["moe_w_gate"][l].reshape(32, 8, 128, 512).transpose(0, 2, 1, 3)),
        "wu_p": np.ascontiguousarray(inp["moe_w_up"][l].reshape(32, 8, 128, 512).transpose(0, 2, 1, 3)),
        "wd_p": np.ascontiguousarray(inp["moe_w_down"][l].reshape(32, 4, 128, 1024).transpose(0, 2, 1, 3)),
    }


def prep_cin(inp, b):
    return np.ascontiguousarray(np.stack([inp["c"][b].reshape(8, 128).T, inp["c_ctx"].reshape(8, 128).T], -1).astype(np.float32))


def prep_common(inp, layers):
    return {
        "nmix_p": np.ascontiguousarray(inp["norm_mix"].reshape(4, 8, 128).transpose(2, 0, 1)),
        "nffn_p": np.ascontiguousarray(inp["norm_ffn"].reshape(4, 8, 128).transpose(2, 0, 1)),
        "bada_p": np.ascontiguousarray(inp["b_ada"].reshape(4, 48, 128).transpose(2, 0, 1)),
        "wada_p": np.ascontiguousarray(np.stack([inp["w_ada"][l].reshape(8, 128, 6144).transpose(1, 0, 2) for l in layers])),
    }


def wout_p(w):
    return np.ascontiguousarray(w.reshape(16, 64, 1024).transpose(1, 0, 2))


N_CORES = 8
_PROG_CACHE = {}


def get_stage(cfg, stage, **kw):
    key = (cfg.SEQ, stage, tuple(sorted(kw.items())))
    if key not in _PROG_CACHE:
        _PROG_CACHE[key] = build_stage(cfg, stage, **kw)
    return _PROG_CACHE[key]


def run_stage(cfg, stage, in_maps, **kw):
    B = get_stage(cfg, stage, **kw)
    names = [n for n in B.dram]
    res = run_bass_kernel_spmd(B.nc, in_maps, core_ids=list(range(N_CORES)))
    return res.results


def exchange_even(cfg, outs):
    ins = []
    for b in range(2):
        cs = outs[b * CPB:(b + 1) * CPB]
        kta = np.concatenate([cs[0]["KT_A_o"][:, :, :NCTX]] + [c["KT_A_o"][:, :, NCTX:] for c in cs], 2)
        ktb = np.concatenate([cs[0]["KT_B_o"][:, :, :NCTX]] + [c["KT_B_o"][:, :, NCTX:] for c in cs], 2)
        va = np.concatenate([cs[0]["V_A_o"][:NCTX]] + [c["V_A_o"][NCTX:] for c in cs], 0)
        vb = np.concatenate([cs[0]["V_B_o"][:NCTX]] + [c["V_B_o"][NCTX:] for c in cs], 0)
        for c in cs:
            ins.append({"QT_A_i": c["QT_A_o"], "QT_B_i": c["QT_B_o"], "KT_A_f": kta, "KT_B_f": ktb, "V_A_f": va, "V_B_f": vb})
    return ins


def exchange_odd(cfg, outs):
    ins = []
    pad = 4 * 64
    for b in range(2):
        cs = outs[b * CPB:(b + 1) * CPB]
        kt = np.concatenate([c["KT_C_o"][:, NCTX:] for c in cs], 1)
        v = np.concatenate([c["V_C_o"][NCTX:] for c in cs], 0)
        ktp = np.concatenate([np.zeros((1024, pad), kt.dtype), kt, np.zeros((1024, pad), kt.dtype)], 1)
        vp = np.concatenate([np.zeros((pad, 1024), v.dtype), v, np.zeros((pad, 1024), v.dtype)], 0)
        for ci, c in enumerate(cs):
            a = ci * cfg.TOK
            n = (cfg.ROWS + 8) * 64
            ins.append({"QT_C_i": c["QT_C_o"],
                        "KT_C_b": np.ascontiguousarray(np.concatenate([c["KT_C_o"][:, :NCTX], ktp[:, a:a + n]], 1)),
                        "V_C_b": np.ascontiguousarray(np.concatenate([c["V_C_o"][:NCTX], vp[a:a + n]], 0))})
    return ins


def forward(inp, SEQ, upto=4, dbg=False, skip_moe=False, trace=None):
    cfg = Cfg(SEQ)
    inp = {k: np.asarray(v, np.float32) for k, v in inp.items()}
    xT = []
    for core in range(N_CORES):
        b, c4 = divmod(core, CPB)
        xl = inp["x"][b, c4 * cfg.TOK:(c4 + 1) * cfg.TOK]
        xT.append(np.ascontiguousarray(np.concatenate([inp["ctx"][b], xl], 0).T))
    tabs = [rope_tables(cfg, c % CPB) for c in range(N_CORES)]
    even = {i: prep_even(inp, i) for i in range(2)}
    extra = [None] * N_CORES
    results = None
    for stage in range(upto + 1):
        l = stage - 1
        last = stage == 4
        layers = [0] if stage == 0 else ([l] if last else [l, l + 1])
        maps = []
        shared = {}
        if stage >= 1:
            shared.update(prep_moe(inp, l))
            shared["wout_p"] = wout_p(inp["ab_w_out"][l // 2] if l % 2 == 0 else inp["na_w_out"][l // 2])
        nxt = stage if stage < 4 else None
        if nxt is not None:
            if nxt % 2 == 0:
                shared.update(even[nxt // 2])
            else:
                shared["wna_p"] = _ktile(inp["na_w_in"][nxt // 2])
        if last:
            shared["nfin_p"] = np.ascontiguousarray(inp["norm_final"].reshape(8, 128).T)
        shared.update(prep_common(inp, layers))
        cins = [prep_cin(inp, b) for b in range(2)]
        for core in range(N_CORES):
            b, c4 = divmod(core, CPB)
            m = dict(shared)
            m["c_in"] = cins[b]
            m["xT"] = xT[core]
            if nxt is not None and nxt % 2 == 0:
                m["tabA"], m["tabB"] = tabs[core]
            if stage >= 1:
                m.update(extra[core])
                if l % 2 == 1:
                    m["btab"] = bias_tables(cfg, inp["na_rpb"][l // 2], c4)
            maps.append(m)
        results = run_stage(cfg, stage, maps, **({"dbg": dbg, "skip_moe": skip_moe} if stage >= 1 else {}))
        if trace is not None:
            trace.append(results)
        if stage >= 1:
            xT = [r["xT_o"] for r in results]
        if nxt is not None:
            extra = exchange_even(cfg, results) if nxt % 2 == 0 else exchange_odd(cfg, results)
    if upto < 4:
        return results
    out = np.empty((2, SEQ, D), np.float32)
    for core in range(N_CORES):
        b, c4 = divmod(core, CPB)
        out[b, c4 * cfg.TOK:(c4 + 1) * cfg.TOK] = results[core]["outT"].T
    return out


GROUPS = [[0, 1, 2, 3], [4, 5, 6, 7]]


def na_slots(cfg):
    P = cfg.NP
    own = lambda us: [("own", u) for u in us]
    out = [(2, own(range(0, 5)))]
    out.append((0, [("edge", r, k) for r in range(CPB) for k in (2, 3)] + own(range(0, 4))))
    out.append((1, [("edge", r, 3) for r in range(CPB)] + own(range(0, 4))))
    out.append((P - 2, own(range(P - 4, P)) + [("edge", r, 0) for r in range(CPB)]))
    out.append((P - 1, own(range(P - 4, P)) + [("edge", r, k) for r in range(CPB) for k in (0, 1)]))
    return out


def na_tile_index(cfg, td):
    EB = cfg.TOKX // 128
    if td[0] == "own":
        return 2 + td[1]
    return EB + td[1] * 4 + td[2]


def bias_tables_fused(cfg, rpb, c4):
    R0 = c4 * cfg.ROWS
    erow = {0: 0, 1: 2, 2: cfg.ROWS - 4, 3: cfg.ROWS - 2}
    kp = np.arange(128)
    q = np.arange(128)
    slots = na_slots(cfg)
    nslot = sum(len(t) for _, t in slots)
    out = np.full((16, 128, nslot, 128), NEG, np.float32)
    si = 0
    for (i, tds) in slots:
        for td in tds:
            if td[0] == "own":
                krow0 = R0 + 2 * td[1]
                ok = True
            else:
                krow0 = td[1] * cfg.ROWS + erow[td[2]]
                ok = td[1] != c4
            kr = (krow0 + kp // 64)[:, None]
            kc = (kp % 64)[:, None]
            r = (R0 + 2 * i + q // 64)[None, :]
            col = (q % 64)[None, :]
            rs = np.clip(r - 4, 0, cfg.SROWS - 8)
            cs = np.clip(col - 8, 0, 48)
            valid = (kr >= rs) & (kr < rs + 8) & (kc >= cs) & (kc < cs + 16) & ok
            vals = rpb[:, np.clip(kr - r + 7, 0, 14), np.clip(kc - col + 15, 0, 30)]
            out[:, :, si, :] = np.where(valid[None], vals, NEG)
            si += 1
    return out


def phase_att_odd_fused(B, need_ctx):
    cfg, p, dr = B.cfg, B.p, B.dram
    OT = dr["OT"]
    TOKX = cfg.TOKX
    EB = TOKX // 128
    NT = EB + 4 * CPB
    NKF = TOKX + 512 * CPB
    slots = na_slots(cfg)
    nslot = sum(len(t) for _, t in slots)
    soff, o_ = {}, 0
    for (i, tds) in slots:
        soff[i] = o_
        o_ += len(tds)
    vts = [B.alloc("nvt%d" % i, [128, NT, 80], BF16) for i in range(2)]
    for i in range(2):
        p.op("pool", lambda e, i=i: e.memset(vts[i][:, :, 64:80], 1.0), writes=["nvt%d" % i])
    vown = dr["V_C_o"].rearrange("(t p) (h d) -> p t h d", p=128, d=64)
    vedge = dr["EV_g"].rearrange("(j r t p) (h d) -> j r p t h d", j=2, r=CPB, p=128, d=64)
    for h in range(16):
        kt, ktk = B.tmp("nkt", [64, NKF], BF16, 2)
        qt, qtk = B.tmp("nqt", [64, TOKX], BF16, 2)
        bt, btk = B.tmp("nbt", [128, nslot, 128], BF16, 2)
        vt, vtk = vts[h % 2], "nvt%d" % (h % 2)
        p.dma(kt[:, 0:TOKX], dr["KT_C_o"][h * 64:(h + 1) * 64, :], writes=[ktk])
        for r in range(CPB):
            eb = ((h // 8) * CPB + r) * 512 + (h % 8) * 64
            p.dma(kt[:, TOKX + r * 512:TOKX + (r + 1) * 512], dr["EK_g"][eb:eb + 64, :], writes=[ktk])
        p.dma(qt[:], dr["QT_C_o"][h * 64:(h + 1) * 64, :], writes=[qtk])
        p.dma(bt[:], dr["btab"][h, :, :, :], writes=[btk], q="pool")
        load_v(B, vt, vtk, vown[:, :, h, :], EB)
        for r in range(CPB):
            for j in range(2):
                p.dma(vt[:, EB + r * 4 + 2 * j:EB + r * 4 + 2 * j + 2, 0:64], vedge[j, r, :, :, h, :], writes=[vtk])
        if need_ctx:
            attend_group(B, kt, ktk, 64, vt, vtk, qt, qtk, 0, NCTX, [(0, None), (1, None)], OT[h, :, 0:NCTX])
        for i in range(cfg.NP):
            if i in soff and i != 2:
                tds, so = dict(slots)[i], soff[i]
            else:
                tds, so = [("own", u) for u in range(i - 2, i + 3)], 0
            tiles = [(0, None), (1, None)] + [(na_tile_index(cfg, td), bt[:, so + ti, :]) for ti, td in enumerate(tds)]
            attend_group(B, kt, ktk, 64, vt, vtk, qt, qtk, NCTX + i * 128, 128, tiles,
                         OT[h, :, NCTX + i * 128:NCTX + (i + 1) * 128], bias_key=btk)


def build_fused(cfg, nlayers=4):
    B = Bld(cfg)
    c = cfg
    xT = B.din("xT", [1024, c.TOKX], F32)
    declare_common(B, 4)
    B.din("tabA", [128, 4, c.TOKX], F32)
    B.din("tabB", [128, 4, c.TOKX], F32)
    B.din("nfin_p", [128, 8], F32)
    nslot = sum(len(t) for _, t in na_slots(cfg))
    for i in range(2):
        B.din("win_p%d" % i, [128, 8, WIN_COLS], F32)
        B.din("wq_p%d" % i, [128, 3, 1024], F32)
        B.din("wkv_p%d" % i, [128, 2, 1024], F32)
        B.din("gains_e%d" % i, [128, 12], F32)
        B.din("wna_p%d" % i, [128, 8, 3072], F32)
        B.din("btab%d" % i, [16, 128, nslot, 128], F32)
    for l in range(4):
        B.din("wout_p%d" % l, [64, 16, 1024], F32)
        B.din("wr_p%d" % l, [128, 8, 36], F32)
        B.din("br_p%d" % l, [1, 36], F32)
        B.din("wg_p%d" % l, [32, 128, 8, 512], F32)
        B.din("wu_p%d" % l, [32, 128, 8, 512], F32)
        B.din("wd_p%d" % l, [32, 128, 4, 1024], F32)
    outT = B.dout("outT", [1024, c.TOK], F32)
    xw = B.dint("xw", [1024, c.TOKX], F32)
    B.dint("OT", [16, 64, c.TOKX], BF16)
    for nm_, shp in (("QT_A_o", [8, 96, c.TOKX]), ("QT_B_o", [8, 64, c.TOKX]), ("KT_A_o", [8, 96, c.TOKX]), ("KT_B_o", [2, 64, c.TOKX]),
                     ("V_A_o", [8 * c.TOKX, 64]), ("V_B_o", [2 * c.TOKX, 64]), ("KT_A_g", [8 * CPB * 96, c.TOKX]), ("KT_B_g", [2 * CPB * 64, c.TOKX]),
                     ("V_A_g", [8 * CPB * c.TOKX, 64]), ("V_B_g", [2 * CPB * c.TOKX, 64]),
                     ("QT_C_o", [1024, c.TOKX]), ("KT_C_o", [1024, c.TOKX]), ("V_C_o", [c.TOKX, 1024]),
                     ("EK_o", [1024, 512]), ("EV_o", [512, 1024]), ("EK_g", [2 * CPB * 512, 512]), ("EV_g", [2 * CPB * 256, 1024])):
        B.dint(nm_, shp, BF16)
    B.dram["V_hm"] = True
    dr = B.dram
    dr["QT_A_i"], dr["QT_B_i"] = dr["QT_A_o"], dr["QT_B_o"]
    p = B.p
    phase_modvec(B, list(range(nlayers))); B.phase_reset()
    for l in range(nlayers):
        last = l == 3
        i = l // 2
        xin = xT if l == 0 else xw
        for k in ("wout_p", "wr_p", "br_p", "wg_p", "wu_p", "wd_p"):
            dr[k] = dr["%s%d" % (k, l)]
        if l % 2 == 0:
            for k in ("win_p", "wq_p", "wkv_p", "gains_e"):
                dr[k] = dr["%s%d" % (k, i)]
            phase_A_even(B, l, xin); B.phase_reset()
            T = c.TOKX
            ka2, kb2 = dr["KT_A_o"].rearrange("h d t -> (h d) t"), dr["KT_B_o"].rearrange("h d t -> (h d) t")
            for h in range(8):
                p.coll("AllGather", GROUPS, ka2[h * 96:(h + 1) * 96, :], dr["KT_A_g"][h * CPB * 96:(h + 1) * CPB * 96, :], writes=["cc_ka%d" % h])
                p.coll("AllGather", GROUPS, dr["V_A_o"][h * T:(h + 1) * T, :], dr["V_A_g"][h * CPB * T:(h + 1) * CPB * T, :], writes=["cc_va%d" % h])
            for h in range(2):
                p.coll("AllGather", GROUPS, kb2[h * 64:(h + 1) * 64, :], dr["KT_B_g"][h * CPB * 64:(h + 1) * CPB * 64, :], writes=["cc_kb%d" % h])
                p.coll("AllGather", GROUPS, dr["V_B_o"][h * T:(h + 1) * T, :], dr["V_B_g"][h * CPB * T:(h + 1) * CPB * T, :], writes=["cc_vb%d" % h])
            B.phase_reset()
            phase_att_even(B, not last, gathered=True); B.phase_reset()
        else:
            dr["wna_p"], dr["btab"] = dr["wna_p%d" % i], dr["btab%d" % i]
            phase_A_odd(B, l, xin); B.phase_reset()
            for j in range(2):
                p.coll("AllGather", GROUPS, dr["EK_o"][j * 512:(j + 1) * 512, :], dr["EK_g"][j * CPB * 512:(j + 1) * CPB * 512, :], writes=["cc_ek%d" % j])
                p.coll("AllGather", GROUPS, dr["EV_o"][j * 256:(j + 1) * 256, :], dr["EV_g"][j * CPB * 256:(j + 1) * CPB * 256, :], writes=["cc_ev%d" % j])
            B.phase_reset()
            phase_att_odd_fused(B, not last); B.phase_reset()
        phase_outproj(B, l, xin, xw, last); B.phase_reset()
        phase_ffn(B, l, xw, last); B.phase_reset()
    if nlayers == 4:
        phase_final(B, xw, outT)
    B.phase_reset()
    B.p.emit()
    return B


def fused_inputs(inp, cfg):
    inp = {k: np.asarray(v, np.float32) for k, v in inp.items()}
    shared = prep_common(inp, [0, 1, 2, 3])
    shared["nfin_p"] = np.ascontiguousarray(inp["norm_final"].reshape(8, 128).T)
    for i in range(2):
        for k, v in prep_even(inp, i).items():
            shared["%s%d" % (k, i)] = v
        shared["wna_p%d" % i] = _ktile(inp["na_w_in"][i])
    for l in range(4):
        for k, v in prep_moe(inp, l).items():
            shared["%s%d" % (k, l)] = v
        shared["wout_p%d" % l] = wout_p(inp["ab_w_out"][l // 2] if l % 2 == 0 else inp["na_w_out"][l // 2])
    cins = [prep_cin(inp, b) for b in range(2)]
    tabs = [rope_tables(cfg, c4) for c4 in range(CPB)]
    bts = [[bias_tables_fused(cfg, inp["na_rpb"][i], c4) for i in range(2)] for c4 in range(CPB)]
    maps = []
    for core in range(N_CORES):
        b, c4 = divmod(core, CPB)
        m = dict(shared)
        m["c_in"] = cins[b]
        xl = inp["x"][b, c4 * cfg.TOK:(c4 + 1) * cfg.TOK]
        m["xT"] = np.ascontiguousarray(np.concatenate([inp["ctx"][b], xl], 0).T)
        m["tabA"], m["tabB"] = tabs[c4]
        m["btab0"], m["btab1"] = bts[c4]
        maps.append(m)
    return maps


def forward_fused(inp, SEQ):
    cfg = Cfg(SEQ)
    key = ("fused", SEQ)
    if key not in _PROG_CACHE:
        _PROG_CACHE[key] = build_fused(cfg)
    B = _PROG_CACHE[key]
    maps = fused_inputs(inp, cfg)
    res = run_bass_kernel_spmd(B.nc, maps, core_ids=list(range(N_CORES))).results
    out = np.empty((2, SEQ, D), np.float32)
    for core in range(N_CORES):
        b, c4 = divmod(core, CPB)
        out[b, c4 * cfg.TOK:(c4 + 1) * cfg.TOK] = res[core]["outT"].T
    return out


def kernel(**inputs):
    return forward_fused(inputs, 16384)
```

```python
import numpy as np
import ml_dtypes
import contextlib
import concourse.bass as bass
import concourse.mybir as mybir
from concourse.bass_utils import run_bass_kernel_spmd

F32 = mybir.dt.float32
BF16 = mybir.dt.bfloat16
AF = mybir.ActivationFunctionType
ALU = mybir.AluOpType
AX = mybir.AxisListType

D = 1024
NCTX = 256
CPB = 4
NEG = -30000.0
EPS = 1e-6
N_DMA_SEMS = 24

CQ, CKV, KRA, KRB, GQA, GQB, GKA, GKB, GV, WIN_COLS = 0, 384, 640, 672, 704, 1216, 1728, 1856, 1984, 2112


class Cfg:
    def __init__(self, SEQ):
        self.SEQ = SEQ
        self.TOK = SEQ // CPB
        self.TOKX = self.TOK + NCTX
        self.ROWS = self.TOK // 64
        self.SROWS = SEQ // 64
        self.NKEY = NCTX + SEQ
        self.NKT = self.NKEY // 128
        self.NP = self.ROWS // 2
        self.NKC = NCTX + (self.ROWS + 8) * 64
        self.NKCT = self.NKC // 128

    def chunks(self, with_ctx=True):
        out = [(0, NCTX)] if with_ctx else []
        for i in range(self.TOK // 512):
            out.append((NCTX + i * 512, 512))
        return out


class Prog:
    ENGS = ("pe", "act", "dve", "pool", "sp")

    def __init__(self, nc):
        self.nc = nc
        self.q = {k: [] for k in self.ENGS}
        self.cnt = {k: 0 for k in ("pe", "act", "dve", "pool")}
        self.sem = {}
        self.dsem = []
        self.dcnt = [0] * N_DMA_SEMS
        self.drr = 0
        self.known = {k: {} for k in self.ENGS}
        self.res = {}
        self.n_inst = 0
        self.n_wait = 0

    def _need(self, eng, deps):
        kn = self.known[eng]
        for (sk, v) in deps:
            if sk == "pe" and eng == "pe":
                continue
            if kn.get(sk, 0) >= v:
                continue
            kn[sk] = v
            self.n_wait += 1
            self.q[eng].append(("wait", sk, v))

    def _collect(self, reads, writes):
        deps = set()
        for r in reads:
            st = self.res.get(r)
            if st and st[0] is not None:
                deps.add(st[0])
        for w in writes:
            st = self.res.get(w)
            if st:
                if st[0] is not None:
                    deps.add(st[0])
                deps.update(st[1])
        return deps

    def _commit(self, me, reads, writes):
        for r in reads:
            st = self.res.setdefault(r, [None, []])
            st[1] = [d for d in st[1] if d[0] != me[0]] + [me]
        for w in writes:
            self.res[w] = [me, []]

    def op(self, eng, fn, reads=(), writes=()):
        deps = self._collect(reads, writes)
        self._need(eng, deps)
        self.cnt[eng] += 1
        me = (eng, self.cnt[eng])
        self.q[eng].append(("op", fn))
        self.n_inst += 1
        self._commit(me, reads, writes)
        return me

    def dma(self, out, in_, reads=(), writes=(), q="sp"):
        deps = self._collect(reads, writes)
        self._need(q, deps)
        k = self.drr
        self.drr = (self.drr + 1) % N_DMA_SEMS
        sk = ("d", k)
        if self.dcnt[k]:
            self._need(q, [(sk, self.dcnt[k])])
        self.dcnt[k] += 16
        me = (sk, self.dcnt[k])
        self.q[q].append(("dma", out, in_, k))
        self.n_inst += 1
        self._commit(me, reads, writes)
        return me

    def coll(self, kind, groups, src, dst, reads=(), writes=()):
        deps = self._collect(reads, writes)
        self._need("pool", deps)
        self.ccnt = getattr(self, "ccnt", 0) + 1
        me = ("cc", self.ccnt)
        self.q["pool"].append(("coll", kind, groups, src, dst))
        self.n_inst += 1
        self._commit(me, reads, writes)
        self._need("pool", [me])
        return me

    def full_barrier(self):
        deps = [(k, self.cnt[k]) for k in self.cnt if self.cnt[k]]
        deps += [(("d", k), self.dcnt[k]) for k in range(N_DMA_SEMS) if self.dcnt[k]]
        if getattr(self, "ccnt", 0):
            deps.append(("cc", self.ccnt))
        for e in self.ENGS:
            self._need(e, deps)
        self.res = {}

    def _sem(self, sk):
        return self.dsem[sk[1]] if isinstance(sk, tuple) else self.sem[sk]

    def emit(self):
        nc = self.nc
        with contextlib.ExitStack() as es:
            for k in ("pe", "act", "dve", "pool", "cc"):
                self.sem[k] = es.enter_context(nc.semaphore("s_" + k))
            for i in range(N_DMA_SEMS):
                self.dsem.append(es.enter_context(nc.semaphore("s_d%d" % i)))
            block = es.enter_context(nc.Block())
            for name, deco in (("sp", block.sync), ("pe", block.tensor), ("act", block.scalar),
                               ("dve", block.vector), ("pool", block.gpsimd)):
                items = self.q[name]

                def body(eng, items=items, name=name):
                    for it in items:
                        if it[0] == "wait":
                            eng.wait_ge(self._sem(it[1]), it[2])
                        elif it[0] == "op":
                            it[1](eng).then_inc(self.sem[name], 1)
                        elif it[0] == "coll":
                            eng.collective_compute(it[1], ALU.bypass, replica_groups=it[2], ins=[it[3]], outs=[it[4]]).then_inc(self.sem["cc"])
                        else:
                            eng.dma_start(out=it[1], in_=it[2]).then_inc(self.dsem[it[3]], 16)
                deco(body)


def _isz(dt):
    return 4 if dt == F32 else 2


class Bld:
    SBUF_BYTES = 229000

    def __init__(self, cfg):
        self.cfg = cfg
        self.nc = bass.Bass("TRN2", target_bir_lowering=False)
        self.p = Prog(self.nc)
        self.off = 16384 + 512
        self.uid = 0
        self.pools = {}
        self.dram = {}
        nc = self.nc
        self.pw = [nc.alloc_psum_tensor("pw%d" % i, [128, 1024], F32) for i in range(4)]
        self.ident_f = self.alloc("ident_f", [128, 128], F32)
        self.ident_b = self.alloc("ident_b", [128, 128], BF16)
        self.ones_f = self.alloc("ones_f", [128, 128], F32)
        self.ones_b = self.alloc("ones_b", [128, 128], BF16)
        self.blk_f = self.alloc("blk_f", [128, 128], F32)
        self.epsD = self.alloc("epsD", [128, 4], F32)
        self.mv = self.alloc("mv", [128, 4, 2, 6, 8], F32)
        p = self.p
        p.op("pool", lambda e: e.memset(self.ident_f[:], 0.0), writes=["ident_f"])
        p.op("pool", lambda e: e.affine_select(out=self.ident_f[:], in_=self.ident_f[:], pattern=[[-1, 128]],
                                               compare_op=ALU.not_equal, fill=1.0, base=0, channel_multiplier=1),
             reads=["ident_f"], writes=["ident_f"])
        p.op("dve", lambda e: e.tensor_copy(out=self.ident_b[:], in_=self.ident_f[:]), reads=["ident_f"], writes=["ident_b"])
        p.op("dve", lambda e: e.memset(self.ones_f[:], 1.0), writes=["ones_f"])
        p.op("dve", lambda e: e.memset(self.ones_b[:], 1.0), writes=["ones_b"])
        p.op("dve", lambda e: e.memset(self.blk_f[:], 0.0), writes=["blk_f"])
        p.op("dve", lambda e: e.memset(self.blk_f[0:64, 0:64], 1.0), reads=["blk_f"], writes=["blk_f"])
        p.op("dve", lambda e: e.memset(self.blk_f[64:128, 64:128], 1.0), reads=["blk_f"], writes=["blk_f"])
        p.op("dve", lambda e: e.memset(self.epsD[:], EPS), writes=["epsD"])
        self.base_off = self.off

    def alloc(self, name, shape, dt):
        nb = int(np.prod(shape[1:])) * _isz(dt)
        nb = (nb + 63) // 64 * 64
        assert self.off + nb <= self.SBUF_BYTES, ("SBUF overflow", name, self.off, nb)
        self.uid += 1
        t = self.nc.alloc_sbuf_tensor_at("%s_%d" % (name, self.uid), list(shape), dt, offset=self.off)
        self.off += nb
        return t

    def phase_reset(self):
        self.p.full_barrier()
        self.off = self.base_off
        self.pools = {}

    def tmp(self, tag, shape, dt, bufs=2):
        if tag not in self.pools:
            self.pools[tag] = [[(self.alloc(tag, shape, dt)) for _ in range(bufs)], 0]
        pl = self.pools[tag]
        t = pl[0][pl[1] % bufs]
        k = "%s#%d@%d" % (tag, pl[1] % bufs, id(pl))
        pl[1] += 1
        return t, k

    def din(self, name, shape, dt):
        self.dram[name] = self.nc.dram_tensor(name, list(shape), dt, kind="ExternalInput").ap()
        return self.dram[name]

    def dout(self, name, shape, dt):
        self.dram[name] = self.nc.dram_tensor(name, list(shape), dt, kind="ExternalOutput").ap()
        return self.dram[name]

    def dint(self, name, shape, dt):
        self.dram[name] = self.nc.dram_tensor(name, list(shape), dt).ap()
        return self.dram[name]

    def bank(self, i):
        return self.pw[i // 2][:, (i % 2) * 512:(i % 2) * 512 + 512], "bank%d" % i

    def mm(self, out, lhsT, rhs, start, stop, reads, writes):
        self.p.op("pe", lambda e: e.matmul(out, lhsT, rhs, start=start, stop=stop), reads, writes)

    def act(self, out, in_, func, reads, writes, bias=None, scale=1.0, accum_out=None):
        kw = {}
        if bias is not None:
            kw["bias"] = bias
        if accum_out is not None:
            kw["accum_out"] = accum_out
        self.p.op("act", lambda e: e.activation(out=out, in_=in_, func=func, scale=scale, **kw), reads, writes)

    def stt(self, out, in0, scalar, in1, op0, op1, reads, writes, eng="dve"):
        self.p.op(eng, lambda e: e.scalar_tensor_tensor(out=out, in0=in0, scalar=scalar, in1=in1, op0=op0, op1=op1), reads, writes)

    def tt(self, out, in0, in1, op, reads, writes, eng="dve"):
        self.p.op(eng, lambda e: e.tensor_tensor(out=out, in0=in0, in1=in1, op=op), reads, writes)

    def ts(self, out, in0, s1, s2, op0, op1, reads, writes, eng="dve", accum_out=None):
        if op1 is None:
            self.p.op(eng, lambda e: e.tensor_scalar(out=out, in0=in0, scalar1=s1, scalar2=None, op0=op0), reads, writes)
        elif accum_out is None:
            self.p.op(eng, lambda e: e.tensor_scalar(out=out, in0=in0, scalar1=s1, scalar2=s2, op0=op0, op1=op1), reads, writes)
        else:
            self.p.op(eng, lambda e: e.tensor_scalar(out=out, in0=in0, scalar1=s1, scalar2=s2, op0=op0, op1=op1, accum_out=accum_out), reads, writes)

    def cp(self, out, in_, reads, writes, eng="dve"):
        if eng == "act":
            self.p.op("act", lambda e: e.copy(out=out, in_=in_), reads, writes)
        else:
            self.p.op(eng, lambda e: e.tensor_copy(out=out, in_=in_), reads, writes)

    def recip(self, out, in_, reads, writes):
        self.p.op("dve", lambda e: e.reciprocal(out=out, in_=in_), reads, writes)

    def rstd_from_sum(self, ps_ap, ps_key, n_feat, n, tag, np_=128):
        t, k = self.tmp(tag, [128, 512], F32, bufs=2)
        self.act(t[0:np_, 0:n], ps_ap, AF.Sqrt, [ps_key, "epsD"], [k], bias=self.epsD[0:np_, 0:1], scale=1.0 / n_feat)
        self.recip(t[0:np_, 0:n], t[0:np_, 0:n], [k], [k])
        return t, k


def phase_modvec(B, layers):
    p = B.p
    c_in = B.dram["c_in"]
    cs, ck = B.tmp("c_s", [128, 8, 2], F32, 1)
    cb, cbk = B.tmp("c_b", [128, 8, 2], BF16, 1)
    p.dma(cs[:], c_in[:, :, :], writes=[ck])
    B.act(cb[:], cs[:], AF.Silu, [ck], [cbk])
    nm, nmk = B.tmp("nm", [128, 4, 8], F32, 1)
    nf, nfk = B.tmp("nf", [128, 4, 8], F32, 1)
    ba, bak = B.tmp("ba", [128, 4, 48], F32, 1)
    p.dma(nm[:], B.dram["nmix_p"][:, :, :], writes=[nmk])
    p.dma(nf[:], B.dram["nffn_p"][:, :, :], writes=[nfk])
    p.dma(ba[:], B.dram["bada_p"][:, :, :], writes=[bak])
    M, Mk = B.tmp("modM", [128, 48, 2], F32, 1)
    for li, l in enumerate(layers):
        wada = B.dram["wada_p"]
        ps, psk = B.bank(0)
        for cbk_i in range(12):
            w, wk = B.tmp("wada_blk", [128, 8, 512], BF16, 2)
            p.dma(w[:], wada[li, :, :, cbk_i * 512:(cbk_i + 1) * 512], writes=[wk], q="pool")
            for f in range(4):
                ft = cbk_i * 4 + f
                for k in range(8):
                    B.mm(ps[:, ft * 2:ft * 2 + 2], w[:, k, f * 128:(f + 1) * 128], cb[:, k, :], k == 0, k == 7,
                         [wk, cbk], [psk])
        B.tt(M[:], ps[:, 0:96].rearrange("p (t j) -> p t j", j=2), ba[:, l, :].unsqueeze(2).to_broadcast([128, 48, 2]),
             ALU.add, [psk, bak], [Mk])
        for j in range(2):
            mvl = B.mv[:, l, j]
            B.stt(mvl[:, 0, :], M[:, 8:16, j], 1.0, nm[:, l, :], ALU.add, ALU.mult, [Mk, nmk], ["mv"])
            B.cp(mvl[:, 1, :], M[:, 0:8, j], [Mk], ["mv"])
            B.cp(mvl[:, 2, :], M[:, 16:24, j], [Mk], ["mv"])
            B.stt(mvl[:, 3, :], M[:, 32:40, j], 1.0, nf[:, l, :], ALU.add, ALU.mult, [Mk, nfk], ["mv"])
            B.cp(mvl[:, 4, :], M[:, 24:32, j], [Mk], ["mv"])
            B.cp(mvl[:, 5, :], M[:, 40:48, j], [Mk], ["mv"])


def norm_mod(B, xt, xk, n, l, j, kind0, want_f32=False, tagp=""):
    sq, sqk = B.tmp("nm_sq" + tagp, [128, 8, 512], F32, 1)
    B.act(sq[:, :, 0:n], xt, AF.Square, [xk], [sqk])
    ps, psk = B.bank(0)
    for k in range(8):
        B.mm(ps[:, 0:n], B.ones_f[:], sq[:, k, 0:n], k == 0, k == 7, ["ones_f", sqk], [psk])
    rs, rsk = B.rstd_from_sum(ps[:, 0:n], psk, float(D), n, "nm_rstd" + tagp)
    hT, hk = B.tmp("hT" + tagp, [128, 8, 512], BF16, 1)
    hf = hfk = None
    if want_f32:
        hf, hfk = B.tmp("hTf" + tagp, [128, 8, 512], F32, 1)
    for k in range(8):
        t, tk = B.tmp("nm_t" + tagp, [128, 512], F32, 2)
        B.stt(t[:, 0:n], xt[:, k, :], B.mv[:, l, j, kind0, k:k + 1], rs[:, 0:n], ALU.mult, ALU.mult, [xk, "mv", rsk], [tk])
        if want_f32:
            B.act(hf[:, k, 0:n], t[:, 0:n], AF.Identity, [tk, "mv"], [hfk], bias=B.mv[:, l, j, kind0 + 1, k:k + 1])
            B.cp(hT[:, k, 0:n], hf[:, k, 0:n], [hfk], [hk])
        else:
            B.act(hT[:, k, 0:n], t[:, 0:n], AF.Identity, [tk, "mv"], [hk], bias=B.mv[:, l, j, kind0 + 1, k:k + 1])
    return hT, hk, hf, hfk


def phase_A_even(B, l, xT_dram):
    cfg, p = B.cfg, B.p
    dr = B.dram
    win, wink = B.tmp("win", [128, 8, WIN_COLS], BF16, 1)
    wq, wqk = B.tmp("wq", [128, 3, 1024], BF16, 1)
    wkv, wkvk = B.tmp("wkv", [128, 2, 1024], BF16, 1)
    gn, gnk = B.tmp("gains", [128, 12], F32, 1)
    for k in range(8):
        p.dma(win[:, k, :], dr["win_p"][:, k, :], writes=[wink], q="pool")
    p.dma(wq[:], dr["wq_p"][:, :, :], writes=[wqk], q="pool")
    p.dma(wkv[:], dr["wkv_p"][:, :, :], writes=[wkvk], q="pool")
    p.dma(gn[:], dr["gains_e"][:, :], writes=[gnk])
    QTA, QTB, KTA, KTB, VA, VB = (dr[k] for k in ("QT_A_o", "QT_B_o", "KT_A_o", "KT_B_o", "V_A_o", "V_B_o"))
    for (c0, n) in cfg.chunks(True):
        j = 1 if c0 == 0 else 0
        xt, xk = B.tmp("xT_a", [128, 8, 512], F32, 1)
        p.dma(xt[:, :, 0:n], xT_dram.rearrange("(k p) t -> p k t", p=128)[:, :, c0:c0 + n], writes=[xk])
        hT, hk, _, _ = norm_mod(B, xt[:, :, 0:n], xk, n, l, j, 0)
        ta, tak = B.tmp("tabA", [128, 4, 512], F32, 1)
        tb, tbk = B.tmp("tabB", [128, 4, 512], F32, 1)
        p.dma(ta[64:96, :, 0:n], dr["tabA"][64:96, :, c0:c0 + n], writes=[tak])
        p.dma(tb[:, :, 0:n], dr["tabB"][:, :, c0:c0 + n], writes=[tbk])

        def proj(ps_ap, psk, col0, m, out_base=0):
            for k in range(8):
                B.mm(ps_ap, win[:, k, col0:col0 + m], hT[:, k, 0:n], k == 0, k == 7, [wink, hk], [psk])

        def latent(col0, ntile, gcol0, nfeat, tag):
            raw, rawk = B.tmp("lat_raw", [128, 3, 512], F32, 1)
            sq, sqk = B.tmp("lat_sq", [128, 3, 512], F32, 1)
            for m in range(ntile):
                ps, psk = B.bank(2 + (m % 2))
                proj(ps[:, 0:n], psk, col0 + m * 128, 128)
                B.cp(raw[:, m, 0:n], ps[:, 0:n], [psk], [rawk], eng="act")
                B.act(sq[:, m, 0:n], ps[:, 0:n], AF.Square, [psk], [sqk])
            ps, psk = B.bank(1)
            for m in range(ntile):
                B.mm(ps[:, 0:n], B.ones_f[:], sq[:, m, 0:n], m == 0, m == ntile - 1, ["ones_f", sqk], [psk])
            rs, rsk = B.rstd_from_sum(ps[:, 0:n], psk, float(nfeat), n, tag + "_rstd")
            o, ok_ = B.tmp(tag + "_n", [128, 3, 512], BF16, 2)
            for m in range(ntile):
                B.stt(o[:, m, 0:n], raw[:, m, 0:n], gn[:, gcol0 + m:gcol0 + m + 1], rs[:, 0:n], ALU.mult, ALU.mult,
                      [rawk, gnk, rsk], [ok_])
            return o, ok_

        cqn, cqnk = latent(CQ, 3, 0, 384, "cq")
        ckvn, ckvnk = latent(CKV, 2, 3, 256, "ckv")

        psA, psAk = B.bank(2)
        psB, psBk = B.bank(3)
        proj(psA[64:96, 0:n], psAk, KRA, 32)
        proj(psB[64:96, 0:n], psBk, KRB, 32)
        kst, kstk = B.tmp("kstage", [128, 512], BF16, 2)
        t1, t1k = B.tmp("rope_t1", [128, 512], F32, 2)
        t2, t2k = B.tmp("rope_t2", [128, 512], F32, 2)
        B.tt(t1[64:96, 0:n], psA[64:96, 0:n], ta[64:96, 2, 0:n], ALU.mult, [psAk, tak], [t1k])
        B.tt(t2[64:96, 0:n], psB[64:96, 0:n], ta[64:96, 3, 0:n], ALU.mult, [psBk, tak], [t2k])
        B.tt(kst[64:96, 0:n], t1[64:96, 0:n], t2[64:96, 0:n], ALU.add, [t1k, t2k], [kstk])
        for h in range(8):
            p.dma(KTA[h, 64:96, c0:c0 + n], kst[64:96, 0:n], reads=[kstk])
        for h in range(8):
            ps, psk = B.bank(2 + (h % 2))
            for k in range(2):
                B.mm(ps[0:64, 0:n], wkv[:, k, h * 64:(h + 1) * 64], ckvn[:, k, 0:n], k == 0, k == 1, [wkvk, ckvnk], [psk])
            kn, knk = B.tmp("knope", [64, 512], BF16, 3)
            B.cp(kn[:, 0:n], ps[0:64, 0:n], [psk], [knk], eng="act" if h % 2 else "dve")
            p.dma(KTA[h, 0:64, c0:c0 + n], kn[:, 0:n], reads=[knk])
        for jt in range(n // 128):
            ps, psk = B.bank(4 + (jt % 2))
            for k in range(2):
                B.mm(ps[:, 0:512], ckvn[:, k, jt * 128:(jt + 1) * 128], wkv[:, k, 512:1024], k == 0, k == 1, [ckvnk, wkvk], [psk])
            v, vk = B.tmp("va_tok", [128, 512], BF16, 2)
            B.cp(v[:], ps[:, 0:512], [psk], [vk], eng="act" if jt % 2 else "dve")
            if "V_hm" in dr:
                p.dma(VA.rearrange("(h t) d -> t h d", h=8)[c0 + jt * 128:c0 + (jt + 1) * 128, :, :], v[:].rearrange("p (h d) -> p h d", d=64), reads=[vk])
            else:
                p.dma(VA[c0 + jt * 128:c0 + (jt + 1) * 128, :], v[:], reads=[vk])
        for h in range(8):
            psA, psAk = B.bank(2 + 2 * (h % 2))
            psB, psBk = B.bank(3 + 2 * (h % 2))
            for k in range(3):
                B.mm(psA[0:96, 0:n], wq[:, k, h * 96:(h + 1) * 96], cqn[:, k, 0:n], k == 0, k == 2, [wqk, cqnk], [psAk])
            for k in range(3):
                B.mm(psB[64:96, 0:n], wq[:, k, 768 + h * 32:768 + (h + 1) * 32], cqn[:, k, 0:n], k == 0, k == 2, [wqk, cqnk], [psBk])
            qs, qsk = B.tmp("qstage", [128, 512], BF16, 3)
            B.act(qs[0:64, 0:n], psA[0:64, 0:n], AF.Copy, [psAk], [qsk], scale=96.0 ** -0.5)
            t1, t1k = B.tmp("rope_t1", [128, 512], F32, 2)
            t2, t2k = B.tmp("rope_t2", [128, 512], F32, 2)
            B.tt(t1[64:96, 0:n], psA[64:96, 0:n], ta[64:96, 0, 0:n], ALU.mult, [psAk, tak], [t1k])
            B.tt(t2[64:96, 0:n], psB[64:96, 0:n], ta[64:96, 1, 0:n], ALU.mult, [psBk, tak], [t2k])
            B.tt(qs[64:96, 0:n], t1[64:96, 0:n], t2[64:96, 0:n], ALU.add, [t1k, t2k, qsk], [qsk])
            p.dma(QTA[h, :, c0:c0 + n], qs[0:96, 0:n], reads=[qsk])

        def gqa_tile(colA, colB, gA, gB, ci, si, dst_fn):
            psA, psAk = B.bank(6)
            psB, psBk = B.bank(7)
            proj(psA[:, 0:n], psAk, colA, 128)
            proj(psB[:, 0:n], psBk, colB, 128)
            sq, sqk = B.tmp("g_sq", [128, 512], F32, 2)
            B.act(sq[:, 0:n], psA[:, 0:n], AF.Square, [psAk], [sqk])
            pss, pssk = B.bank(1)
            B.mm(pss[:, 0:n], B.blk_f[:], sq[:, 0:n], True, True, ["blk_f", sqk], [pssk])
            rs, rsk = B.rstd_from_sum(pss[:, 0:n], pssk, 64.0, n, "g_rstd")
            u1, u1k = B.tmp("g_u1", [128, 512], F32, 2)
            u2, u2k = B.tmp("g_u2", [128, 512], F32, 2)
            B.stt(u1[:, 0:n], psA[:, 0:n], gn[:, gA:gA + 1], tb[:, ci, 0:n], ALU.mult, ALU.mult, [psAk, gnk, tbk], [u1k])
            B.stt(u2[:, 0:n], psB[:, 0:n], gn[:, gB:gB + 1], tb[:, si, 0:n], ALU.mult, ALU.mult, [psBk, gnk, tbk], [u2k])
            B.tt(u1[:, 0:n], u1[:, 0:n], u2[:, 0:n], ALU.add, [u1k, u2k], [u1k], eng="pool")
            o, ok_ = B.tmp("g_out", [128, 512], BF16, 3)
            B.tt(o[:, 0:n], u1[:, 0:n], rs[:, 0:n], ALU.mult, [u1k, rsk], [ok_])
            dst_fn(o, ok_)

        for m in range(4):
            def dst(o, ok_, m=m):
                p.dma(QTB.rearrange("h d t -> (h d) t")[m * 128:(m + 1) * 128, c0:c0 + n], o[:, 0:n], reads=[ok_])
            gqa_tile(GQA + m * 128, GQB + m * 128, 5, 6, 0, 1, dst)

        def dstk(o, ok_):
            p.dma(KTB.rearrange("h d t -> (h d) t")[:, c0:c0 + n], o[:, 0:n], reads=[ok_])
        gqa_tile(GKA, GKB, 7, 8, 2, 3, dstk)
        for jt in range(n // 128):
            ps, psk = B.bank(4 + (jt % 2))
            for k in range(8):
                B.mm(ps[:, 0:128], hT[:, k, jt * 128:(jt + 1) * 128], win[:, k, GV:GV + 128], k == 0, k == 7, [hk, wink], [psk])
            v, vk = B.tmp("vb_tok", [128, 128], BF16, 2)
            B.cp(v[:], ps[:, 0:128], [psk], [vk], eng="act" if jt % 2 else "dve")
            if "V_hm" in dr:
                p.dma(VB.rearrange("(h t) d -> t h d", h=2)[c0 + jt * 128:c0 + (jt + 1) * 128, :, :], v[:].rearrange("p (h d) -> p h d", d=64), reads=[vk])
            else:
                p.dma(VB[c0 + jt * 128:c0 + (jt + 1) * 128, :], v[:], reads=[vk])


def attend_group(B, kt, ktk, d, vt, vtk, qt, qtk, qcol0, nq, key_tiles, out_dram_ap, bias_key=None):
    p = B.p
    per = 1024 // nq
    groups = [key_tiles[i:i + per] for i in range(0, len(key_tiles), per)]
    po, pok = B.bank(6)
    first = True
    pend = []
    for gi, grp in enumerate(groups):
        B._pw = (getattr(B, "_pw", 0) + 1) % 3
        ps = B.pw[B._pw]
        psk = "wide%d" % B._pw
        for ti, (t, bias_ap) in enumerate(grp):
            sl = ps[:, ti * nq:(ti + 1) * nq]
            B.mm(sl, kt[0:d, t * 128:(t + 1) * 128], qt[0:d, qcol0:qcol0 + nq], True, bias_ap is None, [ktk, qtk], [psk])
            if bias_ap is not None:
                B.mm(sl, B.ident_b[:], bias_ap, False, True, ["ident_b", bias_key], [psk])
        pt, ptk = B.tmp("pT", [128, 1024], BF16, 4)
        ncol = len(grp) * nq
        if len(pend) >= 2:
            pend.pop(0)()
        B.act(pt[:, 0:ncol], ps[:, 0:ncol], AF.Exp, [psk], [ptk])

        def pv(grp=grp, pt=pt, ptk=ptk, is_first=first, is_last=(gi == len(groups) - 1)):
            for ti, (t, _) in enumerate(grp):
                B.mm(po[0:65, 0:nq], vt[:, t, 0:65], pt[:, ti * nq:(ti + 1) * nq], is_first and ti == 0,
                     is_last and ti == len(grp) - 1, [vtk, ptk], [pok])
        pend.append(pv)
        first = False
    while pend:
        pend.pop(0)()
    rc, rck = B.tmp("att_rc", [128, 512], F32, 2)
    B.recip(rc[64:65, 0:nq], po[64:65, 0:nq], [pok], [rck])
    pb, pbk = B.bank(7)
    B.mm(pb[0:64, 0:nq], B.ones_f[64:65, 0:64], rc[64:65, 0:nq], True, True, ["ones_f", rck], [pbk])
    bc, bck = B.tmp("att_bc", [64, 512], F32, 2)
    B.cp(bc[:, 0:nq], pb[0:64, 0:nq], [pbk], [bck], eng="act")
    o, ok_ = B.tmp("att_o", [64, 512], BF16, 3)
    B.tt(o[:, 0:nq], po[0:64, 0:nq], bc[:, 0:nq], ALU.mult, [pok, bck], [ok_])
    p.dma(out_dram_ap, o[:, 0:nq], reads=[ok_])


def load_v(B, vt, vtk, vsrc, ntiles, step=10):
    for a in range(0, ntiles, step):
        b_ = min(ntiles, a + step)
        B.p.dma(vt[:, a:b_, 0:64], vsrc[:, a:b_, :], writes=[vtk])


def phase_att_even(B, need_ctx, gathered=False):
    cfg, p, dr = B.cfg, B.p, B.dram
    TOK, TOKX = cfg.TOK, cfg.TOKX
    tpr = TOK // 128
    OT = dr["OT"]
    NKT = cfg.NKT
    vts = [B.alloc("vt%d" % i, [128, NKT, 80], BF16) for i in range(2)]
    for i in range(2):
        p.op("pool", lambda e, i=i: e.memset(vts[i][:, :, 64:80], 1.0), writes=["vt%d" % i])
    for hh in range(16):
        isA = hh < 8
        h = hh if isA else hh - 8
        d = 96 if isA else 64
        kt, ktk = B.tmp("ktA", [96, cfg.NKEY], BF16, 2)
        qt, qtk = B.tmp("qt", [96, cfg.TOKX], BF16, 2)
        vt, vtk = vts[hh % 2], "vt%d" % (hh % 2)
        if gathered:
            kg = dr["KT_A_g"] if isA else dr["KT_B_g"]
            hk, dd = (h, 96) if isA else (h // 4, 64)
            p.dma(kt[0:d, 0:NCTX], kg[(hk * CPB) * dd:(hk * CPB) * dd + d, 0:NCTX], writes=[ktk])
            for r in range(CPB):
                p.dma(kt[0:d, NCTX + r * TOK:NCTX + (r + 1) * TOK], kg[(hk * CPB + r) * dd:(hk * CPB + r) * dd + d, NCTX:TOKX], writes=[ktk])
            p.dma(qt[0:d, :], (dr["QT_A_i"] if isA else dr["QT_B_i"])[h, :, :], writes=[qtk])
            vg = dr["V_A_g"] if isA else dr["V_B_g"]
            vv = vg.rearrange("(a t p) d -> a p t d", p=128, t=TOKX // 128)
            p.dma(vt[:, 0:2, 0:64], vv[hk * CPB, :, 0:2, :], writes=[vtk])
            for r in range(CPB):
                for a in range(0, tpr, 8):
                    b_ = min(tpr, a + 8)
                    p.dma(vt[:, 2 + r * tpr + a:2 + r * tpr + b_, 0:64], vv[hk * CPB + r, :, 2 + a:2 + b_, :], writes=[vtk])
        elif isA:
            p.dma(kt[0:96, :], dr["KT_A_f"][h, :, :], writes=[ktk])
            p.dma(qt[0:96, :], dr["QT_A_i"][h, :, :], writes=[qtk])
            load_v(B, vt, vtk, dr["V_A_f"].rearrange("(t p) (h d) -> p t h d", p=128, d=64)[:, :, h, :], NKT)
        else:
            p.dma(kt[0:64, :], dr["KT_B_f"][h // 4, :, :], writes=[ktk])
            p.dma(qt[0:64, :], dr["QT_B_i"][h, :, :], writes=[qtk])
            load_v(B, vt, vtk, dr["V_B_f"].rearrange("(t p) (h d) -> p t h d", p=128, d=64)[:, :, h // 4, :], NKT)
        if need_ctx:
            attend_group(B, kt, ktk, d, vt, vtk, qt, qtk, 0, NCTX, [(0, None), (1, None)], OT[hh, :, 0:NCTX])
        for (c0, n) in cfg.chunks(False):
            attend_group(B, kt, ktk, d, vt, vtk, qt, qtk, c0, n, [(t, None) for t in range(NKT)], OT[hh, :, c0:c0 + n])


def phase_A_odd(B, l, xT_dram):
    cfg, p, dr = B.cfg, B.p, B.dram
    w, wk = B.tmp("wna", [128, 8, 3072], BF16, 1)
    for k in range(8):
        p.dma(w[:, k, :], dr["wna_p"][:, k, :], writes=[wk], q="pool")
    QT, KT, V = dr["QT_C_o"], dr["KT_C_o"], dr["V_C_o"]
    for (c0, n) in cfg.chunks(True):
        j = 1 if c0 == 0 else 0
        xt, xk = B.tmp("xT_a", [128, 8, 512], F32, 1)
        p.dma(xt[:, :, 0:n], xT_dram.rearrange("(k p) t -> p k t", p=128)[:, :, c0:c0 + n], writes=[xk])
        hT, hk, _, _ = norm_mod(B, xt[:, :, 0:n], xk, n, l, j, 0)
        for which, dst, sc in ((0, QT, 0.125), (1, KT, 1.0)):
            for m in range(8):
                ps, psk = B.bank(2 + (m % 4))
                for k in range(8):
                    B.mm(ps[:, 0:n], w[:, k, which * 1024 + m * 128:which * 1024 + (m + 1) * 128], hT[:, k, 0:n], k == 0, k == 7, [wk, hk], [psk])
                o, ok_ = B.tmp("na_qk", [128, 512], BF16, 3)
                if m % 2:
                    B.act(o[:, 0:n], ps[:, 0:n], AF.Copy, [psk], [ok_], scale=sc)
                else:
                    B.ts(o[:, 0:n], ps[:, 0:n], sc, None, ALU.mult, None, [psk], [ok_])
                p.dma(dst[m * 128:(m + 1) * 128, c0:c0 + n], o[:, 0:n], reads=[ok_])
                if which == 1 and "EK_o" in dr:
                    if c0 == NCTX:
                        p.dma(dr["EK_o"][m * 128:(m + 1) * 128, 0:256], o[:, 0:256], reads=[ok_])
                    if c0 == cfg.TOKX - 512:
                        p.dma(dr["EK_o"][m * 128:(m + 1) * 128, 256:512], o[:, 256:512], reads=[ok_])
        for jt in range(n // 128):
            for hf in range(2):
                ps, psk = B.bank(6 + hf)
                for k in range(8):
                    B.mm(ps[:, 0:512], hT[:, k, jt * 128:(jt + 1) * 128], w[:, k, 2048 + hf * 512:2048 + (hf + 1) * 512], k == 0, k == 7, [hk, wk], [psk])
                v, vk = B.tmp("na_v", [128, 512], BF16, 3)
                B.cp(v[:], ps[:, 0:512], [psk], [vk], eng="act" if hf else "dve")
                p.dma(V[c0 + jt * 128:c0 + (jt + 1) * 128, hf * 512:(hf + 1) * 512], v[:], reads=[vk])
                if "EV_o" in dr:
                    if c0 == NCTX and jt < 2:
                        p.dma(dr["EV_o"][jt * 128:(jt + 1) * 128, hf * 512:(hf + 1) * 512], v[:], reads=[vk])
                    if c0 == cfg.TOKX - 512 and jt >= 2:
                        p.dma(dr["EV_o"][jt * 128:(jt + 1) * 128, hf * 512:(hf + 1) * 512], v[:], reads=[vk])


def na_pair_tiles(cfg, i):
    P = cfg.NP
    if i == 0:
        return list(range(0, 6)), 5
    if i == 1:
        return list(range(1, 6)), 11
    if i == P - 2:
        return list(range(P - 2, P + 3)), 16
    if i == P - 1:
        return list(range(P - 2, P + 4)), 21
    return list(range(i, i + 5)), 0


def phase_att_odd(B, need_ctx):
    cfg, p, dr = B.cfg, B.p, B.dram
    OT = dr["OT"]
    NT = cfg.NKCT
    vts = [B.alloc("nvt%d" % i, [128, NT, 80], BF16) for i in range(2)]
    for i in range(2):
        p.op("pool", lambda e, i=i: e.memset(vts[i][:, :, 64:80], 1.0), writes=["nvt%d" % i])
    for h in range(16):
        kt, ktk = B.tmp("nkt", [64, cfg.NKC], BF16, 2)
        qt, qtk = B.tmp("nqt", [64, cfg.TOKX], BF16, 2)
        bt, btk = B.tmp("nbt", [128, 27, 128], BF16, 2)
        vt, vtk = vts[h % 2], "nvt%d" % (h % 2)
        p.dma(kt[:], dr["KT_C_b"][h * 64:(h + 1) * 64, :], writes=[ktk])
        p.dma(qt[:], dr["QT_C_i"][h * 64:(h + 1) * 64, :], writes=[qtk])
        p.dma(bt[:], dr["btab"][h, :, :, :], writes=[btk], q="pool")
        load_v(B, vt, vtk, dr["V_C_b"].rearrange("(t p) (h d) -> p t h d", p=128, d=64)[:, :, h, :], NT)
        if need_ctx:
            attend_group(B, kt, ktk, 64, vt, vtk, qt, qtk, 0, NCTX, [(0, None), (1, None)], OT[h, :, 0:NCTX])
        for i in range(cfg.NP):
            us, slot = na_pair_tiles(cfg, i)
            tiles = [(0, None), (1, None)] + [(2 + u, bt[:, slot + ui, :]) for ui, u in enumerate(us)]
            attend_group(B, kt, ktk, 64, vt, vtk, qt, qtk, NCTX + i * 128, 128, tiles,
                         OT[h, :, NCTX + i * 128:NCTX + (i + 1) * 128], bias_key=btk)


def phase_outproj(B, l, xT_in, xT_out, last):
    cfg, p, dr = B.cfg, B.p, B.dram
    OT = dr["OT"]
    wo, wok = B.tmp("wout", [64, 16, 1024], BF16, 1)
    for h in range(0, 16, 4):
        p.dma(wo[:, h:h + 4, :], dr["wout_p"][:, h:h + 4, :], writes=[wok], q="pool")
    for (c0, n) in cfg.chunks(not last):
        j = 1 if c0 == 0 else 0
        xt, xk = B.tmp("xT_o", [128, 8, 512], F32, 2)
        p.dma(xt[:, :, 0:n], xT_in.rearrange("(k p) t -> p k t", p=128)[:, :, c0:c0 + n], writes=[xk])
        ot, otk = B.tmp("ot_sb", [64, 16, 512], BF16, 2)
        p.dma(ot[:, :, 0:n], OT.rearrange("h d t -> d h t")[:, :, c0:c0 + n], writes=[otk])
        for f in range(8):
            ps, psk = B.bank(f % 4)
            for hh in range(16):
                B.mm(ps[:, 0:n], wo[:, hh, f * 128:(f + 1) * 128], ot[:, hh, 0:n], hh == 0, hh == 15, [wok, otk], [psk])
            B.stt(xt[:, f, 0:n], ps[:, 0:n], B.mv[:, l, j, 2, f:f + 1], xt[:, f, 0:n], ALU.mult, ALU.add, [psk, "mv", xk], [xk])
        p.dma(xT_out.rearrange("(k p) t -> p k t", p=128)[:, :, c0:c0 + n], xt[:, :, 0:n], reads=[xk])
    if last:
        xt, xk = B.tmp("xT_o", [128, 8, 512], F32, 2)
        p.dma(xt[:, :, 0:NCTX], xT_in.rearrange("(k p) t -> p k t", p=128)[:, :, 0:NCTX], writes=[xk])
        p.dma(xT_out.rearrange("(k p) t -> p k t", p=128)[:, :, 0:NCTX], xt[:, :, 0:NCTX], reads=[xk])


def phase_ffn(B, l, xT, last):
    cfg, p, dr = B.cfg, B.p, B.dram
    wr, wrk = B.tmp("wr", [128, 8, 36], F32, 1)
    p.dma(wr[:], dr["wr_p"][:, :, :], writes=[wrk])
    brt, brk = B.tmp("br", [128, 36], F32, 1)
    p.dma(brt[:], dr["br_p"][0:1, :].partition_broadcast(128), writes=[brk])
    sel, selk = B.tmp("sel", [32, 32, 128], BF16, 1)
    for e_ in range(32):
        B.cp(sel[:, e_, :], B.ident_b[0:32, e_:e_ + 1].to_broadcast([32, 128]), ["ident_b"], [selk], eng="pool" if e_ % 2 else "dve")
    lat_chunks = cfg.chunks(False)
    passes = [lat_chunks[i:i + 2] for i in range(0, len(lat_chunks), 2)]
    if not last:
        passes[0] = [(0, NCTX)] + passes[0]
    wg_d, wu_d, wd_d = dr["wg_p"], dr["wu_p"], dr["wd_p"]
    MAXN = 1280
    xv = xT.rearrange("(k p) t -> p k t", p=128)
    for subs in passes:
        xs, xsk0 = B.tmp("xs", [128, 8, MAXN], F32, 1)
        h2, h2k0 = B.tmp("h2", [128, 8, MAXN], BF16, 1)
        gT, gTk0 = B.tmp("gT", [32, MAXN], BF16, 1)
        offs, o_ = [], 0
        for (c0, n) in subs:
            offs.append(o_)
            o_ += n
        for si, (c0, n) in enumerate(subs):
            j = 1 if c0 == 0 else 0
            so = offs[si]
            xsk, h2k, gTk = "%s_%d" % (xsk0, si), "%s_%d" % (h2k0, si), "%s_%d" % (gTk0, si)
            p.dma(xs[:, :, so:so + n], xv[:, :, c0:c0 + n], writes=[xsk])
            sq, sqk = B.tmp("f_sq", [128, 8, 512], F32, 1)
            B.act(sq[:, :, 0:n], xs[:, :, so:so + n], AF.Square, [xsk], [sqk])
            ps, psk = B.bank(0)
            for k in range(8):
                B.mm(ps[:, 0:n], B.ones_f[:], sq[:, k, 0:n], k == 0, k == 7, ["ones_f", sqk], [psk])
            rs, rsk = B.rstd_from_sum(ps[:, 0:n], psk, float(D), n, "f_rstd")
            hf, hfk = sq, sqk
            for k in range(8):
                t, tk = B.tmp("f_t", [128, 512], F32, 2)
                B.stt(t[:, 0:n], xs[:, k, so:so + n], B.mv[:, l, j, 3, k:k + 1], rs[:, 0:n], ALU.mult, ALU.mult, [xsk, "mv", rsk], [tk])
                B.act(hf[:, k, 0:n], t[:, 0:n], AF.Identity, [tk, "mv", psk], [hfk], bias=B.mv[:, l, j, 4, k:k + 1])
                B.cp(h2[:, k, so:so + n], hf[:, k, 0:n], [hfk], [h2k], eng="pool" if k % 2 else "dve")
            for jt in range(n // 128):
                psl, pslk = B.bank(4 + (jt % 2))
                for k in range(8):
                    B.mm(psl[:, 0:36], hf[:, k, jt * 128:(jt + 1) * 128], wr[:, k, :], k == 0, k == 7, [hfk, wrk], [pslk])
                lg, lgk = B.tmp("r_lg", [128, 36], F32, 2)
                B.tt(lg[:], psl[:, 0:36], brt[:], ALU.add, [pslk, brk], [lgk])
                sc_, sck = B.tmp("r_sc", [128, 8], F32, 2)
                R = [lgk, sck]
                p.op("dve", lambda e, lg=lg, sc_=sc_: e.reduce_max(out=sc_[:, 0:1], in_=lg[:, 0:4], axis=AX.X), R, [sck])
                B.ts(sc_[:, 1:2], sc_[:, 0:1], -1.0, None, ALU.mult, None, [sck], [sck])
                eg, egk = B.tmp("r_eg", [128, 4], F32, 2)
                B.act(eg[:], lg[:, 0:4], AF.Exp, [lgk, sck], [egk, sck], bias=sc_[:, 1:2], accum_out=sc_[:, 2:3])
                goh, gohk = B.tmp("r_goh", [128, 4], F32, 2)
                B.ts(goh[:], lg[:, 0:4], sc_[:, 0:1], None, ALU.is_equal, None, [lgk, sck], [gohk])
                B.ts(goh[:], goh[:], -1.0, 1.0e4, ALU.add, ALU.mult, [gohk], [gohk])
                lem, lemk = B.tmp("r_lem", [128, 4, 8], F32, 2)
                B.tt(lem[:], lg[:, 4:36].rearrange("p (g e) -> p g e", e=8), goh[:].unsqueeze(2).to_broadcast([128, 4, 8]), ALU.add,
                     [lgk, gohk], [lemk])
                lem2 = lem[:].rearrange("p g e -> p (g e)")
                p.op("dve", lambda e, lem2=lem2, sc_=sc_: e.reduce_max(out=sc_[:, 3:4], in_=lem2, axis=AX.X), [lemk, sck], [sck])
                B.ts(sc_[:, 4:5], sc_[:, 3:4], -1.0, None, ALU.mult, None, [sck], [sck])
                ee, eek = B.tmp("r_ee", [128, 32], F32, 2)
                B.act(ee[:], lem2, AF.Exp, [lemk, sck], [eek], bias=sc_[:, 4:5])
                oh1, oh1k = B.tmp("r_oh1", [128, 32], F32, 2)
                B.ts(oh1[:], lem2, sc_[:, 3:4], None, ALU.is_equal, None, [lemk, sck], [oh1k])
                e2, e2k = B.tmp("r_e2", [128, 32], F32, 2)
                B.stt(e2[:], oh1[:], -2.0, ee[:], ALU.mult, ALU.add, [oh1k, eek], [e2k])
                p.op("dve", lambda e, e2=e2, sc_=sc_: e.reduce_max(out=sc_[:, 5:6], in_=e2[:], axis=AX.X), [e2k, sck], [sck])
                oh2, oh2k = B.tmp("r_oh2", [128, 32], F32, 2)
                B.ts(oh2[:], e2[:], sc_[:, 5:6], None, ALU.is_equal, None, [e2k, sck], [oh2k])
                B.tt(oh1[:], oh1[:], oh2[:], ALU.add, [oh1k, oh2k], [oh1k])
                B.ts(sc_[:, 6:7], sc_[:, 5:6], 1.0, None, ALU.add, None, [sck], [sck])
                B.tt(sc_[:, 6:7], sc_[:, 6:7], sc_[:, 2:3], ALU.mult, [sck], [sck])
                B.recip(sc_[:, 7:8], sc_[:, 6:7], [sck], [sck])
                G, Gk = B.tmp("r_G", [128, 32], F32, 2)
                B.stt(G[:], ee[:], sc_[:, 7:8], oh1[:], ALU.mult, ALU.mult, [eek, sck, oh1k], [Gk])
                if "G_dbg" in dr:
                    p.dma(dr["G_dbg"][c0 + jt * 128:c0 + (jt + 1) * 128, :], G[:], reads=[Gk])
                pst, pstk = B.bank(6)
                p.op("pe", lambda e, pst=pst, G=G: e.transpose(pst[0:32, 0:128], G[:], B.ident_f[:]), [Gk, "ident_f"], [pstk])
                B.cp(gT[:, so + jt * 128:so + (jt + 1) * 128], pst[0:32, 0:128], [pstk], [gTk], eng="act")
        def load_w(e_):
            wg, wgk = B.tmp("wg", [128, 8, 512], BF16, 3)
            wu, wuk = B.tmp("wu", [128, 8, 512], BF16, 3)
            wd, wdk = B.tmp("wd", [128, 4, 1024], BF16, 3)
            p.dma(wg[:], wg_d[e_, :, :, :], writes=[wgk], q="pool")
            p.dma(wu[:], wu_d[e_, :, :, :], writes=[wuk], q="pool")
            p.dma(wd[:], wd_d[e_, :, :, :], writes=[wdk], q="pool")
            return wg, wgk, wu, wuk, wd, wdk
        nxt_w = load_w(0)
        pend_down = None
        for e_ in range(32):
            wg, wgk, wu, wuk, wd, wdk = nxt_w
            if e_ + 1 < 32:
                nxt_w = load_w(e_ + 1)
            for si, (c0, n) in enumerate(subs):
                j = 1 if c0 == 0 else 0
                so = offs[si]
                xsk, h2k, gTk = "%s_%d" % (xsk0, si), "%s_%d" % (h2k0, si), "%s_%d" % (gTk0, si)
                pg_, pgk = B.bank(7)
                B.mm(pg_[:, 0:n], sel[:, e_, :], gT[:, so:so + n], True, True, [selk, gTk], [pgk])
                gb, gbk = B.tmp("gb", [128, 512], F32, 2)
                B.cp(gb[:, 0:n], pg_[:, 0:n], [pgk], [gbk], eng="act")
                aT, aTk = B.tmp("aT", [128, 4, 512], BF16, 3)
                for m in range(4):
                    pa, pak = B.bank(0 + (m % 2) * 2)
                    pu, puk = B.bank(1 + (m % 2) * 2)
                    for k in range(8):
                        B.mm(pa[:, 0:n], wg[:, k, m * 128:(m + 1) * 128], h2[:, k, so:so + n], k == 0, k == 7, [wgk, h2k], [pak])
                    for k in range(8):
                        B.mm(pu[:, 0:n], wu[:, k, m * 128:(m + 1) * 128], h2[:, k, so:so + n], k == 0, k == 7, [wuk, h2k], [puk])
                    sg, sgk = B.tmp("sg", [128, 512], F32, 3)
                    B.act(sg[:, 0:n], pa[:, 0:n], AF.Silu, [pak], [sgk])
                    B.tt(sg[:, 0:n], sg[:, 0:n], pu[:, 0:n], ALU.mult, [sgk, puk], [sgk])
                    B.tt(aT[:, m, 0:n], sg[:, 0:n], gb[:, 0:n], ALU.mult, [sgk, gbk], [aTk], eng="pool")

                def down(wd=wd, wdk=wdk, aT=aT, aTk=aTk, so=so, n=n, j=j, xsk=xsk):
                    for f in range(8):
                        pd, pdk = B.bank(4 + (f % 3))
                        for k in range(4):
                            B.mm(pd[:, 0:n], wd[:, k, f * 128:(f + 1) * 128], aT[:, k, 0:n], k == 0, k == 3, [wdk, aTk], [pdk])
                        B.stt(xs[:, f, so:so + n], pd[:, 0:n], B.mv[:, l, j, 5, f:f + 1], xs[:, f, so:so + n], ALU.mult, ALU.add,
                              [pdk, "mv", xsk], [xsk])
                if pend_down is not None:
                    pend_down()
                pend_down = down
        pend_down()
        for si, (c0, n) in enumerate(subs):
            so = offs[si]
            p.dma(xv[:, :, c0:c0 + n], xs[:, :, so:so + n], reads=["%s_%d" % (xsk0, si)])


def phase_final(B, xT_dram, out_dram):
    cfg, p, dr = B.cfg, B.p, B.dram
    nfin, nfk = B.tmp("nfin", [128, 8], F32, 1)
    p.dma(nfin[:], dr["nfin_p"][:, :], writes=[nfk])
    for (c0, n) in cfg.chunks(False):
        xt, xk = B.tmp("xT_a", [128, 8, 512], F32, 2)
        p.dma(xt[:, :, 0:n], xT_dram.rearrange("(k p) t -> p k t", p=128)[:, :, c0:c0 + n], writes=[xk])
        sq, sqk = B.tmp("fin_sq", [128, 8, 512], F32, 1)
        B.act(sq[:, :, 0:n], xt[:, :, 0:n], AF.Square, [xk], [sqk])
        ps, psk = B.bank(0)
        for k in range(8):
            B.mm(ps[:, 0:n], B.ones_f[:], sq[:, k, 0:n], k == 0, k == 7, ["ones_f", sqk], [psk])
        rs, rsk = B.rstd_from_sum(ps[:, 0:n], psk, float(D), n, "fin_rstd")
        o, ok_ = B.tmp("fin_o", [128, 8, 512], F32, 2)
        for k in range(8):
            B.stt(o[:, k, 0:n], xt[:, k, 0:n], nfin[:, k:k + 1], rs[:, 0:n], ALU.mult, ALU.mult, [xk, nfk, rsk], [ok_])
        p.dma(out_dram.rearrange("(k p) t -> p k t", p=128)[:, :, c0 - NCTX:c0 - NCTX + n], o[:, :, 0:n], reads=[ok_])


def declare_common(B, nl):
    B.din("c_in", [128, 8, 2], F32)
    B.din("nmix_p", [128, 4, 8], F32)
    B.din("nffn_p", [128, 4, 8], F32)
    B.din("bada_p", [128, 4, 48], F32)
    B.din("wada_p", [nl, 128, 8, 6144], F32)


def declare_A_even(B):
    c = B.cfg
    B.din("win_p", [128, 8, WIN_COLS], F32)
    B.din("wq_p", [128, 3, 1024], F32)
    B.din("wkv_p", [128, 2, 1024], F32)
    B.din("gains_e", [128, 12], F32)
    B.din("tabA", [128, 4, c.TOKX], F32)
    B.din("tabB", [128, 4, c.TOKX], F32)
    B.dout("QT_A_o", [8, 96, c.TOKX], BF16)
    B.dout("QT_B_o", [8, 64, c.TOKX], BF16)
    B.dout("KT_A_o", [8, 96, c.TOKX], BF16)
    B.dout("KT_B_o", [2, 64, c.TOKX], BF16)
    B.dout("V_A_o", [c.TOKX, 512], BF16)
    B.dout("V_B_o", [c.TOKX, 128], BF16)


def declare_A_odd(B):
    c = B.cfg
    B.din("wna_p", [128, 8, 3072], F32)
    B.dout("QT_C_o", [1024, c.TOKX], BF16)
    B.dout("KT_C_o", [1024, c.TOKX], BF16)
    B.dout("V_C_o", [c.TOKX, 1024], BF16)


def declare_att_even(B):
    c = B.cfg
    B.din("QT_A_i", [8, 96, c.TOKX], BF16)
    B.din("QT_B_i", [8, 64, c.TOKX], BF16)
    B.din("KT_A_f", [8, 96, c.NKEY], BF16)
    B.din("KT_B_f", [2, 64, c.NKEY], BF16)
    B.din("V_A_f", [c.NKEY, 512], BF16)
    B.din("V_B_f", [c.NKEY, 128], BF16)


def declare_att_odd(B):
    c = B.cfg
    B.din("QT_C_i", [1024, c.TOKX], BF16)
    B.din("KT_C_b", [1024, c.NKC], BF16)
    B.din("V_C_b", [c.NKC, 1024], BF16)
    B.din("btab", [16, 128, 27, 128], F32)


def declare_mix_ffn(B, dbg=False):
    c = B.cfg
    B.din("wout_p", [64, 16, 1024], F32)
    B.din("wr_p", [128, 8, 36], F32)
    B.din("br_p", [1, 36], F32)
    B.din("wg_p", [32, 128, 8, 512], F32)
    B.din("wu_p", [32, 128, 8, 512], F32)
    B.din("wd_p", [32, 128, 4, 1024], F32)
    B.dint("OT", [16, 64, c.TOKX], BF16)
    if dbg:
        B.dout("G_dbg", [c.TOKX, 32], F32)


def build_stage(cfg, stage, dbg=False, skip_moe=False):
    B = Bld(cfg)
    xT = B.din("xT", [1024, cfg.TOKX], F32)
    if stage == 0:
        declare_common(B, 1)
        declare_A_even(B)
        phase_modvec(B, [0]); B.phase_reset()
        phase_A_even(B, 0, xT); B.phase_reset()
    else:
        l = stage - 1
        last = stage == 4
        layers = [l] if last else [l, l + 1]
        declare_common(B, len(layers))
        if l % 2 == 0:
            declare_att_even(B)
        else:
            declare_att_odd(B)
        declare_mix_ffn(B, dbg)
        xo = B.dout("xT_o", [1024, cfg.TOKX], F32)
        if last:
            B.din("nfin_p", [128, 8], F32)
            outT = B.dout("outT", [1024, cfg.TOK], F32)
        elif (l + 1) % 2 == 0:
            declare_A_even(B)
        else:
            declare_A_odd(B)
        phase_modvec(B, layers); B.phase_reset()
        if l % 2 == 0:
            phase_att_even(B, not last)
        else:
            phase_att_odd(B, not last)
        B.phase_reset()
        phase_outproj(B, l, xT, xo, last); B.phase_reset()
        if not skip_moe:
            phase_ffn(B, l, xo, last); B.phase_reset()
        if last:
            phase_final(B, xo, outT)
        elif (l + 1) % 2 == 0:
            phase_A_even(B, l + 1, xo)
        else:
            phase_A_odd(B, l + 1, xo)
        B.phase_reset()
    B.p.emit()
    return B


def _ktile(w):
    K, N = w.shape
    return np.ascontiguousarray(w.reshape(K // 128, 128, N).transpose(1, 0, 2))


def prep_even(inp, i):
    w_in = inp["ab_w_in"][i]
    ev32, od32 = np.arange(0, 32, 2), np.arange(1, 32, 2)
    ev64, od64 = np.arange(0, 64, 2), np.arange(1, 64, 2)
    cols = list(range(0, 640))
    cols += list(640 + ev32) + list(640 + od32)
    cols += list(640 + od32) + list(640 + ev32)
    for h in range(8):
        cols += list(672 + h * 64 + ev64) + list(672 + h * 64 + od64)
    for h in range(8):
        cols += list(672 + h * 64 + od64) + list(672 + h * 64 + ev64)
    for h in range(2):
        cols += list(1184 + h * 64 + ev64) + list(1184 + h * 64 + od64)
    for h in range(2):
        cols += list(1184 + h * 64 + od64) + list(1184 + h * 64 + ev64)
    cols += list(range(1312, 1440))
    assert len(cols) == WIN_COLS
    wq = inp["mla_w_q_up"][i]
    qc = []
    for h in range(8):
        qc += list(h * 96 + np.arange(64)) + list(h * 96 + 64 + ev32) + list(h * 96 + 64 + od32)
    for h in range(8):
        qc += list(h * 96 + 64 + od32) + list(h * 96 + 64 + ev32)
    wkv = inp["mla_w_kv_up"][i]
    kc = []
    for h in range(8):
        kc += list(h * 128 + np.arange(64))
    for h in range(8):
        kc += list(h * 128 + 64 + np.arange(64))
    g = np.zeros((128, 12), np.float32)
    g[:, 0:3] = inp["mla_q_norm"][i].reshape(3, 128).T
    g[:, 3:5] = inp["mla_kv_norm"][i].reshape(2, 128).T
    gq, gk = inp["gqa_q_norm"][i], inp["gqa_k_norm"][i]
    g[:, 5] = np.tile(np.concatenate([gq[ev64], gq[od64]]), 2)
    g[:, 6] = np.tile(np.concatenate([gq[od64], gq[ev64]]), 2)
    g[:, 7] = np.tile(np.concatenate([gk[ev64], gk[od64]]), 2)
    g[:, 8] = np.tile(np.concatenate([gk[od64], gk[ev64]]), 2)
    return {"win_p": _ktile(w_in[:, cols]), "wq_p": _ktile(wq[:, qc]), "wkv_p": _ktile(wkv[:, kc]), "gains_e": g}


def rope_tables(cfg, c4):
    t = c4 * cfg.TOK + np.arange(cfg.TOK)
    r = (t // 64).astype(np.float32)
    col = (t % 64).astype(np.float32)

    def ang(rot):
        n = rot // 4
        inv = (10000.0 ** (-np.arange(n, dtype=np.float32) / n)).astype(np.float32)
        return np.concatenate([r[:, None] * inv, col[:, None] * inv], -1).astype(np.float32).T
    aA, aB = ang(32), ang(64)
    tabA = np.zeros((128, 4, cfg.TOKX), np.float32)
    tabB = np.zeros((128, 4, cfg.TOKX), np.float32)
    for tab, a, base, reps, qs in ((tabA, aA, 64, 1, 96.0 ** -0.5), (tabB, aB, 0, 2, 64.0 ** -0.5)):
        hp = a.shape[0]
        c, s = np.cos(a), np.sin(a)
        for rep in range(reps):
            o = base + rep * 2 * hp
            for blk, sgn in ((0, -1.0), (1, 1.0)):
                rows = slice(o + blk * hp, o + (blk + 1) * hp)
                tab[rows, 0, NCTX:] = c * qs
                tab[rows, 1, NCTX:] = sgn * s * qs
                tab[rows, 2, NCTX:] = c
                tab[rows, 3, NCTX:] = sgn * s
                tab[rows, 0, :NCTX] = qs
                tab[rows, 2, :NCTX] = 1.0
    return tabA, tabB


def bias_tables(cfg, rpb, c4):
    P = cfg.NP
    R0 = c4 * cfg.ROWS
    slots = [(2, u) for u in range(2, 7)] + [(0, u) for u in range(0, 6)] + [(1, u) for u in range(1, 6)] \
        + [(P - 2, u) for u in range(P - 2, P + 3)] + [(P - 1, u) for u in range(P - 2, P + 4)]
    kp = np.arange(128)
    q = np.arange(128)
    out = np.full((16, 128, 27, 128), NEG, np.float32)
    for si, (i, u) in enumerate(slots):
        kr = (R0 - 4 + 2 * u + kp // 64)[:, None]
        kc = (kp % 64)[:, None]
        r = (R0 + 2 * i + q // 64)[None, :]
        col = (q % 64)[None, :]
        rs = np.clip(r - 4, 0, cfg.SROWS - 8)
        cs = np.clip(col - 8, 0, 48)
        valid = (kr >= rs) & (kr < rs + 8) & (kc >= cs) & (kc < cs + 16)
        dr_ = np.clip(kr - r + 7, 0, 14)
        dc_ = np.clip(kc - col + 15, 0, 30)
        vals = rpb[:, dr_, dc_]
        out[:, :, si, :] = np.where(valid[None], vals, NEG)
    return out


def prep_moe(inp, l):
    return {
        "wr_p": _ktile(np.concatenate([inp["moe_w_rg"][l], inp["moe_w_re"][l]], 1)),
        "br_p": np.concatenate([inp["moe_b_rg"][l], inp["moe_b_re"][l]])[None, :].astype(np.float32),
        "wg_p": np.ascontiguousarray(inp["moe_w_gate"][l].reshape(32, 8, 128, 512).transpose(0, 2, 1, 3)),
        "wu_p": np.ascontiguousarray(inp["moe_w_up"][l].reshape(32, 8, 128, 512).transpose(0, 2, 1, 3)),
        "wd_p": np.ascontiguousarray(inp["moe_w_down"][l].reshape(32, 4, 128, 1024).transpose(0, 2, 1, 3)),
    }


def prep_cin(inp, b):
    return np.ascontiguousarray(np.stack([inp["c"][b].reshape(8, 128).T, inp["c_ctx"].reshape(8, 128).T], -1).astype(np.float32))


def prep_common(inp, layers):
    return {
        "nmix_p": np.ascontiguousarray(inp["norm_mix"].reshape(4, 8, 128).transpose(2, 0, 1)),
        "nffn_p": np.ascontiguousarray(inp["norm_ffn"].reshape(4, 8, 128).transpose(2, 0, 1)),
        "bada_p": np.ascontiguousarray(inp["b_ada"].reshape(4, 48, 128).transpose(2, 0, 1)),
        "wada_p": np.ascontiguousarray(np.stack([inp["w_ada"][l].reshape(8, 128, 6144).transpose(1, 0, 2) for l in layers])),
    }


def wout_p(w):
    return np.ascontiguousarray(w.reshape(16, 64, 1024).transpose(1, 0, 2))


N_CORES = 8
_PROG_CACHE = {}


def get_stage(cfg, stage, **kw):
    key = (cfg.SEQ, stage, tuple(sorted(kw.items())))
    if key not in _PROG_CACHE:
        _PROG_CACHE[key] = build_stage(cfg, stage, **kw)
    return _PROG_CACHE[key]


def run_stage(cfg, stage, in_maps, **kw):
    B = get_stage(cfg, stage, **kw)
    names = [n for n in B.dram]
    res = run_bass_kernel_spmd(B.nc, in_maps, core_ids=list(range(N_CORES)))
    return res.results


def exchange_even(cfg, outs):
    ins = []
    for b in range(2):
        cs = outs[b * CPB:(b + 1) * CPB]
        kta = np.concatenate([cs[0]["KT_A_o"][:, :, :NCTX]] + [c["KT_A_o"][:, :, NCTX:] for c in cs], 2)
        ktb = np.concatenate([cs[0]["KT_B_o"][:, :, :NCTX]] + [c["KT_B_o"][:, :, NCTX:] for c in cs], 2)
        va = np.concatenate([cs[0]["V_A_o"][:NCTX]] + [c["V_A_o"][NCTX:] for c in cs], 0)
        vb = np.concatenate([cs[0]["V_B_o"][:NCTX]] + [c["V_B_o"][NCTX:] for c in cs], 0)
        for c in cs:
            ins.append({"QT_A_i": c["QT_A_o"], "QT_B_i": c["QT_B_o"], "KT_A_f": kta, "KT_B_f": ktb, "V_A_f": va, "V_B_f": vb})
    return ins


def exchange_odd(cfg, outs):
    ins = []
    pad = 4 * 64
    for b in range(2):
        cs = outs[b * CPB:(b + 1) * CPB]
        kt = np.concatenate([c["KT_C_o"][:, NCTX:] for c in cs], 1)
        v = np.concatenate([c["V_C_o"][NCTX:] for c in cs], 0)
        ktp = np.concatenate([np.zeros((1024, pad), kt.dtype), kt, np.zeros((1024, pad), kt.dtype)], 1)
        vp = np.concatenate([np.zeros((pad, 1024), v.dtype), v, np.zeros((pad, 1024), v.dtype)], 0)
        for ci, c in enumerate(cs):
            a = ci * cfg.TOK
            n = (cfg.ROWS + 8) * 64
            ins.append({"QT_C_i": c["QT_C_o"],
                        "KT_C_b": np.ascontiguousarray(np.concatenate([c["KT_C_o"][:, :NCTX], ktp[:, a:a + n]], 1)),
                        "V_C_b": np.ascontiguousarray(np.concatenate([c["V_C_o"][:NCTX], vp[a:a + n]], 0))})
    return ins


def forward(inp, SEQ, upto=4, dbg=False, skip_moe=False, trace=None):
    cfg = Cfg(SEQ)
    inp = {k: np.asarray(v, np.float32) for k, v in inp.items()}
    xT = []
    for core in range(N_CORES):
        b, c4 = divmod(core, CPB)
        xl = inp["x"][b, c4 * cfg.TOK:(c4 + 1) * cfg.TOK]
        xT.append(np.ascontiguousarray(np.concatenate([inp["ctx"][b], xl], 0).T))
    tabs = [rope_tables(cfg, c % CPB) for c in range(N_CORES)]
    even = {i: prep_even(inp, i) for i in range(2)}
    extra = [None] * N_CORES
    results = None
    for stage in range(upto + 1):
        l = stage - 1
        last = stage == 4
        layers = [0] if stage == 0 else ([l] if last else [l, l + 1])
        maps = []
        shared = {}
        if stage >= 1:
            shared.update(prep_moe(inp, l))
            shared["wout_p"] = wout_p(inp["ab_w_out"][l // 2] if l % 2 == 0 else inp["na_w_out"][l // 2])
        nxt = stage if stage < 4 else None
        if nxt is not None:
            if nxt % 2 == 0:
                shared.update(even[nxt // 2])
            else:
                shared["wna_p"] = _ktile(inp["na_w_in"][nxt // 2])
        if last:
            shared["nfin_p"] = np.ascontiguousarray(inp["norm_final"].reshape(8, 128).T)
        shared.update(prep_common(inp, layers))
        cins = [prep_cin(inp, b) for b in range(2)]
        for core in range(N_CORES):
            b, c4 = divmod(core, CPB)
            m = dict(shared)
            m["c_in"] = cins[b]
            m["xT"] = xT[core]
            if nxt is not None and nxt % 2 == 0:
                m["tabA"], m["tabB"] = tabs[core]
            if stage >= 1:
                m.update(extra[core])
                if l % 2 == 1:
                    m["btab"] = bias_tables(cfg, inp["na_rpb"][l // 2], c4)
            maps.append(m)
        results = run_stage(cfg, stage, maps, **({"dbg": dbg, "skip_moe": skip_moe} if stage >= 1 else {}))
        if trace is not None:
            trace.append(results)
        if stage >= 1:
            xT = [r["xT_o"] for r in results]
        if nxt is not None:
            extra = exchange_even(cfg, results) if nxt % 2 == 0 else exchange_odd(cfg, results)
    if upto < 4:
        return results
    out = np.empty((2, SEQ, D), np.float32)
    for core in range(N_CORES):
        b, c4 = divmod(core, CPB)
        out[b, c4 * cfg.TOK:(c4 + 1) * cfg.TOK] = results[core]["outT"].T
    return out


GROUPS = [[0, 1, 2, 3], [4, 5, 6, 7]]


def na_slots(cfg):
    P = cfg.NP
    own = lambda us: [("own", u) for u in us]
    out = [(2, own(range(0, 5)))]
    out.append((0, [("edge", r, k) for r in range(CPB) for k in (2, 3)] + own(range(0, 4))))
    out.append((1, [("edge", r, 3) for r in range(CPB)] + own(range(0, 4))))
    out.append((P - 2, own(range(P - 4, P)) + [("edge", r, 0) for r in range(CPB)]))
    out.append((P - 1, own(range(P - 4, P)) + [("edge", r, k) for r in range(CPB) for k in (0, 1)]))
    return out


def na_tile_index(cfg, td):
    EB = cfg.TOKX // 128
    if td[0] == "own":
        return 2 + td[1]
    return EB + td[1] * 4 + td[2]


def bias_tables_fused(cfg, rpb, c4):
    R0 = c4 * cfg.ROWS
    erow = {0: 0, 1: 2, 2: cfg.ROWS - 4, 3: cfg.ROWS - 2}
    kp = np.arange(128)
    q = np.arange(128)
    slots = na_slots(cfg)
    nslot = sum(len(t) for _, t in slots)
    out = np.full((16, 128, nslot, 128), NEG, np.float32)
    si = 0
    for (i, tds) in slots:
        for td in tds:
            if td[0] == "own":
                krow0 = R0 + 2 * td[1]
                ok = True
            else:
                krow0 = td[1] * cfg.ROWS + erow[td[2]]
                ok = td[1] != c4
            kr = (krow0 + kp // 64)[:, None]
            kc = (kp % 64)[:, None]
            r = (R0 + 2 * i + q // 64)[None, :]
            col = (q % 64)[None, :]
            rs = np.clip(r - 4, 0, cfg.SROWS - 8)
            cs = np.clip(col - 8, 0, 48)
            valid = (kr >= rs) & (kr < rs + 8) & (kc >= cs) & (kc < cs + 16) & ok
            vals = rpb[:, np.clip(kr - r + 7, 0, 14), np.clip(kc - col + 15, 0, 30)]
            out[:, :, si, :] = np.where(valid[None], vals, NEG)
            si += 1
    return out


def phase_att_odd_fused(B, need_ctx):
    cfg, p, dr = B.cfg, B.p, B.dram
    OT = dr["OT"]
    TOKX = cfg.TOKX
    EB = TOKX // 128
    NT = EB + 4 * CPB
    NKF = TOKX + 512 * CPB
    slots = na_slots(cfg)
    nslot = sum(len(t) for _, t in slots)
    soff, o_ = {}, 0
    for (i, tds) in slots:
        soff[i] = o_
        o_ += len(tds)
    vts = [B.alloc("nvt%d" % i, [128, NT, 80], BF16) for i in range(2)]
    for i in range(2):
        p.op("pool", lambda e, i=i: e.memset(vts[i][:, :, 64:80], 1.0), writes=["nvt%d" % i])
    vown = dr["V_C_o"].rearrange("(t p) (h d) -> p t h d", p=128, d=64)
    vedge = dr["EV_g"].rearrange("(j r t p) (h d) -> j r p t h d", j=2, r=CPB, p=128, d=64)
    for h in range(16):
        kt, ktk = B.tmp("nkt", [64, NKF], BF16, 2)
        qt, qtk = B.tmp("nqt", [64, TOKX], BF16, 2)
        bt, btk = B.tmp("nbt", [128, nslot, 128], BF16, 2)
        vt, vtk = vts[h % 2], "nvt%d" % (h % 2)
        p.dma(kt[:, 0:TOKX], dr["KT_C_o"][h * 64:(h + 1) * 64, :], writes=[ktk])
        for r in range(CPB):
            eb = ((h // 8) * CPB + r) * 512 + (h % 8) * 64
            p.dma(kt[:, TOKX + r * 512:TOKX + (r + 1) * 512], dr["EK_g"][eb:eb + 64, :], writes=[ktk])
        p.dma(qt[:], dr["QT_C_o"][h * 64:(h + 1) * 64, :], writes=[qtk])
        p.dma(bt[:], dr["btab"][h, :, :, :], writes=[btk], q="pool")
        load_v(B, vt, vtk, vown[:, :, h, :], EB)
        for r in range(CPB):
            for j in range(2):
                p.dma(vt[:, EB + r * 4 + 2 * j:EB + r * 4 + 2 * j + 2, 0:64], vedge[j, r, :, :, h, :], writes=[vtk])
        if need_ctx:
            attend_group(B, kt, ktk, 64, vt, vtk, qt, qtk, 0, NCTX, [(0, None), (1, None)], OT[h, :, 0:NCTX])
        for i in range(cfg.NP):
            if i in soff and i != 2:
                tds, so = dict(slots)[i], soff[i]
            else:
                tds, so = [("own", u) for u in range(i - 2, i + 3)], 0
            tiles = [(0, None), (1, None)] + [(na_tile_index(cfg, td), bt[:, so + ti, :]) for ti, td in enumerate(tds)]
            attend_group(B, kt, ktk, 64, vt, vtk, qt, qtk, NCTX + i * 128, 128, tiles,
                         OT[h, :, NCTX + i * 128:NCTX + (i + 1) * 128], bias_key=btk)


def build_fused(cfg, nlayers=4):
    B = Bld(cfg)
    c = cfg
    xT = B.din("xT", [1024, c.TOKX], F32)
    declare_common(B, 4)
    B.din("tabA", [128, 4, c.TOKX], F32)
    B.din("tabB", [128, 4, c.TOKX], F32)
    B.din("nfin_p", [128, 8], F32)
    nslot = sum(len(t) for _, t in na_slots(cfg))
    for i in range(2):
        B.din("win_p%d" % i, [128, 8, WIN_COLS], F32)
        B.din("wq_p%d" % i, [128, 3, 1024], F32)
        B.din("wkv_p%d" % i, [128, 2, 1024], F32)
        B.din("gains_e%d" % i, [128, 12], F32)
        B.din("wna_p%d" % i, [128, 8, 3072], F32)
        B.din("btab%d" % i, [16, 128, nslot, 128], F32)
    for l in range(4):
        B.din("wout_p%d" % l, [64, 16, 1024], F32)
        B.din("wr_p%d" % l, [128, 8, 36], F32)
        B.din("br_p%d" % l, [1, 36], F32)
        B.din("wg_p%d" % l, [32, 128, 8, 512], F32)
        B.din("wu_p%d" % l, [32, 128, 8, 512], F32)
        B.din("wd_p%d" % l, [32, 128, 4, 1024], F32)
    outT = B.dout("outT", [1024, c.TOK], F32)
    xw = B.dint("xw", [1024, c.TOKX], F32)
    B.dint("OT", [16, 64, c.TOKX], BF16)
    for nm_, shp in (("QT_A_o", [8, 96, c.TOKX]), ("QT_B_o", [8, 64, c.TOKX]), ("KT_A_o", [8, 96, c.TOKX]), ("KT_B_o", [2, 64, c.TOKX]),
                     ("V_A_o", [8 * c.TOKX, 64]), ("V_B_o", [2 * c.TOKX, 64]), ("KT_A_g", [8 * CPB * 96, c.TOKX]), ("KT_B_g", [2 * CPB * 64, c.TOKX]),
                     ("V_A_g", [8 * CPB * c.TOKX, 64]), ("V_B_g", [2 * CPB * c.TOKX, 64]),
                     ("QT_C_o", [1024, c.TOKX]), ("KT_C_o", [1024, c.TOKX]), ("V_C_o", [c.TOKX, 1024]),
                     ("EK_o", [1024, 512]), ("EV_o", [512, 1024]), ("EK_g", [2 * CPB * 512, 512]), ("EV_g", [2 * CPB * 256, 1024])):
        B.dint(nm_, shp, BF16)
    B.dram["V_hm"] = True
    dr = B.dram
    dr["QT_A_i"], dr["QT_B_i"] = dr["QT_A_o"], dr["QT_B_o"]
    p = B.p
    phase_modvec(B, list(range(nlayers))); B.phase_reset()
    for l in range(nlayers):
        last = l == 3
        i = l // 2
        xin = xT if l == 0 else xw
        for k in ("wout_p", "wr_p", "br_p", "wg_p", "wu_p", "wd_p"):
            dr[k] = dr["%s%d" % (k, l)]
        if l % 2 == 0:
            for k in ("win_p", "wq_p", "wkv_p", "gains_e"):
                dr[k] = dr["%s%d" % (k, i)]
            phase_A_even(B, l, xin); B.phase_reset()
            T = c.TOKX
            ka2, kb2 = dr["KT_A_o"].rearrange("h d t -> (h d) t"), dr["KT_B_o"].rearrange("h d t -> (h d) t")
            for h in range(8):
                p.coll("AllGather", GROUPS, ka2[h * 96:(h + 1) * 96, :], dr["KT_A_g"][h * CPB * 96:(h + 1) * CPB * 96, :], writes=["cc_ka%d" % h])
                p.coll("AllGather", GROUPS, dr["V_A_o"][h * T:(h + 1) * T, :], dr["V_A_g"][h * CPB * T:(h + 1) * CPB * T, :], writes=["cc_va%d" % h])
            for h in range(2):
                p.coll("AllGather", GROUPS, kb2[h * 64:(h + 1) * 64, :], dr["KT_B_g"][h * CPB * 64:(h + 1) * CPB * 64, :], writes=["cc_kb%d" % h])
                p.coll("AllGather", GROUPS, dr["V_B_o"][h * T:(h + 1) * T, :], dr["V_B_g"][h * CPB * T:(h + 1) * CPB * T, :], writes=["cc_vb%d" % h])
            B.phase_reset()
            phase_att_even(B, not last, gathered=True); B.phase_reset()
        else:
            dr["wna_p"], dr["btab"] = dr["wna_p%d" % i], dr["btab%d" % i]
            phase_A_odd(B, l, xin); B.phase_reset()
            for j in range(2):
                p.coll("AllGather", GROUPS, dr["EK_o"][j * 512:(j + 1) * 512, :], dr["EK_g"][j * CPB * 512:(j + 1) * CPB * 512, :], writes=["cc_ek%d" % j])
                p.coll("AllGather", GROUPS, dr["EV_o"][j * 256:(j + 1) * 256, :], dr["EV_g"][j * CPB * 256:(j + 1) * CPB * 256, :], writes=["cc_ev%d" % j])
            B.phase_reset()
            phase_att_odd_fused(B, not last); B.phase_reset()
        phase_outproj(B, l, xin, xw, last); B.phase_reset()
        phase_ffn(B, l, xw, last); B.phase_reset()
    if nlayers == 4:
        phase_final(B, xw, outT)
    B.phase_reset()
    B.p.emit()
    return B


def fused_inputs(inp, cfg):
    inp = {k: np.asarray(v, np.float32) for k, v in inp.items()}
    shared = prep_common(inp, [0, 1, 2, 3])
    shared["nfin_p"] = np.ascontiguousarray(inp["norm_final"].reshape(8, 128).T)
    for i in range(2):
        for k, v in prep_even(inp, i).items():
            shared["%s%d" % (k, i)] = v
        shared["wna_p%d" % i] = _ktile(inp["na_w_in"][i])
    for l in range(4):
        for k, v in prep_moe(inp, l).items():
            shared["%s%d" % (k, l)] = v
        shared["wout_p%d" % l] = wout_p(inp["ab_w_out"][l // 2] if l % 2 == 0 else inp["na_w_out"][l // 2])
    cins = [prep_cin(inp, b) for b in range(2)]
    tabs = [rope_tables(cfg, c4) for c4 in range(CPB)]
    bts = [[bias_tables_fused(cfg, inp["na_rpb"][i], c4) for i in range(2)] for c4 in range(CPB)]
    maps = []
    for core in range(N_CORES):
        b, c4 = divmod(core, CPB)
        m = dict(shared)
        m["c_in"] = cins[b]
        xl = inp["x"][b, c4 * cfg.TOK:(c4 + 1) * cfg.TOK]
        m["xT"] = np.ascontiguousarray(np.concatenate([inp["ctx"][b], xl], 0).T)
        m["tabA"], m["tabB"] = tabs[c4]
        m["btab0"], m["btab1"] = bts[c4]
        maps.append(m)
    return maps


def forward_fused(inp, SEQ):
    cfg = Cfg(SEQ)
    key = ("fused", SEQ)
    if key not in _PROG_CACHE:
        _PROG_CACHE[key] = build_fused(cfg)
    B = _PROG_CACHE[key]
    maps = fused_inputs(inp, cfg)
    res = run_bass_kernel_spmd(B.nc, maps, core_ids=list(range(N_CORES))).results
    out = np.empty((2, SEQ, D), np.float32)
    for core in range(N_CORES):
        b, c4 = divmod(core, CPB)
        out[b, c4 * cfg.TOK:(c4 + 1) * cfg.TOK] = res[core]["outT"].T
    return out


def kernel(**inputs):
    return forward_fused(inputs, 16384)
```

```python
import numpy as np
import ml_dtypes
import contextlib
import concourse.bass as bass
import concourse.mybir as mybir
from concourse.bass_utils import run_bass_kernel_spmd

F32 = mybir.dt.float32
BF16 = mybir.dt.bfloat16
AF = mybir.ActivationFunctionType
ALU = mybir.AluOpType
AX = mybir.AxisListType

D = 1024
NCTX = 256
CPB = 4
NEG = -30000.0
EPS = 1e-6
N_DMA_SEMS = 24

CQ, CKV, KRA, KRB, GQA, GQB, GKA, GKB, GV, WIN_COLS = 0, 384, 640, 672, 704, 1216, 1728, 1856, 1984, 2112


class Cfg:
    def __init__(self, SEQ):
        self.SEQ = SEQ
        self.TOK = SEQ // CPB
        self.TOKX = self.TOK + NCTX
        self.ROWS = self.TOK // 64
        self.SROWS = SEQ // 64
        self.NKEY = NCTX + SEQ
        self.NKT = self.NKEY // 128
        self.NP = self.ROWS // 2
        self.NKC = NCTX + (self.ROWS + 8) * 64
        self.NKCT = self.NKC // 128

    def chunks(self, with_ctx=True):
        out = [(0, NCTX)] if with_ctx else []
        for i in range(self.TOK // 512):
            out.append((NCTX + i * 512, 512))
        return out


class Prog:
    ENGS = ("pe", "act", "dve", "pool", "sp")

    def __init__(self, nc):
        self.nc = nc
        self.q = {k: [] for k in self.ENGS}
        self.cnt = {k: 0 for k in ("pe", "act", "dve", "pool")}
        self.sem = {}
        self.dsem = []
        self.dcnt = [0] * N_DMA_SEMS
        self.drr = 0
        self.known = {k: {} for k in self.ENGS}
        self.res = {}
        self.n_inst = 0
        self.n_wait = 0

    def _need(self, eng, deps):
        kn = self.known[eng]
        for (sk, v) in deps:
            if sk == "pe" and eng == "pe":
                continue
            if kn.get(sk, 0) >= v:
                continue
            kn[sk] = v
            self.n_wait += 1
            self.q[eng].append(("wait", sk, v))

    def _collect(self, reads, writes):
        deps = set()
        for r in reads:
            st = self.res.get(r)
            if st and st[0] is not None:
                deps.add(st[0])
        for w in writes:
            st = self.res.get(w)
            if st:
                if st[0] is not None:
                    deps.add(st[0])
                deps.update(st[1])
        return deps

    def _commit(self, me, reads, writes):
        for r in reads:
            st = self.res.setdefault(r, [None, []])
            st[1] = [d for d in st[1] if d[0] != me[0]] + [me]
        for w in writes:
            self.res[w] = [me, []]

    def op(self, eng, fn, reads=(), writes=()):
        deps = self._collect(reads, writes)
        self._need(eng, deps)
        self.cnt[eng] += 1
        me = (eng, self.cnt[eng])
        self.q[eng].append(("op", fn))
        self.n_inst += 1
        self._commit(me, reads, writes)
        return me

    def dma(self, out, in_, reads=(), writes=(), q="sp"):
        deps = self._collect(reads, writes)
        self._need(q, deps)
        k = self.drr
        self.drr = (self.drr + 1) % N_DMA_SEMS
        sk = ("d", k)
        if self.dcnt[k]:
            self._need(q, [(sk, self.dcnt[k])])
        self.dcnt[k] += 16
        me = (sk, self.dcnt[k])
        self.q[q].append(("dma", out, in_, k))
        self.n_inst += 1
        self._commit(me, reads, writes)
        return me

    def coll(self, kind, groups, src, dst, reads=(), writes=()):
        deps = self._collect(reads, writes)
        self._need("pool", deps)
        self.ccnt = getattr(self, "ccnt", 0) + 1
        me = ("cc", self.ccnt)
        self.q["pool"].append(("coll", kind, groups, src, dst))
        self.n_inst += 1
        self._commit(me, reads, writes)
        self._need("pool", [me])
        return me

    def full_barrier(self):
        deps = [(k, self.cnt[k]) for k in self.cnt if self.cnt[k]]
        deps += [(("d", k), self.dcnt[k]) for k in range(N_DMA_SEMS) if self.dcnt[k]]
        if getattr(self, "ccnt", 0):
            deps.append(("cc", self.ccnt))
        for e in self.ENGS:
            self._need(e, deps)
        self.res = {}

    def _sem(self, sk):
        return self.dsem[sk[1]] if isinstance(sk, tuple) else self.sem[sk]

    def emit(self):
        nc = self.nc
        with contextlib.ExitStack() as es:
            for k in ("pe", "act", "dve", "pool", "cc"):
                self.sem[k] = es.enter_context(nc.semaphore("s_" + k))
            for i in range(N_DMA_SEMS):
                self.dsem.append(es.enter_context(nc.semaphore("s_d%d" % i)))
            block = es.enter_context(nc.Block())
            for name, deco in (("sp", block.sync), ("pe", block.tensor), ("act", block.scalar),
                               ("dve", block.vector), ("pool", block.gpsimd)):
                items = self.q[name]

                def body(eng, items=items, name=name):
                    for it in items:
                        if it[0] == "wait":
                            eng.wait_ge(self._sem(it[1]), it[2])
                        elif it[0] == "op":
                            it[1](eng).then_inc(self.sem[name], 1)
                        elif it[0] == "coll":
                            eng.collective_compute(it[1], ALU.bypass, replica_groups=it[2], ins=[it[3]], outs=[it[4]]).then_inc(self.sem["cc"])
                        else:
                            eng.dma_start(out=it[1], in_=it[2]).then_inc(self.dsem[it[3]], 16)
                deco(body)


def _isz(dt):
    return 4 if dt == F32 else 2


class Bld:
    SBUF_BYTES = 229000

    def __init__(self, cfg):
        self.cfg = cfg
        self.nc = bass.Bass("TRN2", target_bir_lowering=False)
        self.p = Prog(self.nc)
        self.off = 16384 + 512
        self.uid = 0
        self.pools = {}
        self.dram = {}
        nc = self.nc
        self.pw = [nc.alloc_psum_tensor("pw%d" % i, [128, 1024], F32) for i in range(4)]
        self.ident_f = self.alloc("ident_f", [128, 128], F32)
        self.ident_b = self.alloc("ident_b", [128, 128], BF16)
        self.ones_f = self.alloc("ones_f", [128, 128], F32)
        self.ones_b = self.alloc("ones_b", [128, 128], BF16)
        self.blk_f = self.alloc("blk_f", [128, 128], F32)
        self.epsD = self.alloc("epsD", [128, 4], F32)
        self.mv = self.alloc("mv", [128, 4, 2, 6, 8], F32)
        p = self.p
        p.op("pool", lambda e: e.memset(self.ident_f[:], 0.0), writes=["ident_f"])
        p.op("pool", lambda e: e.affine_select(out=self.ident_f[:], in_=self.ident_f[:], pattern=[[-1, 128]],
                                               compare_op=ALU.not_equal, fill=1.0, base=0, channel_multiplier=1),
             reads=["ident_f"], writes=["ident_f"])
        p.op("dve", lambda e: e.tensor_copy(out=self.ident_b[:], in_=self.ident_f[:]), reads=["ident_f"], writes=["ident_b"])
        p.op("dve", lambda e: e.memset(self.ones_f[:], 1.0), writes=["ones_f"])
        p.op("dve", lambda e: e.memset(self.ones_b[:], 1.0), writes=["ones_b"])
        p.op("dve", lambda e: e.memset(self.blk_f[:], 0.0), writes=["blk_f"])
        p.op("dve", lambda e: e.memset(self.blk_f[0:64, 0:64], 1.0), reads=["blk_f"], writes=["blk_f"])
        p.op("dve", lambda e: e.memset(self.blk_f[64:128, 64:128], 1.0), reads=["blk_f"], writes=["blk_f"])
        p.op("dve", lambda e: e.memset(self.epsD[:], EPS), writes=["epsD"])
        self.base_off = self.off

    def alloc(self, name, shape, dt):
        nb = int(np.prod(shape[1:])) * _isz(dt)
        nb = (nb + 63) // 64 * 64
        assert self.off + nb <= self.SBUF_BYTES, ("SBUF overflow", name, self.off, nb)
        self.uid += 1
        t = self.nc.alloc_sbuf_tensor_at("%s_%d" % (name, self.uid), list(shape), dt, offset=self.off)
        self.off += nb
        return t

    def phase_reset(self):
        self.p.full_barrier()
        self.off = self.base_off
        self.pools = {}

    def tmp(self, tag, shape, dt, bufs=2):
        if tag not in self.pools:
            self.pools[tag] = [[(self.alloc(tag, shape, dt)) for _ in range(bufs)], 0]
        pl = self.pools[tag]
        t = pl[0][pl[1] % bufs]
        k = "%s#%d@%d" % (tag, pl[1] % bufs, id(pl))
        pl[1] += 1
        return t, k

    def din(self, name, shape, dt):
        self.dram[name] = self.nc.dram_tensor(name, list(shape), dt, kind="ExternalInput").ap()
        return self.dram[name]

    def dout(self, name, shape, dt):
        self.dram[name] = self.nc.dram_tensor(name, list(shape), dt, kind="ExternalOutput").ap()
        return self.dram[name]

    def dint(self, name, shape, dt):
        self.dram[name] = self.nc.dram_tensor(name, list(shape), dt).ap()
        return self.dram[name]

    def bank(self, i):
        return self.pw[i // 2][:, (i % 2) * 512:(i % 2) * 512 + 512], "bank%d" % i

    def mm(self, out, lhsT, rhs, start, stop, reads, writes):
        self.p.op("pe", lambda e: e.matmul(out, lhsT, rhs, start=start, stop=stop), reads, writes)

    def act(self, out, in_, func, reads, writes, bias=None, scale=1.0, accum_out=None):
        kw = {}
        if bias is not None:
            kw["bias"] = bias
        if accum_out is not None:
            kw["accum_out"] = accum_out
        self.p.op("act", lambda e: e.activation(out=out, in_=in_, func=func, scale=scale, **kw), reads, writes)

    def stt(self, out, in0, scalar, in1, op0, op1, reads, writes, eng="dve"):
        self.p.op(eng, lambda e: e.scalar_tensor_tensor(out=out, in0=in0, scalar=scalar, in1=in1, op0=op0, op1=op1), reads, writes)

    def tt(self, out, in0, in1, op, reads, writes, eng="dve"):
        self.p.op(eng, lambda e: e.tensor_tensor(out=out, in0=in0, in1=in1, op=op), reads, writes)

    def ts(self, out, in0, s1, s2, op0, op1, reads, writes, eng="dve", accum_out=None):
        if op1 is None:
            self.p.op(eng, lambda e: e.tensor_scalar(out=out, in0=in0, scalar1=s1, scalar2=None, op0=op0), reads, writes)
        elif accum_out is None:
            self.p.op(eng, lambda e: e.tensor_scalar(out=out, in0=in0, scalar1=s1, scalar2=s2, op0=op0, op1=op1), reads, writes)
        else:
            self.p.op(eng, lambda e: e.tensor_scalar(out=out, in0=in0, scalar1=s1, scalar2=s2, op0=op0, op1=op1, accum_out=accum_out), reads, writes)

    def cp(self, out, in_, reads, writes, eng="dve"):
        if eng == "act":
            self.p.op("act", lambda e: e.copy(out=out, in_=in_), reads, writes)
        else:
            self.p.op(eng, lambda e: e.tensor_copy(out=out, in_=in_), reads, writes)

    def recip(self, out, in_, reads, writes):
        self.p.op("dve", lambda e: e.reciprocal(out=out, in_=in_), reads, writes)

    def rstd_from_sum(self, ps_ap, ps_key, n_feat, n, tag, np_=128):
        t, k = self.tmp(tag, [128, 512], F32, bufs=2)
        self.act(t[0:np_, 0:n], ps_ap, AF.Sqrt, [ps_key, "epsD"], [k], bias=self.epsD[0:np_, 0:1], scale=1.0 / n_feat)
        self.recip(t[0:np_, 0:n], t[0:np_, 0:n], [k], [k])
        return t, k


def phase_modvec(B, layers):
    p = B.p
    c_in = B.dram["c_in"]
    cs, ck = B.tmp("c_s", [128, 8, 2], F32, 1)
    cb, cbk = B.tmp("c_b", [128, 8, 2], BF16, 1)
    p.dma(cs[:], c_in[:, :, :], writes=[ck])
    B.act(cb[:], cs[:], AF.Silu, [ck], [cbk])
    nm, nmk = B.tmp("nm", [128, 4, 8], F32, 1)
    nf, nfk = B.tmp("nf", [128, 4, 8], F32, 1)
    ba, bak = B.tmp("ba", [128, 4, 48], F32, 1)
    p.dma(nm[:], B.dram["nmix_p"][:, :, :], writes=[nmk])
    p.dma(nf[:], B.dram["nffn_p"][:, :, :], writes=[nfk])
    p.dma(ba[:], B.dram["bada_p"][:, :, :], writes=[bak])
    M, Mk = B.tmp("modM", [128, 48, 2], F32, 1)
    for li, l in enumerate(layers):
        wada = B.dram["wada_p"]
        ps, psk = B.bank(0)
        for cbk_i in range(12):
            w, wk = B.tmp("wada_blk", [128, 8, 512], BF16, 2)
            p.dma(w[:], wada[li, :, :, cbk_i * 512:(cbk_i + 1) * 512], writes=[wk], q="pool")
            for f in range(4):
                ft = cbk_i * 4 + f
                for k in range(8):
                    B.mm(ps[:, ft * 2:ft * 2 + 2], w[:, k, f * 128:(f + 1) * 128], cb[:, k, :], k == 0, k == 7,
                         [wk, cbk], [psk])
        B.tt(M[:], ps[:, 0:96].rearrange("p (t j) -> p t j", j=2), ba[:, l, :].unsqueeze(2).to_broadcast([128, 48, 2]),
             ALU.add, [psk, bak], [Mk])
        for j in range(2):
            mvl = B.mv[:, l, j]
            B.stt(mvl[:, 0, :], M[:, 8:16, j], 1.0, nm[:, l, :], ALU.add, ALU.mult, [Mk, nmk], ["mv"])
            B.cp(mvl[:, 1, :], M[:, 0:8, j], [Mk], ["mv"])
            B.cp(mvl[:, 2, :], M[:, 16:24, j], [Mk], ["mv"])
            B.stt(mvl[:, 3, :], M[:, 32:40, j], 1.0, nf[:, l, :], ALU.add, ALU.mult, [Mk, nfk], ["mv"])
            B.cp(mvl[:, 4, :], M[:, 24:32, j], [Mk], ["mv"])
            B.cp(mvl[:, 5, :], M[:, 40:48, j], [Mk], ["mv"])


def norm_mod(B, xt, xk, n, l, j, kind0, want_f32=False, tagp=""):
    sq, sqk = B.tmp("nm_sq" + tagp, [128, 8, 512], F32, 1)
    B.act(sq[:, :, 0:n], xt, AF.Square, [xk], [sqk])
    ps, psk = B.bank(0)
    for k in range(8):
        B.mm(ps[:, 0:n], B.ones_f[:], sq[:, k, 0:n], k == 0, k == 7, ["ones_f", sqk], [psk])
    rs, rsk = B.rstd_from_sum(ps[:, 0:n], psk, float(D), n, "nm_rstd" + tagp)
    hT, hk = B.tmp("hT" + tagp, [128, 8, 512], BF16, 1)
    hf = hfk = None
    if want_f32:
        hf, hfk = B.tmp("hTf" + tagp, [128, 8, 512], F32, 1)
    for k in range(8):
        t, tk = B.tmp("nm_t" + tagp, [128, 512], F32, 2)
        B.stt(t[:, 0:n], xt[:, k, :], B.mv[:, l, j, kind0, k:k + 1], rs[:, 0:n], ALU.mult, ALU.mult, [xk, "mv", rsk], [tk])
        if want_f32:
            B.act(hf[:, k, 0:n], t[:, 0:n], AF.Identity, [tk, "mv"], [hfk], bias=B.mv[:, l, j, kind0 + 1, k:k + 1])
            B.cp(hT[:, k, 0:n], hf[:, k, 0:n], [hfk], [hk])
        else:
            B.act(hT[:, k, 0:n], t[:, 0:n], AF.Identity, [tk, "mv"], [hk], bias=B.mv[:, l, j, kind0 + 1, k:k + 1])
    return hT, hk, hf, hfk


def phase_A_even(B, l, xT_dram):
    cfg, p = B.cfg, B.p
    dr = B.dram
    win, wink = B.tmp("win", [128, 8, WIN_COLS], BF16, 1)
    wq, wqk = B.tmp("wq", [128, 3, 1024], BF16, 1)
    wkv, wkvk = B.tmp("wkv", [128, 2, 1024], BF16, 1)
    gn, gnk = B.tmp("gains", [128, 12], F32, 1)
    for k in range(8):
        p.dma(win[:, k, :], dr["win_p"][:, k, :], writes=[wink], q="pool")
    p.dma(wq[:], dr["wq_p"][:, :, :], writes=[wqk], q="pool")
    p.dma(wkv[:], dr["wkv_p"][:, :, :], writes=[wkvk], q="pool")
    p.dma(gn[:], dr["gains_e"][:, :], writes=[gnk])
    QTA, QTB, KTA, KTB, VA, VB = (dr[k] for k in ("QT_A_o", "QT_B_o", "KT_A_o", "KT_B_o", "V_A_o", "V_B_o"))
    for (c0, n) in cfg.chunks(True):
        j = 1 if c0 == 0 else 0
        xt, xk = B.tmp("xT_a", [128, 8, 512], F32, 1)
        p.dma(xt[:, :, 0:n], xT_dram.rearrange("(k p) t -> p k t", p=128)[:, :, c0:c0 + n], writes=[xk])
        hT, hk, _, _ = norm_mod(B, xt[:, :, 0:n], xk, n, l, j, 0)
        ta, tak = B.tmp("tabA", [128, 4, 512], F32, 1)
        tb, tbk = B.tmp("tabB", [128, 4, 512], F32, 1)
        p.dma(ta[64:96, :, 0:n], dr["tabA"][64:96, :, c0:c0 + n], writes=[tak])
        p.dma(tb[:, :, 0:n], dr["tabB"][:, :, c0:c0 + n], writes=[tbk])

        def proj(ps_ap, psk, col0, m, out_base=0):
            for k in range(8):
                B.mm(ps_ap, win[:, k, col0:col0 + m], hT[:, k, 0:n], k == 0, k == 7, [wink, hk], [psk])

        def latent(col0, ntile, gcol0, nfeat, tag):
            raw, rawk = B.tmp("lat_raw", [128, 3, 512], F32, 1)
            sq, sqk = B.tmp("lat_sq", [128, 3, 512], F32, 1)
            for m in range(ntile):
                ps, psk = B.bank(2 + (m % 2))
                proj(ps[:, 0:n], psk, col0 + m * 128, 128)
                B.cp(raw[:, m, 0:n], ps[:, 0:n], [psk], [rawk], eng="act")
                B.act(sq[:, m, 0:n], ps[:, 0:n], AF.Square, [psk], [sqk])
            ps, psk = B.bank(1)
            for m in range(ntile):
                B.mm(ps[:, 0:n], B.ones_f[:], sq[:, m, 0:n], m == 0, m == ntile - 1, ["ones_f", sqk], [psk])
            rs, rsk = B.rstd_from_sum(ps[:, 0:n], psk, float(nfeat), n, tag + "_rstd")
            o, ok_ = B.tmp(tag + "_n", [128, 3, 512], BF16, 2)
            for m in range(ntile):
                B.stt(o[:, m, 0:n], raw[:, m, 0:n], gn[:, gcol0 + m:gcol0 + m + 1], rs[:, 0:n], ALU.mult, ALU.mult,
                      [rawk, gnk, rsk], [ok_])
            return o, ok_

        cqn, cqnk = latent(CQ, 3, 0, 384, "cq")
        ckvn, ckvnk = latent(CKV, 2, 3, 256, "ckv")

        psA, psAk = B.bank(2)
        psB, psBk = B.bank(3)
        proj(psA[64:96, 0:n], psAk, KRA, 32)
        proj(psB[64:96, 0:n], psBk, KRB, 32)
        kst, kstk = B.tmp("kstage", [128, 512], BF16, 2)
        t1, t1k = B.tmp("rope_t1", [128, 512], F32, 2)
        t2, t2k = B.tmp("rope_t2", [128, 512], F32, 2)
        B.tt(t1[64:96, 0:n], psA[64:96, 0:n], ta[64:96, 2, 0:n], ALU.mult, [psAk, tak], [t1k])
        B.tt(t2[64:96, 0:n], psB[64:96, 0:n], ta[64:96, 3, 0:n], ALU.mult, [psBk, tak], [t2k])
        B.tt(kst[64:96, 0:n], t1[64:96, 0:n], t2[64:96, 0:n], ALU.add, [t1k, t2k], [kstk])
        for h in range(8):
            p.dma(KTA[h, 64:96, c0:c0 + n], kst[64:96, 0:n], reads=[kstk])
        for h in range(8):
            ps, psk = B.bank(2 + (h % 2))
            for k in range(2):
                B.mm(ps[0:64, 0:n], wkv[:, k, h * 64:(h + 1) * 64], ckvn[:, k, 0:n], k == 0, k == 1, [wkvk, ckvnk], [psk])
            kn, knk = B.tmp("knope", [64, 512], BF16, 3)
            B.cp(kn[:, 0:n], ps[0:64, 0:n], [psk], [knk], eng="act" if h % 2 else "dve")
            p.dma(KTA[h, 0:64, c0:c0 + n], kn[:, 0:n], reads=[knk])
        for jt in range(n // 128):
            ps, psk = B.bank(4 + (jt % 2))
            for k in range(2):
                B.mm(ps[:, 0:512], ckvn[:, k, jt * 128:(jt + 1) * 128], wkv[:, k, 512:1024], k == 0, k == 1, [ckvnk, wkvk], [psk])
            v, vk = B.tmp("va_tok", [128, 512], BF16, 2)
            B.cp(v[:], ps[:, 0:512], [psk], [vk], eng="act" if jt % 2 else "dve")
            if "V_hm" in dr:
                p.dma(VA.rearrange("(h t) d -> t h d", h=8)[c0 + jt * 128:c0 + (jt + 1) * 128, :, :], v[:].rearrange("p (h d) -> p h d", d=64), reads=[vk])
            else:
                p.dma(VA[c0 + jt * 128:c0 + (jt + 1) * 128, :], v[:], reads=[vk])
        for h in range(8):
            psA, psAk = B.bank(2 + 2 * (h % 2))
            psB, psBk = B.bank(3 + 2 * (h % 2))
            for k in range(3):
                B.mm(psA[0:96, 0:n], wq[:, k, h * 96:(h + 1) * 96], cqn[:, k, 0:n], k == 0, k == 2, [wqk, cqnk], [psAk])
            for k in range(3):
                B.mm(psB[64:96, 0:n], wq[:, k, 768 + h * 32:768 + (h + 1) * 32], cqn[:, k, 0:n], k == 0, k == 2, [wqk, cqnk], [psBk])
            qs, qsk = B.tmp("qstage", [128, 512], BF16, 3)
            B.act(qs[0:64, 0:n], psA[0:64, 0:n], AF.Copy, [psAk], [qsk], scale=96.0 ** -0.5)
            t1, t1k = B.tmp("rope_t1", [128, 512], F32, 2)
            t2, t2k = B.tmp("rope_t2", [128, 512], F32, 2)
            B.tt(t1[64:96, 0:n], psA[64:96, 0:n], ta[64:96, 0, 0:n], ALU.mult, [psAk, tak], [t1k])
            B.tt(t2[64:96, 0:n], psB[64:96, 0:n], ta[64:96, 1, 0:n], ALU.mult, [psBk, tak], [t2k])
            B.tt(qs[64:96, 0:n], t1[64:96, 0:n], t2[64:96, 0:n], ALU.add, [t1k, t2k, qsk], [qsk])
            p.dma(QTA[h, :, c0:c0 + n], qs[0:96, 0:n], reads=[qsk])

        def gqa_tile(colA, colB, gA, gB, ci, si, dst_fn):
            psA, psAk = B.bank(6)
            psB, psBk = B.bank(7)
            proj(psA[:, 0:n], psAk, colA, 128)
            proj(psB[:, 0:n], psBk, colB, 128)
            sq, sqk = B.tmp("g_sq", [128, 512], F32, 2)
            B.act(sq[:, 0:n], psA[:, 0:n], AF.Square, [psAk], [sqk])
            pss, pssk = B.bank(1)
            B.mm(pss[:, 0:n], B.blk_f[:], sq[:, 0:n], True, True, ["blk_f", sqk], [pssk])
            rs, rsk = B.rstd_from_sum(pss[:, 0:n], pssk, 64.0, n, "g_rstd")
            u1, u1k = B.tmp("g_u1", [128, 512], F32, 2)
            u2, u2k = B.tmp("g_u2", [128, 512], F32, 2)
            B.stt(u1[:, 0:n], psA[:, 0:n], gn[:, gA:gA + 1], tb[:, ci, 0:n], ALU.mult, ALU.mult, [psAk, gnk, tbk], [u1k])
            B.stt(u2[:, 0:n], psB[:, 0:n], gn[:, gB:gB + 1], tb[:, si, 0:n], ALU.mult, ALU.mult, [psBk, gnk, tbk], [u2k])
            B.tt(u1[:, 0:n], u1[:, 0:n], u2[:, 0:n], ALU.add, [u1k, u2k], [u1k], eng="pool")
            o, ok_ = B.tmp("g_out", [128, 512], BF16, 3)
            B.tt(o[:, 0:n], u1[:, 0:n], rs[:, 0:n], ALU.mult, [u1k, rsk], [ok_])
            dst_fn(o, ok_)

        for m in range(4):
            def dst(o, ok_, m=m):
                p.dma(QTB.rearrange("h d t -> (h d) t")[m * 128:(m + 1) * 128, c0:c0 + n], o[:, 0:n], reads=[ok_])
            gqa_tile(GQA + m * 128, GQB + m * 128, 5, 6, 0, 1, dst)

        def dstk(o, ok_):
            p.dma(KTB.rearrange("h d t -> (h d) t")[:, c0:c0 + n], o[:, 0:n], reads=[ok_])
        gqa_tile(GKA, GKB, 7, 8, 2, 3, dstk)
        for jt in range(n // 128):
            ps, psk = B.bank(4 + (jt % 2))
            for k in range(8):
                B.mm(ps[:, 0:128], hT[:, k, jt * 128:(jt + 1) * 128], win[:, k, GV:GV + 128], k == 0, k == 7, [hk, wink], [psk])
            v, vk = B.tmp("vb_tok", [128, 128], BF16, 2)
            B.cp(v[:], ps[:, 0:128], [psk], [vk], eng="act" if jt % 2 else "dve")
            if "V_hm" in dr:
                p.dma(VB.rearrange("(h t) d -> t h d", h=2)[c0 + jt * 128:c0 + (jt + 1) * 128, :, :], v[:].rearrange("p (h d) -> p h d", d=64), reads=[vk])
            else:
                p.dma(VB[c0 + jt * 128:c0 + (jt + 1) * 128, :], v[:], reads=[vk])


def attend_group(B, kt, ktk, d, vt, vtk, qt, qtk, qcol0, nq, key_tiles, out_dram_ap, bias_key=None):
    p = B.p
    per = 1024 // nq
    groups = [key_tiles[i:i + per] for i in range(0, len(key_tiles), per)]
    po, pok = B.bank(6)
    first = True
    pend = []
    for gi, grp in enumerate(groups):
        B._pw = (getattr(B, "_pw", 0) + 1) % 3
        ps = B.pw[B._pw]
        psk = "wide%d" % B._pw
        for ti, (t, bias_ap) in enumerate(grp):
            sl = ps[:, ti * nq:(ti + 1) * nq]
            B.mm(sl, kt[0:d, t * 128:(t + 1) * 128], qt[0:d, qcol0:qcol0 + nq], True, bias_ap is None, [ktk, qtk], [psk])
            if bias_ap is not None:
                B.mm(sl, B.ident_b[:], bias_ap, False, True, ["ident_b", bias_key], [psk])
        pt, ptk = B.tmp("pT", [128, 1024], BF16, 4)
        ncol = len(grp) * nq
        if len(pend) >= 2:
            pend.pop(0)()
        B.act(pt[:, 0:ncol], ps[:, 0:ncol], AF.Exp, [psk], [ptk])

        def pv(grp=grp, pt=pt, ptk=ptk, is_first=first, is_last=(gi == len(groups) - 1)):
            for ti, (t, _) in enumerate(grp):
                B.mm(po[0:65, 0:nq], vt[:, t, 0:65], pt[:, ti * nq:(ti + 1) * nq], is_first and ti == 0,
                     is_last and ti == len(grp) - 1, [vtk, ptk], [pok])
        pend.append(pv)
        first = False
    while pend:
        pend.pop(0)()
    rc, rck = B.tmp("att_rc", [128, 512], F32, 2)
    B.recip(rc[64:65, 0:nq], po[64:65, 0:nq], [pok], [rck])
    pb, pbk = B.bank(7)
    B.mm(pb[0:64, 0:nq], B.ones_f[64:65, 0:64], rc[64:65, 0:nq], True, True, ["ones_f", rck], [pbk])
    bc, bck = B.tmp("att_bc", [64, 512], F32, 2)
    B.cp(bc[:, 0:nq], pb[0:64, 0:nq], [pbk], [bck], eng="act")
    o, ok_ = B.tmp("att_o", [64, 512], BF16, 3)
    B.tt(o[:, 0:nq], po[0:64, 0:nq], bc[:, 0:nq], ALU.mult, [pok, bck], [ok_])
    p.dma(out_dram_ap, o[:, 0:nq], reads=[ok_])


def load_v(B, vt, vtk, vsrc, ntiles, step=10):
    for a in range(0, ntiles, step):
        b_ = min(ntiles, a + step)
        B.p.dma(vt[:, a:b_, 0:64], vsrc[:, a:b_, :], writes=[vtk])


def phase_att_even(B, need_ctx, gathered=False):
    cfg, p, dr = B.cfg, B.p, B.dram
    TOK, TOKX = cfg.TOK, cfg.TOKX
    tpr = TOK // 128
    OT = dr["OT"]
    NKT = cfg.NKT
    vts = [B.alloc("vt%d" % i, [128, NKT, 80], BF16) for i in range(2)]
    for i in range(2):
        p.op("pool", lambda e, i=i: e.memset(vts[i][:, :, 64:80], 1.0), writes=["vt%d" % i])
    def load_head(hh):
        isA = hh < 8
        h = hh if isA else hh - 8
        d = 96 if isA else 64
        kt, ktk = B.tmp("ktA", [96, cfg.NKEY], BF16, 2)
        qt, qtk = B.tmp("qt", [96, cfg.TOKX], BF16, 2)
        vt, vtk = vts[hh % 2], "vt%d" % (hh % 2)
        if gathered:
            kg = dr["KT_A_g"] if isA else dr["KT_B_g"]
            hk, dd = (h, 96) if isA else (h // 4, 64)
            p.dma(kt[0:d, 0:NCTX], kg[(hk * CPB) * dd:(hk * CPB) * dd + d, 0:NCTX], writes=[ktk])
            for r in range(CPB):
                p.dma(kt[0:d, NCTX + r * TOK:NCTX + (r + 1) * TOK], kg[(hk * CPB + r) * dd:(hk * CPB + r) * dd + d, NCTX:TOKX], writes=[ktk])
            p.dma(qt[0:d, :], (dr["QT_A_i"] if isA else dr["QT_B_i"])[h, :, :], writes=[qtk])
            vg = dr["V_A_g"] if isA else dr["V_B_g"]
            vv = vg.rearrange("(a t p) d -> a p t d", p=128, t=TOKX // 128)
            p.dma(vt[:, 0:2, 0:64], vv[hk * CPB, :, 0:2, :], writes=[vtk])
            for r in range(CPB):
                for a in range(0, tpr, 8):
                    b_ = min(tpr, a + 8)
                    p.dma(vt[:, 2 + r * tpr + a:2 + r * tpr + b_, 0:64], vv[hk * CPB + r, :, 2 + a:2 + b_, :], writes=[vtk])
        elif isA:
            p.dma(kt[0:96, :], dr["KT_A_f"][h, :, :], writes=[ktk])
            p.dma(qt[0:96, :], dr["QT_A_i"][h, :, :], writes=[qtk])
            load_v(B, vt, vtk, dr["V_A_f"].rearrange("(t p) (h d) -> p t h d", p=128, d=64)[:, :, h, :], NKT)
        else:
            p.dma(kt[0:64, :], dr["KT_B_f"][h // 4, :, :], writes=[ktk])
            p.dma(qt[0:64, :], dr["QT_B_i"][h, :, :], writes=[qtk])
            load_v(B, vt, vtk, dr["V_B_f"].rearrange("(t p) (h d) -> p t h d", p=128, d=64)[:, :, h // 4, :], NKT)
        return kt, ktk, qt, qtk, vt, vtk, d

    nxt = load_head(0)
    for hh in range(16):
        kt, ktk, qt, qtk, vt, vtk, d = nxt
        if hh + 1 < 16:
            nxt = load_head(hh + 1)
        if need_ctx:
            attend_group(B, kt, ktk, d, vt, vtk, qt, qtk, 0, NCTX, [(0, None), (1, None)], OT[hh, :, 0:NCTX])
        for (c0, n) in cfg.chunks(False):
            attend_group(B, kt, ktk, d, vt, vtk, qt, qtk, c0, n, [(t, None) for t in range(NKT)], OT[hh, :, c0:c0 + n])


def phase_A_odd(B, l, xT_dram):
    cfg, p, dr = B.cfg, B.p, B.dram
    w, wk = B.tmp("wna", [128, 8, 3072], BF16, 1)
    for k in range(8):
        p.dma(w[:, k, :], dr["wna_p"][:, k, :], writes=[wk], q="pool")
    QT, KT, V = dr["QT_C_o"], dr["KT_C_o"], dr["V_C_o"]
    for (c0, n) in cfg.chunks(True):
        j = 1 if c0 == 0 else 0
        xt, xk = B.tmp("xT_a", [128, 8, 512], F32, 1)
        p.dma(xt[:, :, 0:n], xT_dram.rearrange("(k p) t -> p k t", p=128)[:, :, c0:c0 + n], writes=[xk])
        hT, hk, _, _ = norm_mod(B, xt[:, :, 0:n], xk, n, l, j, 0)
        for which, dst, sc in ((0, QT, 0.125), (1, KT, 1.0)):
            for m in range(8):
                ps, psk = B.bank(2 + (m % 4))
                for k in range(8):
                    B.mm(ps[:, 0:n], w[:, k, which * 1024 + m * 128:which * 1024 + (m + 1) * 128], hT[:, k, 0:n], k == 0, k == 7, [wk, hk], [psk])
                o, ok_ = B.tmp("na_qk", [128, 512], BF16, 3)
                if m % 2:
                    B.act(o[:, 0:n], ps[:, 0:n], AF.Copy, [psk], [ok_], scale=sc)
                else:
                    B.ts(o[:, 0:n], ps[:, 0:n], sc, None, ALU.mult, None, [psk], [ok_])
                p.dma(dst[m * 128:(m + 1) * 128, c0:c0 + n], o[:, 0:n], reads=[ok_])
                if which == 1 and "EK_o" in dr:
                    if c0 == NCTX:
                        p.dma(dr["EK_o"][m * 128:(m + 1) * 128, 0:256], o[:, 0:256], reads=[ok_])
                    if c0 == cfg.TOKX - 512:
                        p.dma(dr["EK_o"][m * 128:(m + 1) * 128, 256:512], o[:, 256:512], reads=[ok_])
        for jt in range(n // 128):
            for hf in range(2):
                ps, psk = B.bank(6 + hf)
                for k in range(8):
                    B.mm(ps[:, 0:512], hT[:, k, jt * 128:(jt + 1) * 128], w[:, k, 2048 + hf * 512:2048 + (hf + 1) * 512], k == 0, k == 7, [hk, wk], [psk])
                v, vk = B.tmp("na_v", [128, 512], BF16, 3)
                B.cp(v[:], ps[:, 0:512], [psk], [vk], eng="act" if hf else "dve")
                p.dma(V[c0 + jt * 128:c0 + (jt + 1) * 128, hf * 512:(hf + 1) * 512], v[:], reads=[vk])
                if "EV_o" in dr:
                    if c0 == NCTX and jt < 2:
                        p.dma(dr["EV_o"][jt * 128:(jt + 1) * 128, hf * 512:(hf + 1) * 512], v[:], reads=[vk])
                    if c0 == cfg.TOKX - 512 and jt >= 2:
                        p.dma(dr["EV_o"][jt * 128:(jt + 1) * 128, hf * 512:(hf + 1) * 512], v[:], reads=[vk])


def na_pair_tiles(cfg, i):
    P = cfg.NP
    if i == 0:
        return list(range(0, 6)), 5
    if i == 1:
        return list(range(1, 6)), 11
    if i == P - 2:
        return list(range(P - 2, P + 3)), 16
    if i == P - 1:
        return list(range(P - 2, P + 4)), 21
    return list(range(i, i + 5)), 0


def phase_att_odd(B, need_ctx):
    cfg, p, dr = B.cfg, B.p, B.dram
    OT = dr["OT"]
    NT = cfg.NKCT
    vts = [B.alloc("nvt%d" % i, [128, NT, 80], BF16) for i in range(2)]
    for i in range(2):
        p.op("pool", lambda e, i=i: e.memset(vts[i][:, :, 64:80], 1.0), writes=["nvt%d" % i])
    for h in range(16):
        kt, ktk = B.tmp("nkt", [64, cfg.NKC], BF16, 2)
        qt, qtk = B.tmp("nqt", [64, cfg.TOKX], BF16, 2)
        bt, btk = B.tmp("nbt", [128, 27, 128], BF16, 2)
        vt, vtk = vts[h % 2], "nvt%d" % (h % 2)
        p.dma(kt[:], dr["KT_C_b"][h * 64:(h + 1) * 64, :], writes=[ktk])
        p.dma(qt[:], dr["QT_C_i"][h * 64:(h + 1) * 64, :], writes=[qtk])
        p.dma(bt[:], dr["btab"][h, :, :, :], writes=[btk], q="pool")
        load_v(B, vt, vtk, dr["V_C_b"].rearrange("(t p) (h d) -> p t h d", p=128, d=64)[:, :, h, :], NT)
        if need_ctx:
            attend_group(B, kt, ktk, 64, vt, vtk, qt, qtk, 0, NCTX, [(0, None), (1, None)], OT[h, :, 0:NCTX])
        for i in range(cfg.NP):
            us, slot = na_pair_tiles(cfg, i)
            tiles = [(0, None), (1, None)] + [(2 + u, bt[:, slot + ui, :]) for ui, u in enumerate(us)]
            attend_group(B, kt, ktk, 64, vt, vtk, qt, qtk, NCTX + i * 128, 128, tiles,
                         OT[h, :, NCTX + i * 128:NCTX + (i + 1) * 128], bias_key=btk)


def phase_outproj(B, l, xT_in, xT_out, last):
    cfg, p, dr = B.cfg, B.p, B.dram
    OT = dr["OT"]
    wo, wok = B.tmp("wout", [64, 16, 1024], BF16, 1)
    for h in range(0, 16, 4):
        p.dma(wo[:, h:h + 4, :], dr["wout_p"][:, h:h + 4, :], writes=[wok], q="pool")
    for (c0, n) in cfg.chunks(not last):
        j = 1 if c0 == 0 else 0
        xt, xk = B.tmp("xT_o", [128, 8, 512], F32, 2)
        p.dma(xt[:, :, 0:n], xT_in.rearrange("(k p) t -> p k t", p=128)[:, :, c0:c0 + n], writes=[xk])
        ot, otk = B.tmp("ot_sb", [64, 16, 512], BF16, 2)
        p.dma(ot[:, :, 0:n], OT.rearrange("h d t -> d h t")[:, :, c0:c0 + n], writes=[otk])
        for f in range(8):
            ps, psk = B.bank(f % 4)
            for hh in range(16):
                B.mm(ps[:, 0:n], wo[:, hh, f * 128:(f + 1) * 128], ot[:, hh, 0:n], hh == 0, hh == 15, [wok, otk], [psk])
            B.stt(xt[:, f, 0:n], ps[:, 0:n], B.mv[:, l, j, 2, f:f + 1], xt[:, f, 0:n], ALU.mult, ALU.add, [psk, "mv", xk], [xk])
        p.dma(xT_out.rearrange("(k p) t -> p k t", p=128)[:, :, c0:c0 + n], xt[:, :, 0:n], reads=[xk])
    if last:
        xt, xk = B.tmp("xT_o", [128, 8, 512], F32, 2)
        p.dma(xt[:, :, 0:NCTX], xT_in.rearrange("(k p) t -> p k t", p=128)[:, :, 0:NCTX], writes=[xk])
        p.dma(xT_out.rearrange("(k p) t -> p k t", p=128)[:, :, 0:NCTX], xt[:, :, 0:NCTX], reads=[xk])


def phase_ffn(B, l, xT, last):
    cfg, p, dr = B.cfg, B.p, B.dram
    wr, wrk = B.tmp("wr", [128, 8, 36], F32, 1)
    p.dma(wr[:], dr["wr_p"][:, :, :], writes=[wrk])
    brt, brk = B.tmp("br", [128, 36], F32, 1)
    p.dma(brt[:], dr["br_p"][0:1, :].partition_broadcast(128), writes=[brk])
    sel, selk = B.tmp("sel", [32, 32, 128], BF16, 1)
    for e_ in range(32):
        B.cp(sel[:, e_, :], B.ident_b[0:32, e_:e_ + 1].to_broadcast([32, 128]), ["ident_b"], [selk], eng="pool" if e_ % 2 else "dve")
    lat_chunks = cfg.chunks(False)
    passes = [lat_chunks[i:i + 2] for i in range(0, len(lat_chunks), 2)]
    if not last:
        passes[0] = [(0, NCTX)] + passes[0]
    wg_d, wu_d, wd_d = dr["wg_p"], dr["wu_p"], dr["wd_p"]
    MAXN = 1280
    xv = xT.rearrange("(k p) t -> p k t", p=128)
    for subs in passes:
        xs, xsk0 = B.tmp("xs", [128, 8, MAXN], F32, 1)
        h2, h2k0 = B.tmp("h2", [128, 8, MAXN], BF16, 1)
        gT, gTk0 = B.tmp("gT", [32, MAXN], BF16, 1)
        offs, o_ = [], 0
        for (c0, n) in subs:
            offs.append(o_)
            o_ += n
        for si, (c0, n) in enumerate(subs):
            j = 1 if c0 == 0 else 0
            so = offs[si]
            xsk, h2k, gTk = "%s_%d" % (xsk0, si), "%s_%d" % (h2k0, si), "%s_%d" % (gTk0, si)
            p.dma(xs[:, :, so:so + n], xv[:, :, c0:c0 + n], writes=[xsk])
            sq, sqk = B.tmp("f_sq", [128, 8, 512], F32, 1)
            B.act(sq[:, :, 0:n], xs[:, :, so:so + n], AF.Square, [xsk], [sqk])
            ps, psk = B.bank(0)
            for k in range(8):
                B.mm(ps[:, 0:n], B.ones_f[:], sq[:, k, 0:n], k == 0, k == 7, ["ones_f", sqk], [psk])
            rs, rsk = B.rstd_from_sum(ps[:, 0:n], psk, float(D), n, "f_rstd")
            hf, hfk = sq, sqk
            for k in range(8):
                t, tk = B.tmp("f_t", [128, 512], F32, 2)
                B.stt(t[:, 0:n], xs[:, k, so:so + n], B.mv[:, l, j, 3, k:k + 1], rs[:, 0:n], ALU.mult, ALU.mult, [xsk, "mv", rsk], [tk])
                B.act(hf[:, k, 0:n], t[:, 0:n], AF.Identity, [tk, "mv", psk], [hfk], bias=B.mv[:, l, j, 4, k:k + 1])
                B.cp(h2[:, k, so:so + n], hf[:, k, 0:n], [hfk], [h2k], eng="pool" if k % 2 else "dve")
            for jt in range(n // 128):
                psl, pslk = B.bank(4 + (jt % 2))
                for k in range(8):
                    B.mm(psl[:, 0:36], hf[:, k, jt * 128:(jt + 1) * 128], wr[:, k, :], k == 0, k == 7, [hfk, wrk], [pslk])
                lg, lgk = B.tmp("r_lg", [128, 36], F32, 2)
                B.tt(lg[:], psl[:, 0:36], brt[:], ALU.add, [pslk, brk], [lgk])
                sc_, sck = B.tmp("r_sc", [128, 8], F32, 2)
                R = [lgk, sck]
                p.op("dve", lambda e, lg=lg, sc_=sc_: e.reduce_max(out=sc_[:, 0:1], in_=lg[:, 0:4], axis=AX.X), R, [sck])
                B.ts(sc_[:, 1:2], sc_[:, 0:1], -1.0, None, ALU.mult, None, [sck], [sck])
                eg, egk = B.tmp("r_eg", [128, 4], F32, 2)
                B.act(eg[:], lg[:, 0:4], AF.Exp, [lgk, sck], [egk, sck], bias=sc_[:, 1:2], accum_out=sc_[:, 2:3])
                goh, gohk = B.tmp("r_goh", [128, 4], F32, 2)
                B.ts(goh[:], lg[:, 0:4], sc_[:, 0:1], None, ALU.is_equal, None, [lgk, sck], [gohk])
                B.ts(goh[:], goh[:], -1.0, 1.0e4, ALU.add, ALU.mult, [gohk], [gohk])
                lem, lemk = B.tmp("r_lem", [128, 4, 8], F32, 2)
                B.tt(lem[:], lg[:, 4:36].rearrange("p (g e) -> p g e", e=8), goh[:].unsqueeze(2).to_broadcast([128, 4, 8]), ALU.add,
                     [lgk, gohk], [lemk])
                lem2 = lem[:].rearrange("p g e -> p (g e)")
                p.op("dve", lambda e, lem2=lem2, sc_=sc_: e.reduce_max(out=sc_[:, 3:4], in_=lem2, axis=AX.X), [lemk, sck], [sck])
                B.ts(sc_[:, 4:5], sc_[:, 3:4], -1.0, None, ALU.mult, None, [sck], [sck])
                ee, eek = B.tmp("r_ee", [128, 32], F32, 2)
                B.act(ee[:], lem2, AF.Exp, [lemk, sck], [eek], bias=sc_[:, 4:5])
                oh1, oh1k = B.tmp("r_oh1", [128, 32], F32, 2)
                B.ts(oh1[:], lem2, sc_[:, 3:4], None, ALU.is_equal, None, [lemk, sck], [oh1k])
                e2, e2k = B.tmp("r_e2", [128, 32], F32, 2)
                B.stt(e2[:], oh1[:], -2.0, ee[:], ALU.mult, ALU.add, [oh1k, eek], [e2k])
                p.op("dve", lambda e, e2=e2, sc_=sc_: e.reduce_max(out=sc_[:, 5:6], in_=e2[:], axis=AX.X), [e2k, sck], [sck])
                oh2, oh2k = B.tmp("r_oh2", [128, 32], F32, 2)
                B.ts(oh2[:], e2[:], sc_[:, 5:6], None, ALU.is_equal, None, [e2k, sck], [oh2k])
                B.tt(oh1[:], oh1[:], oh2[:], ALU.add, [oh1k, oh2k], [oh1k])
                B.ts(sc_[:, 6:7], sc_[:, 5:6], 1.0, None, ALU.add, None, [sck], [sck])
                B.tt(sc_[:, 6:7], sc_[:, 6:7], sc_[:, 2:3], ALU.mult, [sck], [sck])
                B.recip(sc_[:, 7:8], sc_[:, 6:7], [sck], [sck])
                G, Gk = B.tmp("r_G", [128, 32], F32, 2)
                B.stt(G[:], ee[:], sc_[:, 7:8], oh1[:], ALU.mult, ALU.mult, [eek, sck, oh1k], [Gk])
                if "G_dbg" in dr:
                    p.dma(dr["G_dbg"][c0 + jt * 128:c0 + (jt + 1) * 128, :], G[:], reads=[Gk])
                pst, pstk = B.bank(6)
                p.op("pe", lambda e, pst=pst, G=G: e.transpose(pst[0:32, 0:128], G[:], B.ident_f[:]), [Gk, "ident_f"], [pstk])
                B.cp(gT[:, so + jt * 128:so + (jt + 1) * 128], pst[0:32, 0:128], [pstk], [gTk], eng="act")
        def load_w(e_):
            wg, wgk = B.tmp("wg", [128, 8, 512], BF16, 3)
            wu, wuk = B.tmp("wu", [128, 8, 512], BF16, 3)
            wd, wdk = B.tmp("wd", [128, 4, 1024], BF16, 3)
            p.dma(wg[:], wg_d[e_, :, :, :], writes=[wgk], q="pool")
            p.dma(wu[:], wu_d[e_, :, :, :], writes=[wuk], q="pool")
            p.dma(wd[:], wd_d[e_, :, :, :], writes=[wdk], q="pool")
            return wg, wgk, wu, wuk, wd, wdk
        nxt_w = load_w(0)
        pend_down = None
        for e_ in range(32):
            wg, wgk, wu, wuk, wd, wdk = nxt_w
            if e_ + 1 < 32:
                nxt_w = load_w(e_ + 1)
            for si, (c0, n) in enumerate(subs):
                j = 1 if c0 == 0 else 0
                so = offs[si]
                xsk, h2k, gTk = "%s_%d" % (xsk0, si), "%s_%d" % (h2k0, si), "%s_%d" % (gTk0, si)
                pg_, pgk = B.bank(7)
                B.mm(pg_[:, 0:n], sel[:, e_, :], gT[:, so:so + n], True, True, [selk, gTk], [pgk])
                gb, gbk = B.tmp("gb", [128, 512], F32, 2)
                B.cp(gb[:, 0:n], pg_[:, 0:n], [pgk], [gbk], eng="act")
                aT, aTk = B.tmp("aT", [128, 4, 512], BF16, 3)
                for m in range(4):
                    pa, pak = B.bank(0 + (m % 2) * 2)
                    pu, puk = B.bank(1 + (m % 2) * 2)
                    for k in range(8):
                        B.mm(pa[:, 0:n], wg[:, k, m * 128:(m + 1) * 128], h2[:, k, so:so + n], k == 0, k == 7, [wgk, h2k], [pak])
                    for k in range(8):
                        B.mm(pu[:, 0:n], wu[:, k, m * 128:(m + 1) * 128], h2[:, k, so:so + n], k == 0, k == 7, [wuk, h2k], [puk])
                    sg, sgk = B.tmp("sg", [128, 512], F32, 3)
                    B.act(sg[:, 0:n], pa[:, 0:n], AF.Silu, [pak], [sgk])
                    B.tt(sg[:, 0:n], sg[:, 0:n], pu[:, 0:n], ALU.mult, [sgk, puk], [sgk])
                    B.tt(aT[:, m, 0:n], sg[:, 0:n], gb[:, 0:n], ALU.mult, [sgk, gbk], [aTk], eng="pool")

                def down(wd=wd, wdk=wdk, aT=aT, aTk=aTk, so=so, n=n, j=j, xsk=xsk):
                    for f in range(8):
                        pd, pdk = B.bank(4 + (f % 3))
                        for k in range(4):
                            B.mm(pd[:, 0:n], wd[:, k, f * 128:(f + 1) * 128], aT[:, k, 0:n], k == 0, k == 3, [wdk, aTk], [pdk])
                        B.stt(xs[:, f, so:so + n], pd[:, 0:n], B.mv[:, l, j, 5, f:f + 1], xs[:, f, so:so + n], ALU.mult, ALU.add,
                              [pdk, "mv", xsk], [xsk])
                if pend_down is not None:
                    pend_down()
                pend_down = down
        pend_down()
        for si, (c0, n) in enumerate(subs):
            so = offs[si]
            p.dma(xv[:, :, c0:c0 + n], xs[:, :, so:so + n], reads=["%s_%d" % (xsk0, si)])


def phase_final(B, xT_dram, out_dram):
    cfg, p, dr = B.cfg, B.p, B.dram
    nfin, nfk = B.tmp("nfin", [128, 8], F32, 1)
    p.dma(nfin[:], dr["nfin_p"][:, :], writes=[nfk])
    for (c0, n) in cfg.chunks(False):
        xt, xk = B.tmp("xT_a", [128, 8, 512], F32, 2)
        p.dma(xt[:, :, 0:n], xT_dram.rearrange("(k p) t -> p k t", p=128)[:, :, c0:c0 + n], writes=[xk])
        sq, sqk = B.tmp("fin_sq", [128, 8, 512], F32, 1)
        B.act(sq[:, :, 0:n], xt[:, :, 0:n], AF.Square, [xk], [sqk])
        ps, psk = B.bank(0)
        for k in range(8):
            B.mm(ps[:, 0:n], B.ones_f[:], sq[:, k, 0:n], k == 0, k == 7, ["ones_f", sqk], [psk])
        rs, rsk = B.rstd_from_sum(ps[:, 0:n], psk, float(D), n, "fin_rstd")
        o, ok_ = B.tmp("fin_o", [128, 8, 512], F32, 2)
        for k in range(8):
            B.stt(o[:, k, 0:n], xt[:, k, 0:n], nfin[:, k:k + 1], rs[:, 0:n], ALU.mult, ALU.mult, [xk, nfk, rsk], [ok_])
        p.dma(out_dram.rearrange("(k p) t -> p k t", p=128)[:, :, c0 - NCTX:c0 - NCTX + n], o[:, :, 0:n], reads=[ok_])


def declare_common(B, nl):
    B.din("c_in", [128, 8, 2], F32)
    B.din("nmix_p", [128, 4, 8], F32)
    B.din("nffn_p", [128, 4, 8], F32)
    B.din("bada_p", [128, 4, 48], F32)
    B.din("wada_p", [nl, 128, 8, 6144], F32)


def declare_A_even(B):
    c = B.cfg
    B.din("win_p", [128, 8, WIN_COLS], F32)
    B.din("wq_p", [128, 3, 1024], F32)
    B.din("wkv_p", [128, 2, 1024], F32)
    B.din("gains_e", [128, 12], F32)
    B.din("tabA", [128, 4, c.TOKX], F32)
    B.din("tabB", [128, 4, c.TOKX], F32)
    B.dout("QT_A_o", [8, 96, c.TOKX], BF16)
    B.dout("QT_B_o", [8, 64, c.TOKX], BF16)
    B.dout("KT_A_o", [8, 96, c.TOKX], BF16)
    B.dout("KT_B_o", [2, 64, c.TOKX], BF16)
    B.dout("V_A_o", [c.TOKX, 512], BF16)
    B.dout("V_B_o", [c.TOKX, 128], BF16)


def declare_A_odd(B):
    c = B.cfg
    B.din("wna_p", [128, 8, 3072], F32)
    B.dout("QT_C_o", [1024, c.TOKX], BF16)
    B.dout("KT_C_o", [1024, c.TOKX], BF16)
    B.dout("V_C_o", [c.TOKX, 1024], BF16)


def declare_att_even(B):
    c = B.cfg
    B.din("QT_A_i", [8, 96, c.TOKX], BF16)
    B.din("QT_B_i", [8, 64, c.TOKX], BF16)
    B.din("KT_A_f", [8, 96, c.NKEY], BF16)
    B.din("KT_B_f", [2, 64, c.NKEY], BF16)
    B.din("V_A_f", [c.NKEY, 512], BF16)
    B.din("V_B_f", [c.NKEY, 128], BF16)


def declare_att_odd(B):
    c = B.cfg
    B.din("QT_C_i", [1024, c.TOKX], BF16)
    B.din("KT_C_b", [1024, c.NKC], BF16)
    B.din("V_C_b", [c.NKC, 1024], BF16)
    B.din("btab", [16, 128, 27, 128], F32)


def declare_mix_ffn(B, dbg=False):
    c = B.cfg
    B.din("wout_p", [64, 16, 1024], F32)
    B.din("wr_p", [128, 8, 36], F32)
    B.din("br_p", [1, 36], F32)
    B.din("wg_p", [32, 128, 8, 512], F32)
    B.din("wu_p", [32, 128, 8, 512], F32)
    B.din("wd_p", [32, 128, 4, 1024], F32)
    B.dint("OT", [16, 64, c.TOKX], BF16)
    if dbg:
        B.dout("G_dbg", [c.TOKX, 32], F32)


def build_stage(cfg, stage, dbg=False, skip_moe=False):
    B = Bld(cfg)
    xT = B.din("xT", [1024, cfg.TOKX], F32)
    if stage == 0:
        declare_common(B, 1)
        declare_A_even(B)
        phase_modvec(B, [0]); B.phase_reset()
        phase_A_even(B, 0, xT); B.phase_reset()
    else:
        l = stage - 1
        last = stage == 4
        layers = [l] if last else [l, l + 1]
        declare_common(B, len(layers))
        if l % 2 == 0:
            declare_att_even(B)
        else:
            declare_att_odd(B)
        declare_mix_ffn(B, dbg)
        xo = B.dout("xT_o", [1024, cfg.TOKX], F32)
        if last:
            B.din("nfin_p", [128, 8], F32)
            outT = B.dout("outT", [1024, cfg.TOK], F32)
        elif (l + 1) % 2 == 0:
            declare_A_even(B)
        else:
            declare_A_odd(B)
        phase_modvec(B, layers); B.phase_reset()
        if l % 2 == 0:
            phase_att_even(B, not last)
        else:
            phase_att_odd(B, not last)
        B.phase_reset()
        phase_outproj(B, l, xT, xo, last); B.phase_reset()
        if not skip_moe:
            phase_ffn(B, l, xo, last); B.phase_reset()
        if last:
            phase_final(B, xo, outT)
        elif (l + 1) % 2 == 0:
            phase_A_even(B, l + 1, xo)
        else:
            phase_A_odd(B, l + 1, xo)
        B.phase_reset()
    B.p.emit()
    return B


def _ktile(w):
    K, N = w.shape
    return np.ascontiguousarray(w.reshape(K // 128, 128, N).transpose(1, 0, 2))


def prep_even(inp, i):
    w_in = inp["ab_w_in"][i]
    ev32, od32 = np.arange(0, 32, 2), np.arange(1, 32, 2)
    ev64, od64 = np.arange(0, 64, 2), np.arange(1, 64, 2)
    cols = list(range(0, 640))
    cols += list(640 + ev32) + list(640 + od32)
    cols += list(640 + od32) + list(640 + ev32)
    for h in range(8):
        cols += list(672 + h * 64 + ev64) + list(672 + h * 64 + od64)
    for h in range(8):
        cols += list(672 + h * 64 + od64) + list(672 + h * 64 + ev64)
    for h in range(2):
        cols += list(1184 + h * 64 + ev64) + list(1184 + h * 64 + od64)
    for h in range(2):
        cols += list(1184 + h * 64 + od64) + list(1184 + h * 64 + ev64)
    cols += list(range(1312, 1440))
    assert len(cols) == WIN_COLS
    wq = inp["mla_w_q_up"][i]
    qc = []
    for h in range(8):
        qc += list(h * 96 + np.arange(64)) + list(h * 96 + 64 + ev32) + list(h * 96 + 64 + od32)
    for h in range(8):
        qc += list(h * 96 + 64 + od32) + list(h * 96 + 64 + ev32)
    wkv = inp["mla_w_kv_up"][i]
    kc = []
    for h in range(8):
        kc += list(h * 128 + np.arange(64))
    for h in range(8):
        kc += list(h * 128 + 64 + np.arange(64))
    g = np.zeros((128, 12), np.float32)
    g[:, 0:3] = inp["mla_q_norm"][i].reshape(3, 128).T
    g[:, 3:5] = inp["mla_kv_norm"][i].reshape(2, 128).T
    gq, gk = inp["gqa_q_norm"][i], inp["gqa_k_norm"][i]
    g[:, 5] = np.tile(np.concatenate([gq[ev64], gq[od64]]), 2)
    g[:, 6] = np.tile(np.concatenate([gq[od64], gq[ev64]]), 2)
    g[:, 7] = np.tile(np.concatenate([gk[ev64], gk[od64]]), 2)
    g[:, 8] = np.tile(np.concatenate([gk[od64], gk[ev64]]), 2)
    return {"win_p": _ktile(w_in[:, cols]), "wq_p": _ktile(wq[:, qc]), "wkv_p": _ktile(wkv[:, kc]), "gains_e": g}


def rope_tables(cfg, c4):
    t = c4 * cfg.TOK + np.arange(cfg.TOK)
    r = (t // 64).astype(np.float32)
    col = (t % 64).astype(np.float32)

    def ang(rot):
        n = rot // 4
        inv = (10000.0 ** (-np.arange(n, dtype=np.float32) / n)).astype(np.float32)
        return np.concatenate([r[:, None] * inv, col[:, None] * inv], -1).astype(np.float32).T
    aA, aB = ang(32), ang(64)
    tabA = np.zeros((128, 4, cfg.TOKX), np.float32)
    tabB = np.zeros((128, 4, cfg.TOKX), np.float32)
    for tab, a, base, reps, qs in ((tabA, aA, 64, 1, 96.0 ** -0.5), (tabB, aB, 0, 2, 64.0 ** -0.5)):
        hp = a.shape[0]
        c, s = np.cos(a), np.sin(a)
        for rep in range(reps):
            o = base + rep * 2 * hp
            for blk, sgn in ((0, -1.0), (1, 1.0)):
                rows = slice(o + blk * hp, o + (blk + 1) * hp)
                tab[rows, 0, NCTX:] = c * qs
                tab[rows, 1, NCTX:] = sgn * s * qs
                tab[rows, 2, NCTX:] = c
                tab[rows, 3, NCTX:] = sgn * s
                tab[rows, 0, :NCTX] = qs
                tab[rows, 2, :NCTX] = 1.0
    return tabA, tabB


def bias_tables(cfg, rpb, c4):
    P = cfg.NP
    R0 = c4 * cfg.ROWS
    slots = [(2, u) for u in range(2, 7)] + [(0, u) for u in range(0, 6)] + [(1, u) for u in range(1, 6)] \
        + [(P - 2, u) for u in range(P - 2, P + 3)] + [(P - 1, u) for u in range(P - 2, P + 4)]
    kp = np.arange(128)
    q = np.arange(128)
    out = np.full((16, 128, 27, 128), NEG, np.float32)
    for si, (i, u) in enumerate(slots):
        kr = (R0 - 4 + 2 * u + kp // 64)[:, None]
        kc = (kp % 64)[:, None]
        r = (R0 + 2 * i + q // 64)[None, :]
        col = (q % 64)[None, :]
        rs = np.clip(r - 4, 0, cfg.SROWS - 8)
        cs = np.clip(col - 8, 0, 48)
        valid = (kr >= rs) & (kr < rs + 8) & (kc >= cs) & (kc < cs + 16)
        dr_ = np.clip(kr - r + 7, 0, 14)
        dc_ = np.clip(kc - col + 15, 0, 30)
        vals = rpb[:, dr_, dc_]
        out[:, :, si, :] = np.where(valid[None], vals, NEG)
    return out


def prep_moe(inp, l):
    return {
        "wr_p": _ktile(np.concatenate([inp["moe_w_rg"][l], inp["moe_w_re"][l]], 1)),
        "br_p": np.concatenate([inp["moe_b_rg"][l], inp["moe_b_re"][l]])[None, :].astype(np.float32),
        "wg_p": np.ascontiguousarray(inp["moe_w_gate"][l].reshape(32, 8, 128, 512).transpose(0, 2, 1, 3)),
        "wu_p": np.ascontiguousarray(inp["moe_w_up"][l].reshape(32, 8, 128, 512).transpose(0, 2, 1, 3)),
        "wd_p": np.ascontiguousarray(inp["moe_w_down"][l].reshape(32, 4, 128, 1024).transpose(0, 2, 1, 3)),
    }


def prep_cin(inp, b):
    return np.ascontiguousarray(np.stack([inp["c"][b].reshape(8, 128).T, inp["c_ctx"].reshape(8, 128).T], -1).astype(np.float32))


def prep_common(inp, layers):
    return {
        "nmix_p": np.ascontiguousarray(inp["norm_mix"].reshape(4, 8, 128).transpose(2, 0, 1)),
        "nffn_p": np.ascontiguousarray(inp["norm_ffn"].reshape(4, 8, 128).transpose(2, 0, 1)),
        "bada_p": np.ascontiguousarray(inp["b_ada"].reshape(4, 48, 128).transpose(2, 0, 1)),
        "wada_p": np.ascontiguousarray(np.stack([inp["w_ada"][l].reshape(8, 128, 6144).transpose(1, 0, 2) for l in layers])),
    }


def wout_p(w):
    return np.ascontiguousarray(w.reshape(16, 64, 1024).transpose(1, 0, 2))


N_CORES = 8
_PROG_CACHE = {}


def get_stage(cfg, stage, **kw):
    key = (cfg.SEQ, stage, tuple(sorted(kw.items())))
    if key not in _PROG_CACHE:
        _PROG_CACHE[key] = build_stage(cfg, stage, **kw)
    return _PROG_CACHE[key]


def run_stage(cfg, stage, in_maps, **kw):
    B = get_stage(cfg, stage, **kw)
    names = [n for n in B.dram]
    res = run_bass_kernel_spmd(B.nc, in_maps, core_ids=list(range(N_CORES)))
    return res.results


def exchange_even(cfg, outs):
    ins = []
    for b in range(2):
        cs = outs[b * CPB:(b + 1) * CPB]
        kta = np.concatenate([cs[0]["KT_A_o"][:, :, :NCTX]] + [c["KT_A_o"][:, :, NCTX:] for c in cs], 2)
        ktb = np.concatenate([cs[0]["KT_B_o"][:, :, :NCTX]] + [c["KT_B_o"][:, :, NCTX:] for c in cs], 2)
        va = np.concatenate([cs[0]["V_A_o"][:NCTX]] + [c["V_A_o"][NCTX:] for c in cs], 0)
        vb = np.concatenate([cs[0]["V_B_o"][:NCTX]] + [c["V_B_o"][NCTX:] for c in cs], 0)
        for c in cs:
            ins.append({"QT_A_i": c["QT_A_o"], "QT_B_i": c["QT_B_o"], "KT_A_f": kta, "KT_B_f": ktb, "V_A_f": va, "V_B_f": vb})
    return ins


def exchange_odd(cfg, outs):
    ins = []
    pad = 4 * 64
    for b in range(2):
        cs = outs[b * CPB:(b + 1) * CPB]
        kt = np.concatenate([c["KT_C_o"][:, NCTX:] for c in cs], 1)
        v = np.concatenate([c["V_C_o"][NCTX:] for c in cs], 0)
        ktp = np.concatenate([np.zeros((1024, pad), kt.dtype), kt, np.zeros((1024, pad), kt.dtype)], 1)
        vp = np.concatenate([np.zeros((pad, 1024), v.dtype), v, np.zeros((pad, 1024), v.dtype)], 0)
        for ci, c in enumerate(cs):
            a = ci * cfg.TOK
            n = (cfg.ROWS + 8) * 64
            ins.append({"QT_C_i": c["QT_C_o"],
                        "KT_C_b": np.ascontiguousarray(np.concatenate([c["KT_C_o"][:, :NCTX], ktp[:, a:a + n]], 1)),
                        "V_C_b": np.ascontiguousarray(np.concatenate([c["V_C_o"][:NCTX], vp[a:a + n]], 0))})
    return ins


def forward(inp, SEQ, upto=4, dbg=False, skip_moe=False, trace=None):
    cfg = Cfg(SEQ)
    inp = {k: np.asarray(v, np.float32) for k, v in inp.items()}
    xT = []
    for core in range(N_CORES):
        b, c4 = divmod(core, CPB)
        xl = inp["x"][b, c4 * cfg.TOK:(c4 + 1) * cfg.TOK]
        xT.append(np.ascontiguousarray(np.concatenate([inp["ctx"][b], xl], 0).T))
    tabs = [rope_tables(cfg, c % CPB) for c in range(N_CORES)]
    even = {i: prep_even(inp, i) for i in range(2)}
    extra = [None] * N_CORES
    results = None
    for stage in range(upto + 1):
        l = stage - 1
        last = stage == 4
        layers = [0] if stage == 0 else ([l] if last else [l, l + 1])
        maps = []
        shared = {}
        if stage >= 1:
            shared.update(prep_moe(inp, l))
            shared["wout_p"] = wout_p(inp["ab_w_out"][l // 2] if l % 2 == 0 else inp["na_w_out"][l // 2])
        nxt = stage if stage < 4 else None
        if nxt is not None:
            if nxt % 2 == 0:
                shared.update(even[nxt // 2])
            else:
                shared["wna_p"] = _ktile(inp["na_w_in"][nxt // 2])
        if last:
            shared["nfin_p"] = np.ascontiguousarray(inp["norm_final"].reshape(8, 128).T)
        shared.update(prep_common(inp, layers))
        cins = [prep_cin(inp, b) for b in range(2)]
        for core in range(N_CORES):
            b, c4 = divmod(core, CPB)
            m = dict(shared)
            m["c_in"] = cins[b]
            m["xT"] = xT[core]
            if nxt is not None and nxt % 2 == 0:
                m["tabA"], m["tabB"] = tabs[core]
            if stage >= 1:
                m.update(extra[core])
                if l % 2 == 1:
                    m["btab"] = bias_tables(cfg, inp["na_rpb"][l // 2], c4)
            maps.append(m)
        results = run_stage(cfg, stage, maps, **({"dbg": dbg, "skip_moe": skip_moe} if stage >= 1 else {}))
        if trace is not None:
            trace.append(results)
        if stage >= 1:
            xT = [r["xT_o"] for r in results]
        if nxt is not None:
            extra = exchange_even(cfg, results) if nxt % 2 == 0 else exchange_odd(cfg, results)
    if upto < 4:
        return results
    out = np.empty((2, SEQ, D), np.float32)
    for core in range(N_CORES):
        b, c4 = divmod(core, CPB)
        out[b, c4 * cfg.TOK:(c4 + 1) * cfg.TOK] = results[core]["outT"].T
    return out


GROUPS = [[0, 1, 2, 3], [4, 5, 6, 7]]


def na_slots(cfg):
    P = cfg.NP
    own = lambda us: [("own", u) for u in us]
    out = [(2, own(range(0, 5)))]
    out.append((0, [("edge", r, k) for r in range(CPB) for k in (2, 3)] + own(range(0, 4))))
    out.append((1, [("edge", r, 3) for r in range(CPB)] + own(range(0, 4))))
    out.append((P - 2, own(range(P - 4, P)) + [("edge", r, 0) for r in range(CPB)]))
    out.append((P - 1, own(range(P - 4, P)) + [("edge", r, k) for r in range(CPB) for k in (0, 1)]))
    return out


def na_tile_index(cfg, td):
    EB = cfg.TOKX // 128
    if td[0] == "own":
        return 2 + td[1]
    return EB + td[1] * 4 + td[2]


def bias_tables_fused(cfg, rpb, c4):
    R0 = c4 * cfg.ROWS
    erow = {0: 0, 1: 2, 2: cfg.ROWS - 4, 3: cfg.ROWS - 2}
    kp = np.arange(128)
    q = np.arange(128)
    slots = na_slots(cfg)
    nslot = sum(len(t) for _, t in slots)
    out = np.full((16, 128, nslot, 128), NEG, np.float32)
    si = 0
    for (i, tds) in slots:
        for td in tds:
            if td[0] == "own":
                krow0 = R0 + 2 * td[1]
                ok = True
            else:
                krow0 = td[1] * cfg.ROWS + erow[td[2]]
                ok = td[1] != c4
            kr = (krow0 + kp // 64)[:, None]
            kc = (kp % 64)[:, None]
            r = (R0 + 2 * i + q // 64)[None, :]
            col = (q % 64)[None, :]
            rs = np.clip(r - 4, 0, cfg.SROWS - 8)
            cs = np.clip(col - 8, 0, 48)
            valid = (kr >= rs) & (kr < rs + 8) & (kc >= cs) & (kc < cs + 16) & ok
            vals = rpb[:, np.clip(kr - r + 7, 0, 14), np.clip(kc - col + 15, 0, 30)]
            out[:, :, si, :] = np.where(valid[None], vals, NEG)
            si += 1
    return out


def phase_att_odd_fused(B, need_ctx):
    cfg, p, dr = B.cfg, B.p, B.dram
    OT = dr["OT"]
    TOKX = cfg.TOKX
    EB = TOKX // 128
    NT = EB + 4 * CPB
    NKF = TOKX + 512 * CPB
    slots = na_slots(cfg)
    nslot = sum(len(t) for _, t in slots)
    soff, o_ = {}, 0
    for (i, tds) in slots:
        soff[i] = o_
        o_ += len(tds)
    vts = [B.alloc("nvt%d" % i, [128, NT, 80], BF16) for i in range(2)]
    for i in range(2):
        p.op("pool", lambda e, i=i: e.memset(vts[i][:, :, 64:80], 1.0), writes=["nvt%d" % i])
    vown = dr["V_C_o"].rearrange("(t p) (h d) -> p t h d", p=128, d=64)
    vedge = dr["EV_g"].rearrange("(j r t p) (h d) -> j r p t h d", j=2, r=CPB, p=128, d=64)
    def load_head(h):
        kt, ktk = B.tmp("nkt", [64, NKF], BF16, 2)
        qt, qtk = B.tmp("nqt", [64, TOKX], BF16, 2)
        bt, btk = B.tmp("nbt", [128, nslot, 128], BF16, 2)
        vt, vtk = vts[h % 2], "nvt%d" % (h % 2)
        p.dma(kt[:, 0:TOKX], dr["KT_C_o"][h * 64:(h + 1) * 64, :], writes=[ktk])
        for r in range(CPB):
            eb = ((h // 8) * CPB + r) * 512 + (h % 8) * 64
            p.dma(kt[:, TOKX + r * 512:TOKX + (r + 1) * 512], dr["EK_g"][eb:eb + 64, :], writes=[ktk])
        p.dma(qt[:], dr["QT_C_o"][h * 64:(h + 1) * 64, :], writes=[qtk])
        p.dma(bt[:], dr["btab"][h, :, :, :], writes=[btk], q="pool")
        load_v(B, vt, vtk, vown[:, :, h, :], EB)
        for r in range(CPB):
            for j in range(2):
                p.dma(vt[:, EB + r * 4 + 2 * j:EB + r * 4 + 2 * j + 2, 0:64], vedge[j, r, :, :, h, :], writes=[vtk])
        return kt, ktk, qt, qtk, bt, btk, vt, vtk

    nxt = load_head(0)
    for h in range(16):
        kt, ktk, qt, qtk, bt, btk, vt, vtk = nxt
        if h + 1 < 16:
            nxt = load_head(h + 1)
        if need_ctx:
            attend_group(B, kt, ktk, 64, vt, vtk, qt, qtk, 0, NCTX, [(0, None), (1, None)], OT[h, :, 0:NCTX])
        for i in range(cfg.NP):
            if i in soff and i != 2:
                tds, so = dict(slots)[i], soff[i]
            else:
                tds, so = [("own", u) for u in range(i - 2, i + 3)], 0
            tiles = [(0, None), (1, None)] + [(na_tile_index(cfg, td), bt[:, so + ti, :]) for ti, td in enumerate(tds)]
            attend_group(B, kt, ktk, 64, vt, vtk, qt, qtk, NCTX + i * 128, 128, tiles,
                         OT[h, :, NCTX + i * 128:NCTX + (i + 1) * 128], bias_key=btk)


def build_fused(cfg, nlayers=4):
    B = Bld(cfg)
    c = cfg
    xT = B.din("xT", [1024, c.TOKX], F32)
    declare_common(B, 4)
    B.din("tabA", [128, 4, c.TOKX], F32)
    B.din("tabB", [128, 4, c.TOKX], F32)
    B.din("nfin_p", [128, 8], F32)
    nslot = sum(len(t) for _, t in na_slots(cfg))
    for i in range(2):
        B.din("win_p%d" % i, [128, 8, WIN_COLS], F32)
        B.din("wq_p%d" % i, [128, 3, 1024], F32)
        B.din("wkv_p%d" % i, [128, 2, 1024], F32)
        B.din("gains_e%d" % i, [128, 12], F32)
        B.din("wna_p%d" % i, [128, 8, 3072], F32)
        B.din("btab%d" % i, [16, 128, nslot, 128], F32)
    for l in range(4):
        B.din("wout_p%d" % l, [64, 16, 1024], F32)
        B.din("wr_p%d" % l, [128, 8, 36], F32)
        B.din("br_p%d" % l, [1, 36], F32)
        B.din("wg_p%d" % l, [32, 128, 8, 512], F32)
        B.din("wu_p%d" % l, [32, 128, 8, 512], F32)
        B.din("wd_p%d" % l, [32, 128, 4, 1024], F32)
    outT = B.dout("outT", [1024, c.TOK], F32)
    xw = B.dint("xw", [1024, c.TOKX], F32)
    B.dint("OT", [16, 64, c.TOKX], BF16)
    for nm_, shp in (("QT_A_o", [8, 96, c.TOKX]), ("QT_B_o", [8, 64, c.TOKX]), ("KT_A_o", [8, 96, c.TOKX]), ("KT_B_o", [2, 64, c.TOKX]),
                     ("V_A_o", [8 * c.TOKX, 64]), ("V_B_o", [2 * c.TOKX, 64]), ("KT_A_g", [8 * CPB * 96, c.TOKX]), ("KT_B_g", [2 * CPB * 64, c.TOKX]),
                     ("V_A_g", [8 * CPB * c.TOKX, 64]), ("V_B_g", [2 * CPB * c.TOKX, 64]),
                     ("QT_C_o", [1024, c.TOKX]), ("KT_C_o", [1024, c.TOKX]), ("V_C_o", [c.TOKX, 1024]),
                     ("EK_o", [1024, 512]), ("EV_o", [512, 1024]), ("EK_g", [2 * CPB * 512, 512]), ("EV_g", [2 * CPB * 256, 1024])):
        B.dint(nm_, shp, BF16)
    B.dram["V_hm"] = True
    dr = B.dram
    dr["QT_A_i"], dr["QT_B_i"] = dr["QT_A_o"], dr["QT_B_o"]
    p = B.p
    phase_modvec(B, list(range(nlayers))); B.phase_reset()
    for l in range(nlayers):
        last = l == 3
        i = l // 2
        xin = xT if l == 0 else xw
        for k in ("wout_p", "wr_p", "br_p", "wg_p", "wu_p", "wd_p"):
            dr[k] = dr["%s%d" % (k, l)]
        if l % 2 == 0:
            for k in ("win_p", "wq_p", "wkv_p", "gains_e"):
                dr[k] = dr["%s%d" % (k, i)]
            phase_A_even(B, l, xin); B.phase_reset()
            T = c.TOKX
            ka2, kb2 = dr["KT_A_o"].rearrange("h d t -> (h d) t"), dr["KT_B_o"].rearrange("h d t -> (h d) t")
            for h in range(8):
                p.coll("AllGather", GROUPS, ka2[h * 96:(h + 1) * 96, :], dr["KT_A_g"][h * CPB * 96:(h + 1) * CPB * 96, :], writes=["cc_ka%d" % h])
                p.coll("AllGather", GROUPS, dr["V_A_o"][h * T:(h + 1) * T, :], dr["V_A_g"][h * CPB * T:(h + 1) * CPB * T, :], writes=["cc_va%d" % h])
            for h in range(2):
                p.coll("AllGather", GROUPS, kb2[h * 64:(h + 1) * 64, :], dr["KT_B_g"][h * CPB * 64:(h + 1) * CPB * 64, :], writes=["cc_kb%d" % h])
                p.coll("AllGather", GROUPS, dr["V_B_o"][h * T:(h + 1) * T, :], dr["V_B_g"][h * CPB * T:(h + 1) * CPB * T, :], writes=["cc_vb%d" % h])
            B.phase_reset()
            phase_att_even(B, not last, gathered=True); B.phase_reset()
        else:
            dr["wna_p"], dr["btab"] = dr["wna_p%d" % i], dr["btab%d" % i]
            phase_A_odd(B, l, xin); B.phase_reset()
            for j in range(2):
                p.coll("AllGather", GROUPS, dr["EK_o"][j * 512:(j + 1) * 512, :], dr["EK_g"][j * CPB * 512:(j + 1) * CPB * 512, :], writes=["cc_ek%d" % j])
                p.coll("AllGather", GROUPS, dr["EV_o"][j * 256:(j + 1) * 256, :], dr["EV_g"][j * CPB * 256:(j + 1) * CPB * 256, :], writes=["cc_ev%d" % j])
            B.phase_reset()
            phase_att_odd_fused(B, not last); B.phase_reset()
        phase_outproj(B, l, xin, xw, last); B.phase_reset()
        phase_ffn(B, l, xw, last); B.phase_reset()
    if nlayers == 4:
        phase_final(B, xw, outT)
    B.phase_reset()
    B.p.emit()
    return B


def fused_inputs(inp, cfg):
    inp = {k: np.asarray(v, np.float32) for k, v in inp.items()}
    shared = prep_common(inp, [0, 1, 2, 3])
    shared["nfin_p"] = np.ascontiguousarray(inp["norm_final"].reshape(8, 128).T)
    for i in range(2):
        for k, v in prep_even(inp, i).items():
            shared["%s%d" % (k, i)] = v
        shared["wna_p%d" % i] = _ktile(inp["na_w_in"][i])
    for l in range(4):
        for k, v in prep_moe(inp, l).items():
            shared["%s%d" % (k, l)] = v
        shared["wout_p%d" % l] = wout_p(inp["ab_w_out"][l // 2] if l % 2 == 0 else inp["na_w_out"][l // 2])
    cins = [prep_cin(inp, b) for b in range(2)]
    tabs = [rope_tables(cfg, c4) for c4 in range(CPB)]
    bts = [[bias_tables_fused(cfg, inp["na_rpb"][i], c4) for i in range(2)] for c4 in range(CPB)]
    maps = []
    for core in range(N_CORES):
        b, c4 = divmod(core, CPB)
        m = dict(shared)
        m["c_in"] = cins[b]
        xl = inp["x"][b, c4 * cfg.TOK:(c4 + 1) * cfg.TOK]
        m["xT"] = np.ascontiguousarray(np.concatenate([inp["ctx"][b], xl], 0).T)
        m["tabA"], m["tabB"] = tabs[c4]
        m["btab0"], m["btab1"] = bts[c4]
        maps.append(m)
    return maps


def forward_fused(inp, SEQ):
    cfg = Cfg(SEQ)
    key = ("fused", SEQ)
    if key not in _PROG_CACHE:
        _PROG_CACHE[key] = build_fused(cfg)
    B = _PROG_CACHE[key]
    maps = fused_inputs(inp, cfg)
    res = run_bass_kernel_spmd(B.nc, maps, core_ids=list(range(N_CORES))).results
    out = np.empty((2, SEQ, D), np.float32)
    for core in range(N_CORES):
        b, c4 = divmod(core, CPB)
        out[b, c4 * cfg.TOK:(c4 + 1) * cfg.TOK] = res[core]["outT"].T
    return out


def kernel(**inputs):
    return forward_fused(inputs, 16384)
```
